# Optimizing a Trainium2 kernel written in Bass

```python
import math
import jax
import jax.numpy as jnp
from jax import lax
import numpy as np

D_MODEL = 4096
BATCH = 4
SEQ = 2048
DEPTH = 1
DEC_BATCH = 128
DEC_SEQ = 1
PAST_LEN = 16384
PAGE_SIZE = 128

N_META = 16
D_MIX = D_MODEL
D_SSM = D_MIX // 2
SSM_GROUP = 16
N_SSM_GROUPS = D_SSM // SSM_GROUP
SSM_STATE = 64
DT_MIN = 1e-3
DT_MAX = 1e-1
D_GLA = D_MIX - D_SSM
GLA_HEADS = 4
GLA_DK = D_GLA // 2 // GLA_HEADS
GLA_DV = D_GLA // GLA_HEADS
GLA_QK = GLA_HEADS * GLA_DK
GLA_GATE_RANK = 16
GLA_TAU = 16.0
GLA_CHUNK = 64
SPLIT_POINTS = (D_SSM, D_SSM + GLA_QK, D_SSM + 2 * GLA_QK, D_SSM + 2 * GLA_QK + D_GLA, D_SSM + 2 * GLA_QK + 2 * D_GLA)
D_IN = D_SSM + 2 * GLA_QK + 2 * D_GLA + GLA_GATE_RANK
PEER_HEADS = 8
PEER_NKEYS = 128
PEER_N = PEER_NKEYS * PEER_NKEYS
PEER_QDIM = 256
PEER_HALF = PEER_QDIM // 2
PEER_TOPK = 16
PEER_BLOCK = 64
EPS = 1e-6

kernel_name = 'hymba_s5_gla_peer_step'


def _rmsnorm(x, g):
    xf = x.astype(jnp.float32)
    y = xf * lax.rsqrt(jnp.mean(xf * xf, axis=-1, keepdims=True) + EPS)
    return (y * g.astype(jnp.float32)).astype(x.dtype)


def _complex_scan_op(e1, e2):
    a1r, a1i, b1r, b1i = e1
    a2r, a2i, b2r, b2i = e2
    return (a2r * a1r - a2i * a1i,
            a2r * a1i + a2i * a1r,
            a2r * b1r - a2i * b1i + b2r,
            a2r * b1i + a2i * b1r + b2i)


def _s5_mix(u, h0_re, h0_im, p):
    f32 = jnp.float32
    B, L, _ = u.shape
    uf = u.astype(f32)
    ug = uf.reshape(B, L, N_SSM_GROUPS, SSM_GROUP)
    lr = p['s5_lam_re'].astype(f32)
    li = p['s5_lam_im'].astype(f32)
    dt = jnp.exp(p['s5_log_dt'].astype(f32))[:, None]
    mag = jnp.exp(lr * dt)
    ar = mag * jnp.cos(li * dt)
    ai = mag * jnp.sin(li * dt)
    den = lr * lr + li * li
    nr = ar - 1.0
    qr = (nr * lr + ai * li) / den
    qi = (ai * lr - nr * li) / den
    b_re = p['s5_b_re'].astype(f32)
    b_im = p['s5_b_im'].astype(f32)
    bbr = qr[..., None] * b_re - qi[..., None] * b_im
    bbi = qr[..., None] * b_im + qi[..., None] * b_re
    xr = jnp.einsum('blgh,gph->blgp', ug, bbr)
    xi = jnp.einsum('blgh,gph->blgp', ug, bbi)
    h0r = h0_re.astype(f32)
    h0i = h0_im.astype(f32)
    xr = xr.at[:, 0].add(ar * h0r - ai * h0i)
    xi = xi.at[:, 0].add(ar * h0i + ai * h0r)
    Ar = jnp.broadcast_to(ar, xr.shape)
    Ai = jnp.broadcast_to(ai, xr.shape)
    _, _, hr, hi = lax.associative_scan(_complex_scan_op, (Ar, Ai, xr, xi), axis=1)
    y = (jnp.einsum('blgp,ghp->blgh', hr, p['s5_c_re'].astype(f32))
         - jnp.einsum('blgp,ghp->blgh', hi, p['s5_c_im'].astype(f32)))
    y = y.reshape(B, L, D_SSM) + p['s5_d'].astype(f32) * uf
    z = jax.nn.gelu(y)
    z = z * jax.nn.sigmoid(z @ p['s5_w_glu'].astype(f32) + p['s5_b_glu'].astype(f32))
    return z, hr[:, -1], hi[:, -1]


def _gla_chunk(S, q, k, v, lg):
    C = q.shape[1]
    b = jnp.cumsum(lg, axis=1)
    o = jnp.einsum('bihk,bhkv->bihv', q * jnp.exp(b), S)
    causal = (jnp.arange(C)[:, None] >= jnp.arange(C)[None, :])[None, :, :, None, None]
    diff = b[:, :, None] - b[:, None, :]
    decay = jnp.where(causal, jnp.exp(jnp.where(causal, diff, 0.0)), 0.0)
    scores = jnp.einsum('bihk,bjhk,bijhk->bijh', q, k, decay)
    o = o + jnp.einsum('bijh,bjhv->bihv', scores, v)
    bl = b[:, -1]
    S_new = jnp.exp(bl)[..., None] * S + jnp.einsum('bjhk,bjhv->bhkv', k * jnp.exp(bl[:, None] - b), v)
    return S_new, o


def _gla_blocks(S0, q, k, v, lg, chunk):
    B, L = q.shape[0], q.shape[1]
    n = L // chunk

    def to_blocks(t):
        return jnp.moveaxis(t.reshape((B, n, chunk) + t.shape[2:]), 1, 0)

    def step(S, inp):
        qc, kc, vc, gc = inp
        return _gla_chunk(S, qc, kc, vc, gc)

    S, o = lax.scan(step, S0, (to_blocks(q), to_blocks(k), to_blocks(v), to_blocks(lg)))
    return S, jnp.moveaxis(o, 0, 1).reshape((B, L) + o.shape[3:])


def _chunk_len(n):
    return GLA_CHUNK if n % GLA_CHUNK == 0 else n


def _peer(xn, w_q, keys, u_tab, v_tab):
    f32 = jnp.float32
    B, L, D = xn.shape
    T = B * L
    nblk = -(-T // PEER_BLOCK)
    xf = jnp.pad(xn.reshape(T, D), ((0, nblk * PEER_BLOCK - T), (0, 0)))
    k1 = keys[:, 0].astype(f32)
    k2 = keys[:, 1].astype(f32)

    def block(xb):
        nt = xb.shape[0]
        q = (xb @ w_q).astype(f32).reshape(nt, PEER_HEADS, 2, PEER_HALF)
        s1 = jnp.einsum('thc,hnc->thn', q[:, :, 0], k1)
        s2 = jnp.einsum('thc,hnc->thn', q[:, :, 1], k2)
        v1, i1 = lax.top_k(s1, PEER_TOPK)
        v2, i2 = lax.top_k(s2, PEER_TOPK)
        cand = (v1[..., :, None] + v2[..., None, :]).reshape(nt, PEER_HEADS, PEER_TOPK * PEER_TOPK)
        cid = (i1[..., :, None] * PEER_NKEYS + i2[..., None, :]).reshape(nt, PEER_HEADS, PEER_TOPK * PEER_TOPK)
        sc, sel = lax.top_k(cand, PEER_TOPK)
        eid = jnp.take_along_axis(cid, sel, axis=-1)
        g = jax.nn.softmax(sc, axis=-1)
        act = jax.nn.gelu(jnp.einsum('td,thkd->thk', xb, u_tab[eid]).astype(f32))
        return jnp.einsum('thk,thkd->td', (g * act).astype(xb.dtype), v_tab[eid])

    y = lax.map(block, xf.reshape(nblk, PEER_BLOCK, D))
    return y.reshape(nblk * PEER_BLOCK, D)[:T].reshape(B, L, D)


def _hybrid_layer(x, h0_re, h0_im, S0, n_lead, p):
    f32 = jnp.float32
    B, L, _ = x.shape
    xn = _rmsnorm(x, p['norm_mix_g'])
    proj = xn @ p['w_in']
    u, xq, xk, xv, xr, xg = jnp.split(proj, SPLIT_POINTS, axis=-1)
    y_ssm, hT_re, hT_im = _s5_mix(u, h0_re, h0_im, p)
    y_ssm = _rmsnorm(y_ssm, p['s5_norm_g'])
    lg = jax.nn.log_sigmoid((xg @ p['gla_w_gate2'] + p['gla_b_gate2']).astype(f32)) / GLA_TAU
    lg = lg.reshape(B, L, GLA_HEADS, GLA_DK)
    q = xq.astype(f32).reshape(B, L, GLA_HEADS, GLA_DK) * (GLA_DK ** -0.5)
    k = xk.astype(f32).reshape(B, L, GLA_HEADS, GLA_DK)
    v = xv.astype(f32).reshape(B, L, GLA_HEADS, GLA_DV)
    S0 = S0.astype(f32)
    if n_lead > 0:
        S_mid, o_lead = _gla_chunk(S0, q[:, :n_lead], k[:, :n_lead], v[:, :n_lead], lg[:, :n_lead])
        S_T, o_rest = _gla_blocks(S_mid, q[:, n_lead:], k[:, n_lead:], v[:, n_lead:], lg[:, n_lead:], _chunk_len(L - n_lead))
        o = jnp.concatenate([o_lead, o_rest], axis=1)
    else:
        S_T, o = _gla_blocks(S0, q, k, v, lg, _chunk_len(L))
    o = o * lax.rsqrt(jnp.mean(o * o, axis=-1, keepdims=True) + EPS)
    o = o.reshape(B, L, D_GLA) * p['gla_norm_g'].astype(f32) * jax.nn.silu(xr.astype(f32))
    mix = jnp.concatenate([y_ssm.astype(x.dtype), o.astype(x.dtype)], axis=-1) @ p['w_out']
    h = x + mix.astype(x.dtype)
    h = h + _peer(_rmsnorm(h, p['norm_ffn_g']), p['peer_w_q'], p['peer_keys'], p['peer_u'], p['peer_v']).astype(x.dtype)
    return h, hT_re, hT_im, S_T


def setup_inputs(seed: int = 0) -> dict:
    key = jax.random.key(seed)
    ks = jax.random.split(key, 32)
    nrm = jax.random.normal
    f32 = jnp.float32
    G, P, Hs = N_SSM_GROUPS, SSM_STATE, SSM_GROUP
    lam_im0 = jnp.pi * jnp.arange(P, dtype=f32)
    return {
        'x_prompt': nrm(ks[0], (BATCH, SEQ, D_MODEL), f32),
        'x_sample': nrm(ks[1], (DEC_BATCH, DEC_SEQ, D_MODEL), f32),
        'state_s5_re': 0.5 * nrm(ks[2], (DEPTH, DEC_BATCH, G, P), f32),
        'state_s5_im': 0.5 * nrm(ks[3], (DEPTH, DEC_BATCH, G, P), f32),
        'state_gla': 0.5 * nrm(ks[4], (DEPTH, DEC_BATCH, GLA_HEADS, GLA_DK, GLA_DV), f32),
        'meta_tokens': nrm(ks[5], (N_META, D_MODEL), f32),
        'norm_mix_g': 1.0 + 0.02 * nrm(ks[6], (DEPTH, D_MODEL), f32),
        'w_in': nrm(ks[7], (DEPTH, D_MODEL, D_IN), f32) * D_MODEL ** -0.5,
        's5_lam_re': -0.5 + 0.01 * nrm(ks[8], (DEPTH, G, P), f32),
        's5_lam_im': lam_im0 + 0.01 * nrm(ks[9], (DEPTH, G, P), f32),
        's5_log_dt': jax.random.uniform(ks[10], (DEPTH, G), f32, math.log(DT_MIN), math.log(DT_MAX)),
        's5_b_re': nrm(ks[11], (DEPTH, G, P, Hs), f32) * (2 * Hs) ** -0.5,
        's5_b_im': nrm(ks[12], (DEPTH, G, P, Hs), f32) * (2 * Hs) ** -0.5,
        's5_c_re': nrm(ks[13], (DEPTH, G, Hs, P), f32) * (2 * P) ** -0.5,
        's5_c_im': nrm(ks[14], (DEPTH, G, Hs, P), f32) * (2 * P) ** -0.5,
        's5_d': nrm(ks[15], (DEPTH, D_SSM), f32),
        's5_w_glu': nrm(ks[16], (DEPTH, D_SSM, D_SSM), f32) * D_SSM ** -0.5,
        's5_b_glu': 0.01 * nrm(ks[17], (DEPTH, D_SSM), f32),
        's5_norm_g': 1.0 + 0.02 * nrm(ks[18], (DEPTH, D_SSM), f32),
        'gla_w_gate2': nrm(ks[19], (DEPTH, GLA_GATE_RANK, GLA_QK), f32) * GLA_GATE_RANK ** -0.5,
        'gla_b_gate2': 0.5 * nrm(ks[20], (DEPTH, GLA_QK), f32),
        'gla_norm_g': 1.0 + 0.02 * nrm(ks[21], (DEPTH, D_GLA), f32),
        'w_out': nrm(ks[22], (DEPTH, D_MIX, D_MODEL), f32) * D_MIX ** -0.5,
        'norm_ffn_g': 1.0 + 0.02 * nrm(ks[23], (DEPTH, D_MODEL), f32),
        'peer_w_q': nrm(ks[24], (DEPTH, D_MODEL, PEER_HEADS * PEER_QDIM), f32) * D_MODEL ** -0.5,
        'peer_keys': nrm(ks[25], (DEPTH, PEER_HEADS, 2, PEER_NKEYS, PEER_HALF), f32) * PEER_HALF ** -0.5,
        'peer_u': nrm(ks[26], (DEPTH, PEER_N, D_MODEL), f32) * D_MODEL ** -0.5,
        'peer_v': 0.3 * nrm(ks[27], (DEPTH, PEER_N, D_MODEL), f32),
        'norm_final_g': 1.0 + 0.02 * nrm(ks[28], (D_MODEL,), f32),
    }


def reference(x_prompt, x_sample, state_s5_re, state_s5_im, state_gla, meta_tokens,
              norm_mix_g, w_in, s5_lam_re, s5_lam_im, s5_log_dt, s5_b_re, s5_b_im,
              s5_c_re, s5_c_im, s5_d, s5_w_glu, s5_b_glu, s5_norm_g,
              gla_w_gate2, gla_b_gate2, gla_norm_g, w_out, norm_ffn_g,
              peer_w_q, peer_keys, peer_u, peer_v, norm_final_g):
    f32 = jnp.float32
    B = x_prompt.shape[0]
    meta = jnp.broadcast_to(meta_tokens[None].astype(x_prompt.dtype), (B, N_META, D_MODEL))
    hp = jnp.concatenate([meta, x_prompt], axis=1)
    hs = x_sample
    zero_s5 = jnp.zeros((B, N_SSM_GROUPS, SSM_STATE), f32)
    zero_gla = jnp.zeros((B, GLA_HEADS, GLA_DK, GLA_DV), f32)
    sp_re, sp_im, sp_gla, ss_re, ss_im, ss_gla = [], [], [], [], [], []
    for l in range(DEPTH):
        p = {
            'norm_mix_g': norm_mix_g[l], 'w_in': w_in[l],
            's5_lam_re': s5_lam_re[l], 's5_lam_im': s5_lam_im[l], 's5_log_dt': s5_log_dt[l],
            's5_b_re': s5_b_re[l], 's5_b_im': s5_b_im[l], 's5_c_re': s5_c_re[l], 's5_c_im': s5_c_im[l],
            's5_d': s5_d[l], 's5_w_glu': s5_w_glu[l], 's5_b_glu': s5_b_glu[l], 's5_norm_g': s5_norm_g[l],
            'gla_w_gate2': gla_w_gate2[l], 'gla_b_gate2': gla_b_gate2[l], 'gla_norm_g': gla_norm_g[l],
            'w_out': w_out[l], 'norm_ffn_g': norm_ffn_g[l],
            'peer_w_q': peer_w_q[l], 'peer_keys': peer_keys[l], 'peer_u': peer_u[l], 'peer_v': peer_v[l],
        }
        hp, pr, pi_, pg = _hybrid_layer(hp, zero_s5, zero_s5, zero_gla, N_META, p)
        hs, sr, si, sg = _hybrid_layer(hs, state_s5_re[l], state_s5_im[l], state_gla[l], 0, p)
        sp_re.append(pr)
        sp_im.append(pi_)
        sp_gla.append(pg)
        ss_re.append(sr)
        ss_im.append(si)
        ss_gla.append(sg)
    y_prompt = _rmsnorm(hp, norm_final_g)[:, N_META:]
    y_sample = _rmsnorm(hs, norm_final_g)
    return (y_prompt, y_sample,
            jnp.stack(sp_re, 0), jnp.stack(sp_im, 0), jnp.stack(sp_gla, 0),
            jnp.stack(ss_re, 0), jnp.stack(ss_im, 0), jnp.stack(ss_gla, 0))
```

```python
import contextlib
import numpy as np
import concourse.bass as bass
import concourse.mybir as mybir
from concourse.bass_utils import run_bass_kernel_spmd

F32 = mybir.dt.float32
BF16 = mybir.dt.bfloat16
I32 = mybir.dt.int32
U32 = mybir.dt.uint32
ALU = mybir.AluOpType
AF = mybir.ActivationFunctionType
AX = mybir.AxisListType

D = 4096
DIN = 8208
NT = 1056
LO = 1040
NS = 16
NPF = 1024
EPS = 1e-6
TWO_PI = 6.283185307179586
C1 = 6.28125
C2 = TWO_PI - C1
PI = 3.141592653589793

OWN_TILES = [(i * 128, 128) for i in range(8)] + [(1024, 32)]
PRE_TILES = [(i * 128, 128) for i in range(8)]
OWN_PIECES = [(0, 352), (352, 352), (704, 352)]
PRE_PIECES = [(0, 512), (512, 512)]
OWN_CHUNKS = [(0, 16)] + [(16 + 128 * i, 128) for i in range(8)]
PRE_CHUNKS = [(128 * i, 128) for i in range(8)]

CO_ID, CO_JP, CO_MT, CO_IO, CO_MR, CO_CM, CO_ON = 0, 128, 256, 384, 640, 648, 1672
CO_M2 = 1800
CO_SEL = 1800 + 512
NCST = 1800 + 512 + 128


def _consts():
    c = np.zeros((128, NCST), np.float32)
    p = np.arange(128)
    c[p, CO_ID + p] = 1.0
    c[p, CO_JP + (p + 64) % 128] = 1.0
    c[:, CO_MT:CO_MT + 128] = (p[None, :] >= p[:, None]).astype(np.float32)
    c[:, CO_IO:CO_IO + 256] = np.arange(256, dtype=np.float32)[None, :]
    for gl in range(8):
        c[:, CO_MR + gl] = (p // 16 == gl)
        c[:, CO_CM + gl * 128: CO_CM + (gl + 1) * 128] = (p[None, :] // 16 == gl)
    c[:, CO_ON:CO_ON + 128] = 1.0
    for prl in range(4):
        c[0:64, CO_M2 + prl * 128: CO_M2 + (prl + 1) * 128] = (p[None, :] // 16 == 2 * prl)
        c[64:128, CO_M2 + prl * 128: CO_M2 + (prl + 1) * 128] = (p[None, :] // 16 == 2 * prl + 1)
    pr = np.arange(64)
    c[:, CO_SEL:CO_SEL + 64] = (p[:, None] == 2 * pr[None, :])
    c[:, CO_SEL + 64:CO_SEL + 128] = (p[:, None] == 2 * pr[None, :] + 1)
    return c


class Buf:
    __slots__ = ("w", "r")

    def __init__(self):
        self.w = None
        self.r = {}


class T:
    def __init__(self, t):
        self.t = t
        self.b = Buf()

    def __getitem__(self, k):
        return self.t[k]


class KB:
    def __init__(self, nc, es):
        self.nc = nc
        self.es = [es]
        self.eng = {"pe": nc.tensor, "dve": nc.vector, "act": nc.scalar, "pool": nc.gpsimd, "sp": nc.sync}
        self.sem = {}
        self.cnt = {}
        self.waited = {e: {} for e in self.eng}
        for e in ("pe", "dve", "act", "pool"):
            self.sem[e] = es.enter_context(nc.semaphore("s_" + e))
            self.cnt[e] = 0
        self.dq = {}
        for q, n in (("sp", 8), ("pool", 6), ("act", 2)):
            keys = []
            for i in range(n):
                k = "d_%s%d" % (q, i)
                self.sem[k] = es.enter_context(nc.semaphore(k))
                self.cnt[k] = 0
                keys.append(k)
            self.dq[q] = [keys, 0]
        self.nid = 0
        self.rot = list(range(8))
        self.roti = 0

    def sb(self, shape, dt, name=None):
        self.nid += 1
        return T(self.es[-1].enter_context(self.nc.sbuf_tensor(name or ("t%d" % self.nid), list(shape), dt)))

    @contextlib.contextmanager
    def scope(self):
        es = contextlib.ExitStack()
        self.es.append(es)
        try:
            with es:
                yield
                self.barrier()
        finally:
            self.es.pop()

    def _wait(self, en, key, val):
        if val <= 0 or self.waited[en].get(key, 0) >= val:
            return
        self.eng[en].wait_ge(self.sem[key], val)
        self.waited[en][key] = val

    def _deps(self, en, r, w, acc):
        deps = {}

        def add(st):
            if st is not None:
                deps[st[0]] = max(deps.get(st[0], 0), st[1])
        for b in r:
            add(b.w)
        for b in w:
            if not acc:
                add(b.w)
            for k, v in b.r.items():
                add((k, v))
        for k, v in deps.items():
            if k == "pe" and en == "pe":
                continue
            self._wait(en, k, v)

    @staticmethod
    def _bufs(xs):
        return [x if isinstance(x, Buf) else x.b for x in xs]

    def op(self, en, fn, r=(), w=(), acc=False, inc=True):
        r = self._bufs(r)
        w = self._bufs(w)
        self._deps(en, r, w, acc)
        inst = fn(self.eng[en])
        if inc:
            self.cnt[en] += 1
            inst.then_inc(self.sem[en], 1)
            c = self.cnt[en]
        else:
            c = self.cnt[en] + 1
        for b in r:
            b.r[en] = c
        for b in w:
            b.w = (en, c)
            b.r = {}

    def V(self, fn, r=(), w=()):
        self.op("dve", fn, r, w)

    def A(self, fn, r=(), w=()):
        self.op("act", fn, r, w)

    def G(self, fn, r=(), w=()):
        self.op("pool", fn, r, w)

    def P(self, fn, r=(), w=(), acc=False, inc=True):
        self.op("pe", fn, r, w, acc, inc)

    def dma(self, q, out, in_, r=(), w=(), indirect=None):
        r = self._bufs(r)
        w = self._bufs(w)
        self._deps(q, r, w, False)
        keys, i = self.dq[q]
        key = keys[i % len(keys)]
        self.dq[q][1] = i + 1
        if indirect is not None:
            inst = self.eng[q].indirect_dma_start(out=out, out_offset=None, in_=in_, in_offset=indirect)
        else:
            inst = self.eng[q].dma_start(out=out, in_=in_)
        self.cnt[key] += 16
        inst.then_inc(self.sem[key], 16)
        c = self.cnt[key]
        for b in r:
            b.r[key] = c
        for b in w:
            b.w = (key, c)
            b.r = {}

    def barrier(self):
        for en in self.eng:
            for key in self.sem:
                self._wait(en, key, self.cnt[key])

    def finish(self):
        for key in self.sem:
            self._wait("sp", key, self.cnt[key])


_PH = 3


def build():
    nc = bass.Bass("TRN2", target_bir_lowering=False)

    def din(name, shape, dt=F32):
        return nc.dram_tensor(name, list(shape), dt, kind="ExternalInput").ap()

    def dout(name, shape, dt=F32):
        return nc.dram_tensor(name, list(shape), dt, kind="ExternalOutput").ap()

    xo = din("xo", [NT, D])
    xp = din("xp", [NPF, D])
    s5re = din("s5re", [NS, 128, 64])
    s5im = din("s5im", [NS, 128, 64])
    sgla = din("sgla", [NS, 4, 256, 512])
    cst_d = din("cst", [128, NCST])
    norm_mix_g = din("norm_mix_g", [D])
    w_in = din("w_in", [D, DIN])
    lam_re = din("lam_re", [128, 64])
    lam_im = din("lam_im", [128, 64])
    log_dt = din("log_dt", [128])
    b_re = din("b_re", [128, 64, 16])
    b_im = din("b_im", [128, 64, 16])
    c_re = din("c_re", [128, 16, 64])
    c_im = din("c_im", [128, 16, 64])
    s5_d = din("s5_d", [2048])
    w_glu = din("w_glu", [2048, 2048])
    b_glu = din("b_glu", [2048])
    s5_norm_g = din("s5_norm_g", [2048])
    w_gate2 = din("w_gate2", [16, 1024])
    b_gate2 = din("b_gate2", [1024])
    gla_norm_g = din("gla_norm_g", [2048])
    w_out = din("w_out", [D, D])
    norm_ffn_g = din("norm_ffn_g", [D])
    w_q = din("w_q", [D, 2048])
    keys = din("keys", [16, 128, 128])
    peer_u = din("peer_u", [16384, D])
    peer_v = din("peer_v", [16384, D])
    norm_final_g = din("norm_final_g", [D])

    yo = dout("yo", [NT, D])
    o_s5 = dout("o_s5", [2, 64, 128])
    o_gla = dout("o_gla", [8, 128, 512])
    o_s5s = dout("o_s5s", [NS, 2, 64, 128])
    o_glas = dout("o_glas", [NS, 8, 128, 512])
    def dscr(name, shape, dt=F32):
        return nc.dram_tensor(name, list(shape), dt, kind="Internal").ap()

    h_scr = dscr("h_scr", [NT, D])
    z_scr = dscr("z_scr", [128, 16, NT], BF16)
    v_scr = dscr("v_scr", [NT, 2048], BF16)
    sr_scr = dscr("sr_scr", [NT, 2048], BF16)
    vp_scr = dscr("vp_scr", [NPF, 2048], BF16)
    u_scr = dscr("u_scr", [16, 128, NT])
    up_scr = dscr("up_scr", [16, 128, NPF])
    ub_scr = dscr("ub_scr", [16384, D], BF16)
    vb_scr = dscr("vb_scr", [16384, D], BF16)
    B_ub, B_vb = Buf(), Buf()
    B_h, B_z, B_v, B_sr, B_vp, B_u, B_up = Buf(), Buf(), Buf(), Buf(), Buf(), Buf(), Buf()

    es = contextlib.ExitStack()
    with es:
        kb = KB(nc, es)
        V, A, G, P, dma = kb.V, kb.A, kb.G, kb.P, kb.dma
        big = [es.enter_context(nc.psum_tensor("pbig%d" % i, [128, 1024], F32)) for i in range(4)]

        class PB:
            def __init__(self, t, o):
                self.t_ = t
                self.o = o
                self.b = Buf()

            def __getitem__(self, key):
                rows, colsl = key
                a = colsl.start or 0
                bnd = 512 if colsl.stop is None else colsl.stop
                return self.t_[rows, self.o + a:self.o + bnd]
        banks = [PB(big[i // 2], 512 * (i % 2)) for i in range(8)]

        def nextbank():
            b = banks[kb.rot[kb.roti % len(kb.rot)]]
            kb.roti += 1
            return b

        cst = kb.sb([128, NCST], F32, "cst_sb")
        dma("sp", cst[:], cst_d[:, :], w=[cst])
        ident = cst.t[:, CO_ID:CO_ID + 128]
        jperm = cst.t[:, CO_JP:CO_JP + 128]
        iota256 = cst.t[:, CO_IO:CO_IO + 256]
        ones = cst.t[:, CO_ON:CO_ON + 128]
        identb = kb.sb([128, 128], BF16, "identb")
        V(lambda e: e.tensor_copy(out=identb[:], in_=ident), r=[cst], w=[identb])
        cols = kb.sb([128, 64], F32, "cols")
        HIN = kb.sb([128, 128], F32, "HIN")
        HINI = kb.sb([128, 128], F32, "HINI")
        Sh = [None]
        conv_jobs = [(ub_scr, peer_u, B_ub, i) for i in range(64)] + [(vb_scr, peer_v, B_vb, i) for i in range(64)]

        def conv_step(nmax=1):
            for _ in range(nmax):
                if not conv_jobs:
                    return
                dst, srcd, bb, i = conv_jobs.pop(0)
                dma("pool", dst[256 * i:256 * (i + 1), :], srcd[256 * i:256 * (i + 1), :], w=[bb])
        wsl = []
        wctr = [0]

        @contextlib.contextmanager
        def wslots(n):
            with kb.scope():
                wsl[:] = [kb.sb([128, 32, 256], BF16) for _ in range(n)]
                yield
                wsl[:] = []

        def colvec(vec, nt, dst_ap, neg=False):
            tmp = kb.sb([16, 128], F32)
            dma("sp", tmp[:nt, :], vec.rearrange("(j p) -> j p", p=128), w=[tmp])
            bk = nextbank()
            P(lambda e: e.matmul(bk[:, :nt], lhsT=tmp[:nt, :], rhs=cst.t[:nt, CO_ID:CO_ID + nt], start=True, stop=True),
              r=[tmp, cst], w=[bk])
            if neg:
                V(lambda e: e.tensor_scalar(out=dst_ap, in0=bk[:, :nt], scalar1=-1.0, scalar2=None, op0=ALU.mult),
                  r=[bk], w=[cols])
            else:
                V(lambda e: e.tensor_copy(out=dst_ap, in_=bk[:, :nt]), r=[bk], w=[cols])

        def transp32(dst_ap, dstT, src_ap, srcT):
            bk = nextbank()
            P(lambda e: e.matmul(bk[:, :128], lhsT=src_ap, rhs=ident, start=True, stop=True), r=[srcT, cst], w=[bk])
            A(lambda e: e.activation(out=dst_ap, in_=bk[:, :128], func=AF.Copy), r=[bk], w=[dstT])

        with kb.scope():
            colvec(s5_d, 16, cols.t[:, 0:16])
            colvec(b_glu, 16, cols.t[:, 16:32])
            colvec(s5_norm_g, 16, cols.t[:, 32:48])
            colvec(b_gate2, 8, cols.t[:, 48:56], neg=True)

        def s5_setup(PRV, PIV, BT, CT, AH0, PRP, PIP, NPIP, CTI, AH0I):
          with kb.scope():
            PRV = kb.sb([128, 11, 128], F32, "PRV")
            PIV = kb.sb([128, 11, 128], F32, "PIV")
            N1 = kb.sb([128, 128], F32)
            N2 = kb.sb([128, 128], F32)
            LR = kb.sb([128, 128], F32)
            LI = kb.sb([128, 128], F32)
            DT = kb.sb([128, 128], F32)
            RD = kb.sb([128, 128], F32)
            TH = kb.sb([128, 128], F32)
            AIpm = kb.sb([128, 128], F32)
            dma("sp", N1[:, 0:64], lam_re[:, :], w=[N1])
            dma("sp", N1[:, 64:128], lam_re[:, :], w=[N1])
            dma("sp", N2[:, 0:64], lam_im[:, :], w=[N2])
            dma("sp", N2[:, 64:128], lam_im[:, :], w=[N2])
            transp32(LR[:], LR, N1[:], N1)
            transp32(LI[:], LI, N2[:], N2)
            dma("sp", DT[:], log_dt.partition_broadcast(128), w=[DT])
            A(lambda e: e.activation(out=DT[:], in_=DT[:], func=AF.Exp), r=[DT], w=[DT])
            V(lambda e: e.tensor_tensor(out=RD[:], in0=LR[:], in1=DT[:], op=ALU.mult), r=[LR, DT], w=[RD])
            V(lambda e: e.tensor_tensor(out=TH[:], in0=LI[:], in1=DT[:], op=ALU.mult), r=[LI, DT], w=[TH])
            mag = kb.sb([128, 128], F32)
            thk = kb.sb([128, 128], F32)
            xs_ = kb.sb([128, 128], F32)
            ni = kb.sb([128, 128], I32)
            nf = kb.sb([128, 128], F32)
            rr = kb.sb([128, 128], F32)
            sn = kb.sb([128, 128], F32)
            cs = kb.sb([128, 128], F32)
            AIu = kb.sb([128, 128], F32)

            def sin_of(dst, src, shift):
                V(lambda e: e.tensor_scalar(out=xs_[:], in0=src[:], scalar1=shift, scalar2=1.0 / TWO_PI, op0=ALU.add, op1=ALU.mult),
                  r=[src], w=[xs_])
                V(lambda e: e.tensor_copy(out=ni[:], in_=xs_[:]), r=[xs_], w=[ni])
                V(lambda e: e.tensor_copy(out=nf[:], in_=ni[:]), r=[ni], w=[nf])
                V(lambda e: e.tensor_scalar(out=xs_[:], in0=src[:], scalar1=shift, scalar2=None, op0=ALU.add), r=[src], w=[xs_])
                V(lambda e: e.scalar_tensor_tensor(out=rr[:], in0=nf[:], scalar=-C1, in1=xs_[:], op0=ALU.mult, op1=ALU.add),
                  r=[nf, xs_], w=[rr])
                V(lambda e: e.scalar_tensor_tensor(out=rr[:], in0=nf[:], scalar=-C2, in1=rr[:], op0=ALU.mult, op1=ALU.add),
                  r=[nf, rr], w=[rr])
                V(lambda e: e.tensor_scalar(out=rr[:], in0=rr[:], scalar1=PI, scalar2=-PI, op0=ALU.min, op1=ALU.max), r=[rr], w=[rr])
                A(lambda e: e.activation(out=dst[:], in_=rr[:], func=AF.Sin), r=[rr], w=[dst])

            for k in range(11):
                sc = float(1 << k)
                A(lambda e: e.activation(out=mag[:], in_=RD[:], func=AF.Exp, scale=sc), r=[RD], w=[mag])
                V(lambda e: e.tensor_scalar(out=thk[:], in0=TH[:], scalar1=sc, scalar2=None, op0=ALU.mult), r=[TH], w=[thk])
                sin_of(sn, thk, 0.0)
                sin_of(cs, thk, PI / 2)
                V(lambda e: e.tensor_tensor(out=PRV[:, k, :], in0=mag[:], in1=cs[:], op=ALU.mult), r=[mag, cs], w=[PRV])
                V(lambda e: e.tensor_tensor(out=PIV[:, k, :], in0=mag[:], in1=sn[:], op=ALU.mult), r=[mag, sn], w=[PIV])
                if k == 0:
                    V(lambda e: e.tensor_copy(out=AIu[:], in_=PIV[:, 0, :]), r=[PIV], w=[AIu])
                V(lambda e: e.tensor_scalar(out=PIV[0:64, k, :], in0=PIV[0:64, k, :], scalar1=-1.0, scalar2=None, op0=ALU.mult),
                  r=[PIV], w=[PIV])
                if k == 0:
                    V(lambda e: e.tensor_copy(out=AIpm[:], in_=PIV[:, 0, :]), r=[PIV], w=[AIpm])
            den = kb.sb([128, 128], F32)
            t1 = kb.sb([128, 128], F32)
            t2 = kb.sb([128, 128], F32)
            nr = kb.sb([128, 128], F32)
            QR = kb.sb([128, 128], F32)
            QI = kb.sb([128, 128], F32)
            V(lambda e: e.tensor_tensor(out=den[:], in0=LR[:], in1=LR[:], op=ALU.mult), r=[LR], w=[den])
            V(lambda e: e.tensor_tensor(out=t1[:], in0=LI[:], in1=LI[:], op=ALU.mult), r=[LI], w=[t1])
            V(lambda e: e.tensor_tensor(out=den[:], in0=den[:], in1=t1[:], op=ALU.add), r=[den, t1], w=[den])
            V(lambda e: e.reciprocal(out=den[:], in_=den[:]), r=[den], w=[den])
            V(lambda e: e.tensor_scalar(out=nr[:], in0=PRV[:, 0, :], scalar1=-1.0, scalar2=None, op0=ALU.add), r=[PRV], w=[nr])
            V(lambda e: e.tensor_tensor(out=t1[:], in0=nr[:], in1=LR[:], op=ALU.mult), r=[nr, LR], w=[t1])
            V(lambda e: e.tensor_tensor(out=t2[:], in0=AIu[:], in1=LI[:], op=ALU.mult), r=[AIu, LI], w=[t2])
            V(lambda e: e.tensor_tensor(out=t1[:], in0=t1[:], in1=t2[:], op=ALU.add), r=[t1, t2], w=[t1])
            V(lambda e: e.tensor_tensor(out=QR[:], in0=t1[:], in1=den[:], op=ALU.mult), r=[t1, den], w=[QR])
            V(lambda e: e.tensor_tensor(out=t1[:], in0=AIu[:], in1=LR[:], op=ALU.mult), r=[AIu, LR], w=[t1])
            V(lambda e: e.tensor_tensor(out=t2[:], in0=nr[:], in1=LI[:], op=ALU.mult), r=[nr, LI], w=[t2])
            V(lambda e: e.tensor_tensor(out=t1[:], in0=t1[:], in1=t2[:], op=ALU.subtract), r=[t1, t2], w=[t1])
            V(lambda e: e.tensor_tensor(out=QI[:], in0=t1[:], in1=den[:], op=ALU.mult), r=[t1, den], w=[QI])
            V(lambda e: e.tensor_scalar(out=QI[0:64, :], in0=QI[0:64, :], scalar1=-1.0, scalar2=None, op0=ALU.mult), r=[QI], w=[QI])
            Bsame = kb.sb([128, 128, 16], F32)
            Bswap = kb.sb([128, 128, 16], F32)
            bre_v = b_re.rearrange("g p h -> p g h")
            bim_v = b_im.rearrange("g p h -> p g h")
            dma("sp", Bsame[0:64, :, :], bre_v, w=[Bsame])
            dma("sp", Bsame[64:128, :, :], bim_v, w=[Bsame])
            dma("sp", Bswap[0:64, :, :], bim_v, w=[Bswap])
            dma("sp", Bswap[64:128, :, :], bre_v, w=[Bswap])
            V(lambda e: e.tensor_tensor(out=Bsame[:], in0=Bsame[:], in1=QR[:].unsqueeze(2).to_broadcast([128, 128, 16]), op=ALU.mult),
              r=[Bsame, QR], w=[Bsame])
            V(lambda e: e.tensor_tensor(out=Bswap[:], in0=Bswap[:], in1=QI[:].unsqueeze(2).to_broadcast([128, 128, 16]), op=ALU.mult),
              r=[Bswap, QI], w=[Bswap])
            V(lambda e: e.tensor_tensor(out=Bsame[:], in0=Bsame[:], in1=Bswap[:], op=ALU.add), r=[Bsame, Bswap], w=[Bsame])
            for j in range(16):
                transp32(BT[:, j, :], BT, Bsame[:, 8 * j:8 * j + 8, :].rearrange("p g h -> p (g h)"), Bsame)
            prv4 = PRV.t[:, :, :].rearrange("p k (pr two) -> p k pr two", two=2)
            piv4 = PIV.t[:, :, :].rearrange("p k (pr two) -> p k pr two", two=2)
            V(lambda e: e.tensor_copy(out=PRP[0:64, :, :], in_=prv4[0:64, :, :, 0]), r=[PRV], w=[PRP])
            V(lambda e: e.tensor_copy(out=PRP[64:128, :, :], in_=prv4[64:128, :, :, 1]), r=[PRV], w=[PRP])
            V(lambda e: e.tensor_scalar(out=PIP[0:64, :, :], in0=piv4[0:64, :, :, 0], scalar1=-1.0, scalar2=None, op0=ALU.mult), r=[PIV], w=[PIP])
            V(lambda e: e.tensor_copy(out=PIP[64:128, :, :], in_=piv4[64:128, :, :, 1]), r=[PIV], w=[PIP])
            V(lambda e: e.tensor_scalar(out=NPIP[:, :, :], in0=PIP[:, :, :], scalar1=-1.0, scalar2=None, op0=ALU.mult), r=[PIP], w=[NPIP])
            Cn = kb.sb([128, 16, 128], F32)
            for (srcc, dstT, neg) in ((c_re, CT, False), (c_im, CTI, True)):
                dma("sp", Cn[:, :, 0:64], srcc.rearrange("(j q) h p -> (q h) j p", q=8), r=[CT, CTI], w=[Cn])
                dma("sp", Cn[:, :, 64:128], srcc.rearrange("(j q) h p -> (q h) j p", q=8), w=[Cn])
                for j in range(16):
                    transp32(dstT[:, j, :], dstT, Cn[:, j, :], Cn)
                if neg:
                    V(lambda e: e.tensor_scalar(out=dstT[:, :, :], in0=dstT[:, :, :], scalar1=-1.0, scalar2=None, op0=ALU.mult), r=[dstT], w=[dstT])
            sel0 = cst.t[:, CO_SEL:CO_SEL + 64]
            sel1 = cst.t[:, CO_SEL + 64:CO_SEL + 128]
            Nr = [kb.sb([128, 64], F32) for _ in range(2)]
            Ni = [kb.sb([128, 64], F32) for _ in range(2)]
            for i in range(NS):
                nr_, ni_ = Nr[i % 2], Ni[i % 2]
                dma("sp", nr_[:, :], s5re[i], w=[nr_])
                dma("sp", ni_[:, :], s5im[i], w=[ni_])
                b1 = nextbank()
                P(lambda e: e.matmul(b1[0:64, 0:64], lhsT=nr_[:, :], rhs=sel0, start=True, stop=True), r=[nr_, cst], w=[b1], inc=False)
                P(lambda e: e.matmul(b1[64:128, 0:64], lhsT=nr_[:, :], rhs=sel1, start=True, stop=True), r=[nr_, cst], w=[b1], acc=True)
                b2 = nextbank()
                P(lambda e: e.matmul(b2[0:64, 0:64], lhsT=ni_[:, :], rhs=sel0, start=True, stop=True), r=[ni_, cst], w=[b2], inc=False)
                P(lambda e: e.matmul(b2[64:128, 0:64], lhsT=ni_[:, :], rhs=sel1, start=True, stop=True), r=[ni_, cst], w=[b2], acc=True)
                V(lambda e: e.tensor_tensor(out=t1[:, 0:64], in0=b1[:, 0:64], in1=PRP[:, 0, :], op=ALU.mult), r=[b1, PRP], w=[t1])
                V(lambda e: e.tensor_tensor(out=t2[:, 0:64], in0=b2[:, 0:64], in1=NPIP[:, 0, :], op=ALU.mult), r=[b2, NPIP], w=[t2])
                V(lambda e: e.tensor_tensor(out=AH0[:, i, :], in0=t1[:, 0:64], in1=t2[:, 0:64], op=ALU.add), r=[t1, t2], w=[AH0])
                V(lambda e: e.tensor_tensor(out=t1[:, 0:64], in0=b2[:, 0:64], in1=PRP[:, 0, :], op=ALU.mult), r=[b2, PRP], w=[t1])
                V(lambda e: e.tensor_tensor(out=t2[:, 0:64], in0=b1[:, 0:64], in1=PIP[:, 0, :], op=ALU.mult), r=[b1, PIP], w=[t2])
                V(lambda e: e.tensor_tensor(out=AH0I[:, i, :], in0=t1[:, 0:64], in1=t2[:, 0:64], op=ALU.add), r=[t1, t2], w=[AH0I])

        def bcast_gain(vec, n):
            gt = kb.sb([128, n], F32)
            dma("sp", gt[:], vec.partition_broadcast(128), w=[gt])
            return gt

        def normA(X, XB, tiles, gvec, xnT):
            with kb.scope():
                gt = bcast_gain(gvec, D)
                as_ = [kb.sb([128, D], F32) for _ in range(2)]
                b = kb.sb([128, D], BF16)
                sts_ = [kb.sb([128, 4], F32) for _ in range(2)]
                for i, (t0, m) in enumerate(tiles):
                    a = as_[i % 2]
                    st = sts_[i % 2]
                    dma("sp", a[:m, :], X[t0:t0 + m, :], r=[XB] if XB is not None else [], w=[a])
                    V(lambda e: e.memset(st[:m, :], 0.0), w=[st])
                    A(lambda e: e.activation(out=b[:m, :], in_=a[:m, :], func=AF.Square, accum_out=st[:m, 0:1]), r=[a], w=[b, st])
                    V(lambda e: e.tensor_scalar(out=st[:m, 1:2], in0=st[:m, 0:1], scalar1=1.0 / D, scalar2=EPS, op0=ALU.mult, op1=ALU.add),
                      r=[st], w=[st])
                    A(lambda e: e.activation(out=st[:m, 2:3], in_=st[:m, 1:2], func=AF.Sqrt), r=[st], w=[st])
                    V(lambda e: e.reciprocal(out=st[:m, 3:4], in_=st[:m, 2:3]), r=[st], w=[st])
                    V(lambda e: e.scalar_tensor_tensor(out=b[:m, :], in0=a[:m, :], scalar=st[:m, 3:4], in1=gt[:m, :], op0=ALU.mult, op1=ALU.mult),
                      r=[a, st, gt], w=[b])
                    for q in range(8):
                        bk = nextbank()
                        for k4 in range(4):
                            kc = 4 * q + k4
                            P(lambda e: e.matmul(bk[:, k4 * 128:k4 * 128 + m], lhsT=b[:m, kc * 128:(kc + 1) * 128], rhs=identb[:m, :m],
                                                 start=True, stop=True), r=[b, identb], w=[bk], acc=k4 > 0, inc=(k4 == 3))
                        src = bk[:, :].rearrange("p (k c) -> p k c", c=128)[:, :, :m]
                        if q % 2 == 0:
                            A(lambda e: e.activation(out=xnT[:, 4 * q:4 * q + 4, t0:t0 + m], in_=src, func=AF.Copy), r=[bk], w=[xnT])
                        else:
                            V(lambda e: e.tensor_copy(out=xnT[:, 4 * q:4 * q + 4, t0:t0 + m], in_=src), r=[bk], w=[xnT])

        def loadW(wd, KC, b0, bw):
            W = wsl[wctr[0] % len(wsl)]
            wctr[0] += 1
            dma("pool", W[:, :KC, :bw], wd[:, b0:b0 + bw].rearrange("(kc p) c -> p kc c", p=128), w=[W])
            return W

        def projF(xnT, KC, pieces, wd, c0, ncols, cons):
            for b0 in range(c0, c0 + ncols, 256):
                bw = min(256, c0 + ncols - b0)
                W = loadW(wd, KC, b0, bw)
                for cc in range(0, bw, 128):
                    cw = min(128, bw - cc)
                    for (t0, tn) in pieces:
                        bk = nextbank()
                        for kc in range(KC):
                            P(lambda e: e.matmul(bk[:cw, :tn], lhsT=W[:, kc, cc:cc + cw], rhs=xnT[:, kc, t0:t0 + tn],
                                                 start=(kc == 0), stop=(kc == KC - 1)), r=[W, xnT], w=[bk], acc=kc > 0, inc=(kc == KC - 1))
                        cons((b0 + cc - c0) // 128, cw, t0, tn, bk)

        def projT(xnT, KC, tiles, wd, c0, ncols, cons):
            for b0 in range(c0, c0 + ncols, 256):
                W = loadW(wd, KC, b0, 256)
                for (t0, m) in tiles:
                    bk = nextbank()
                    for kc in range(KC):
                        P(lambda e: e.matmul(bk[:m, :256], lhsT=xnT[:, kc, t0:t0 + m], rhs=W[:, kc, :256],
                                             start=(kc == 0), stop=(kc == KC - 1)), r=[W, xnT], w=[bk], acc=kc > 0, inc=(kc == KC - 1))
                    cons(b0 - c0, 256, t0, m, bk)

        def proj_u(X, tiles, pieces, n, udst, Bu):
            with kb.scope():
                xnT = kb.sb([128, 32, n], BF16)
                normA(X, None, tiles, norm_mix_g, xnT)
                with wslots(2):
                    ust = [kb.sb([128, 512], F32) for _ in range(2)]
                    ctr = [0]

                    def cons_u(ci, cw, t0, tn, bk):
                        u_ = ust[ctr[0] % 2]
                        ctr[0] += 1
                        A(lambda e: e.activation(out=u_[:, :tn], in_=bk[:, :tn], func=AF.Copy), r=[bk], w=[u_])
                        dma("sp", udst[ci, :, t0:t0 + tn], u_[:, :tn], r=[u_], w=[Bu])
                    projF(xnT, 32, pieces, w_in, 0, 2048, cons_u)

        def s5_run(usrc, Bu, n, pieces, own, tabs):
            (PRV, PIV, BT, CT, AH0, HSN, HF, PRP, PIP, NPIP, CTI, AH0I, HSNI, HFI, HINI) = tabs
            with kb.scope():
                Hn = (1 + NT) if own else NPF
                nb_ = 2 if own else 1
                npar = 2 if own else 4
                HB = [[[kb.sb([128, Hn], F32) for _ in range(2)] for _ in range(nb_)] for _ in range(npar)]
                HD = [[[Buf() for _ in range(2)] for _ in range(nb_)] for _ in range(npar)]
                BTm = [[kb.sb([128, 128], F32) for _ in range(2)] for _ in range(npar)]
                CTm = [[kb.sb([128, 128], F32) for _ in range(2)] for _ in range(2)]
                uTj = [kb.sb([128, n], F32) for _ in range(2)]
                if own:
                    ysb = kb.sb([128, NT], F32)
                    tg = kb.sb([128, NT], F32)
                    zst = [kb.sb([128, NT], BF16) for _ in range(2)]
                    ybank = [banks[5], banks[6], banks[7]]
                    kb.rot = [0, 1, 2, 3, 4]
                off = 1 if own else 0
                Lt = 1 + LO
                for pp in range(64 // npar):
                    prs = tuple(npar * pp + i_ for i_ in range(npar))
                    j = (npar * pp) // 4
                    uT = uTj[j % 2]
                    conv_step(npar)
                    if (npar * pp) % 4 == 0:
                        dma("sp", uT[:, :], usrc[j, :, :], r=[Bu], w=[uT])
                    for par, pr in enumerate(prs):
                        prl = pr % 4
                        glA, glB = 2 * prl, 2 * prl + 1
                        for c in range(2):
                            bm = BTm[par][c]
                            G(lambda e: e.tensor_scalar(out=bm[:, 0:64], in0=BT[:, j, 64 * c:64 * c + 64], scalar1=cst.t[:, CO_MR + glA:CO_MR + glA + 1],
                                                        scalar2=None, op0=ALU.mult), r=[BT, cst], w=[bm])
                            G(lambda e: e.tensor_scalar(out=bm[:, 64:128], in0=BT[:, j, 64 * c:64 * c + 64], scalar1=cst.t[:, CO_MR + glB:CO_MR + glB + 1],
                                                        scalar2=None, op0=ALU.mult), r=[BT, cst], w=[bm])
                            if own:
                                cm = CTm[par][c]
                                csrc = CT if c == 0 else CTI
                                G(lambda e: e.tensor_tensor(out=cm[:], in0=csrc[:, j, :], in1=cst.t[:, CO_M2 + prl * 128:CO_M2 + (prl + 1) * 128],
                                                            op=ALU.mult), r=[csrc, cst], w=[cm])
                            H0 = HB[par][0][c]
                            for (t0, tn) in pieces:
                                bk = nextbank()
                                P(lambda e: e.matmul(bk[:, :tn], lhsT=bm[:], rhs=uT[:, t0:t0 + tn], start=True, stop=True), r=[bm, uT], w=[bk])
                                A(lambda e: e.activation(out=H0[:, off + t0:off + t0 + tn], in_=bk[:, :tn], func=AF.Copy), r=[bk, HD[par][0][c]], w=[H0])
                            if own:
                                hin = HIN if c == 0 else HINI
                                A(lambda e: e.activation(out=H0[:, 0:1], in_=hin[:, pr:pr + 1], func=AF.Copy), r=[hin], w=[H0])
                    if own:
                        ci = 0
                        for k in range(11):
                            d = 1 << k
                            for par, pr in enumerate(prs):
                                sR, sI = HB[par][ci]
                                dR, dI = HB[par][1 - ci]
                                sRh, sIh = HD[par][ci]
                                dRh, dIh = HD[par][1 - ci]
                                A(lambda e: e.activation(out=dR[:, 0:d], in_=sR[:, 0:d], func=AF.Copy), r=[sR, sRh], w=[dRh])
                                A(lambda e: e.activation(out=dI[:, 0:d], in_=sI[:, 0:d], func=AF.Copy), r=[sI, sIh], w=[dIh])
                            for stage in range(2):
                                for par, pr in enumerate(prs):
                                    sR, sI = HB[par][ci]
                                    dR, dI = HB[par][1 - ci]
                                    sRh, sIh = HD[par][ci]
                                    if stage == 0:
                                        V(lambda e: e.scalar_tensor_tensor(out=dR[:, d:Lt], in0=sR[:, 0:Lt - d], scalar=PRP[:, k, pr:pr + 1], in1=sR[:, d:Lt],
                                                                           op0=ALU.mult, op1=ALU.add), r=[sR, sRh, PRP], w=[dR])
                                        V(lambda e: e.scalar_tensor_tensor(out=dI[:, d:Lt], in0=sI[:, 0:Lt - d], scalar=PRP[:, k, pr:pr + 1], in1=sI[:, d:Lt],
                                                                           op0=ALU.mult, op1=ALU.add), r=[sI, sIh, PRP], w=[dI])
                                    else:
                                        V(lambda e: e.scalar_tensor_tensor(out=dR[:, d:Lt], in0=sI[:, 0:Lt - d], scalar=NPIP[:, k, pr:pr + 1], in1=dR[:, d:Lt],
                                                                           op0=ALU.mult, op1=ALU.add), r=[sI, sIh, NPIP, dR], w=[dR])
                                        V(lambda e: e.scalar_tensor_tensor(out=dI[:, d:Lt], in0=sR[:, 0:Lt - d], scalar=PIP[:, k, pr:pr + 1], in1=dI[:, d:Lt],
                                                                           op0=ALU.mult, op1=ALU.add), r=[sR, sRh, PIP, dI], w=[dI])
                            ci = 1 - ci
                        for par, pr in enumerate(prs):
                            prl = pr % 4
                            for c in range(2):
                                H = HB[par][ci][c]
                                Hh = HD[par][ci][c]
                                H0 = HB[par][0][c]
                                ah = AH0 if c == 0 else AH0I
                                hsn = HSN if c == 0 else HSNI
                                hf = HF if c == 0 else HFI
                                V(lambda e: e.tensor_tensor(out=H[:, 1 + LO:1 + NT], in0=H0[:, 1 + LO:1 + NT], in1=ah[:, :, pr], op=ALU.add),
                                  r=[H0, ah], w=[H])
                                A(lambda e: e.activation(out=hsn[:, :, pr], in_=H[:, 1 + LO:1 + NT], func=AF.Copy), r=[H], w=[hsn])
                                A(lambda e: e.activation(out=hf[:, pr:pr + 1], in_=H[:, LO:LO + 1], func=AF.Copy), r=[H], w=[hf])
                                cm = CTm[par][c]
                                for pi, (t0, tn) in enumerate(pieces):
                                    P(lambda e: e.matmul(ybank[pi][:, :tn], lhsT=cm[:], rhs=H[:, 1 + t0:1 + t0 + tn],
                                                         start=(prl == 0 and c == 0), stop=(prl == 3 and c == 1)), r=[cm, H, Hh], w=[ybank[pi]],
                                      acc=not (prl == 0 and c == 0))
                        if pp % 2 == 1:
                            z_ = zst[j % 2]
                            for pi, (t0, tn) in enumerate(pieces):
                                V(lambda e: e.scalar_tensor_tensor(out=ysb[:, t0:t0 + tn], in0=uT[:, t0:t0 + tn], scalar=cols[:, j:j + 1],
                                                                   in1=ybank[pi][:, :tn], op0=ALU.mult, op1=ALU.add),
                                  r=[uT, cols, ybank[pi]], w=[ysb])
                            A(lambda e: e.activation(out=tg[:], in_=ysb[:], func=AF.Square), r=[ysb], w=[tg])
                            V(lambda e: e.tensor_scalar(out=tg[:], in0=tg[:], scalar1=0.044715, scalar2=1.0, op0=ALU.mult, op1=ALU.add), r=[tg], w=[tg])
                            V(lambda e: e.tensor_tensor(out=tg[:], in0=tg[:], in1=ysb[:], op=ALU.mult), r=[tg, ysb], w=[tg])
                            A(lambda e: e.activation(out=tg[:], in_=tg[:], func=AF.Sigmoid, scale=1.5957691216057308), r=[tg], w=[tg])
                            V(lambda e: e.tensor_tensor(out=z_[:, :], in0=ysb[:], in1=tg[:], op=ALU.mult), r=[tg, ysb], w=[z_])
                            dma("sp", z_scr[:, j, :], z_[:, :], r=[z_], w=[B_z])
                    else:
                        s0 = 0
                        for k in range(9, -1, -1):
                            half = 1 << k
                            lo = slice(s0, s0 + half)
                            hi = slice(s0 + half, s0 + 2 * half)
                            for stage in range(2):
                                for par, pr in enumerate(prs):
                                    R, I = HB[par][0]
                                    if stage == 0:
                                        V(lambda e: e.scalar_tensor_tensor(out=R[:, hi], in0=R[:, lo], scalar=PRP[:, k, pr:pr + 1], in1=R[:, hi],
                                                                           op0=ALU.mult, op1=ALU.add), r=[R, PRP], w=[R])
                                        V(lambda e: e.scalar_tensor_tensor(out=I[:, hi], in0=I[:, lo], scalar=PRP[:, k, pr:pr + 1], in1=I[:, hi],
                                                                           op0=ALU.mult, op1=ALU.add), r=[I, PRP], w=[I])
                                    else:
                                        V(lambda e: e.scalar_tensor_tensor(out=R[:, hi], in0=I[:, lo], scalar=NPIP[:, k, pr:pr + 1], in1=R[:, hi],
                                                                           op0=ALU.mult, op1=ALU.add), r=[I, NPIP, R], w=[R])
                                        V(lambda e: e.scalar_tensor_tensor(out=I[:, hi], in0=R[:, lo], scalar=PIP[:, k, pr:pr + 1], in1=I[:, hi],
                                                                           op0=ALU.mult, op1=ALU.add), r=[R, PIP, I], w=[I])
                            s0 += half
                        for par, pr in enumerate(prs):
                            R, I = HB[par][0]
                            A(lambda e: e.activation(out=HIN[:, pr:pr + 1], in_=R[:, NPF - 1:NPF], func=AF.Copy), r=[R], w=[HIN])
                            A(lambda e: e.activation(out=HINI[:, pr:pr + 1], in_=I[:, NPF - 1:NPF], func=AF.Copy), r=[I], w=[HINI])
                kb.rot = list(range(8))

        def s5_glu_norm():
            with kb.scope():
                zT = kb.sb([128, 16, NT], BF16)
                zg = kb.sb([128, 16, NT], F32)
                dma("sp", zT[:, :, :], z_scr[:, :, :], r=[B_z], w=[zT])
                sg = [kb.sb([128, 352], F32) for _ in range(2)]
                sq = [kb.sb([128, 352], F32) for _ in range(2)]
                rstd = kb.sb([128, NT], F32)
                ctr = [0]

                def cons(ci, cw, t0, tn, bk):
                    s_ = sg[ctr[0] % 2]
                    ctr[0] += 1
                    A(lambda e: e.activation(out=s_[:, :tn], in_=bk[:, :tn], func=AF.Sigmoid, bias=cols[:, 16 + ci:17 + ci], scale=1.0),
                      r=[bk, cols], w=[s_])
                    V(lambda e: e.tensor_tensor(out=zg[:, ci, t0:t0 + tn], in0=zT[:, ci, t0:t0 + tn], in1=s_[:, :tn], op=ALU.mult),
                      r=[zT, s_], w=[zg])
                with wslots(2):
                    projF(zT, 16, OWN_PIECES, w_glu, 0, 2048, cons)
                nb = [banks[5], banks[6], banks[7]]
                kb.rot = [0, 1, 2, 3, 4]
                for ci in range(16):
                    for pi, (t0, tn) in enumerate(OWN_PIECES):
                        s_ = sq[(ci * 3 + pi) % 2]
                        A(lambda e: e.activation(out=s_[:, :tn], in_=zg[:, ci, t0:t0 + tn], func=AF.Square), r=[zg], w=[s_])
                        P(lambda e: e.matmul(nb[pi][:, :tn], lhsT=ones, rhs=s_[:, :tn], start=(ci == 0), stop=(ci == 15)),
                          r=[s_, cst], w=[nb[pi]], acc=ci > 0)
                for pi, (t0, tn) in enumerate(OWN_PIECES):
                    V(lambda e: e.tensor_scalar(out=rstd[:, t0:t0 + tn], in0=nb[pi][:, :tn], scalar1=1.0 / 2048, scalar2=EPS, op0=ALU.mult, op1=ALU.add),
                      r=[nb[pi]], w=[rstd])
                A(lambda e: e.activation(out=rstd[:], in_=rstd[:], func=AF.Sqrt), r=[rstd], w=[rstd])
                V(lambda e: e.reciprocal(out=rstd[:], in_=rstd[:]), r=[rstd], w=[rstd])
                for ci in range(16):
                    V(lambda e: e.scalar_tensor_tensor(out=zT[:, ci, :], in0=zg[:, ci, :], scalar=cols[:, 32 + ci:33 + ci], in1=rstd[:],
                                                       op0=ALU.mult, op1=ALU.mult), r=[zg, cols, rstd], w=[zT])
                dma("sp", z_scr[:, :, :], zT[:, :, :], r=[zT], w=[B_z])
                kb.rot = list(range(8))

        def gla_proj(X, n, pieces, tiles, chunks, own, kT, qT, EB, ebs, vdst, Bv):
            with kb.scope():
                xnT = kb.sb([128, 32, n], BF16)
                normA(X, None, tiles, norm_mix_g, xnT)
                with wslots(2):
                    xgT = kb.sb([16, n], F32)
                    wg2 = kb.sb([16, 1024], F32)
                    e1 = [kb.sb([128, 512], F32) for _ in range(2)]
                    tmp = kb.sb([128, 128], F32)
                    btmp = kb.sb([128, 2, n], F32)
                    vst = [kb.sb([128, 256], BF16) for _ in range(2)]
                    ctr = [0]
                    dma("sp", wg2[:], w_gate2[:, :], w=[wg2])

                    def cons_g(ci, cw, t0, tn, bk):
                        A(lambda e: e.activation(out=xgT[:16, t0:t0 + tn], in_=bk[:16, :tn], func=AF.Copy), r=[bk], w=[xgT])
                    projF(xnT, 32, pieces, w_in, 8192, 16, cons_g)
                    for cp in range(4):
                        for cl in range(2):
                            c8 = 2 * cp + cl
                            for (t0, tn) in pieces:
                                bk = nextbank()
                                P(lambda e: e.matmul(bk[:, :tn], lhsT=wg2[:16, c8 * 128:(c8 + 1) * 128], rhs=xgT[:16, t0:t0 + tn], start=True, stop=True),
                                  r=[wg2, xgT], w=[bk])
                                e_ = e1[ctr[0] % 2]
                                ctr[0] += 1
                                A(lambda e: e.activation(out=e_[:, :tn], in_=bk[:, :tn], func=AF.Exp, scale=-1.0, bias=cols[:, 48 + c8:49 + c8]),
                                  r=[bk, cols], w=[e_])
                                A(lambda e: e.activation(out=btmp[:, cl, t0:t0 + tn], in_=e_[:, :tn], func=AF.Ln, bias=1.0, scale=1.0), r=[e_], w=[btmp])
                            for ci_, (a0, C) in enumerate(chunks):
                                V(lambda e: e.tensor_tensor_scan(out=tmp[:, :C], data0=ones[:, :C], data1=btmp[:, cl, a0:a0 + C], initial=0.0,
                                                                 op0=ALU.mult, op1=ALU.add), r=[btmp, cst], w=[tmp])
                                V(lambda e: e.tensor_scalar(out=btmp[:, cl, a0:a0 + C], in0=tmp[:, :C], scalar1=-1.0 / 16, scalar2=None, op0=ALU.mult),
                                  r=[tmp], w=[btmp])
                                A(lambda e: e.activation(out=EB[:, c8, ci_:ci_ + 1], in_=btmp[:, cl, a0 + C - 1:a0 + C], func=AF.Exp), r=[btmp], w=[EB])
                            if own:
                                V(lambda e: e.tensor_scalar(out=btmp[:, cl, LO:NT], in0=btmp[:, cl, LO:NT], scalar1=-1.0 / 16, scalar2=None, op0=ALU.mult),
                                  r=[btmp], w=[btmp])
                                A(lambda e: e.activation(out=ebs[:, c8, :], in_=btmp[:, cl, LO:NT], func=AF.Exp), r=[btmp], w=[ebs])

                        def cons_k(ci, cw, t0, tn, bk):
                            e_ = e1[ctr[0] % 2]
                            ctr[0] += 1
                            A(lambda e: e.activation(out=e_[:, :tn], in_=btmp[:, ci, t0:t0 + tn], func=AF.Exp, scale=-1.0), r=[btmp], w=[e_])
                            V(lambda e: e.tensor_tensor(out=kT[:, 2 * cp + ci, t0:t0 + tn], in0=bk[:, :tn], in1=e_[:, :tn], op=ALU.mult), r=[bk, e_], w=[kT])
                        projF(xnT, 32, pieces, w_in, 3072 + 256 * cp, 256, cons_k)

                        def cons_q(ci, cw, t0, tn, bk):
                            e_ = e1[ctr[0] % 2]
                            ctr[0] += 1
                            A(lambda e: e.activation(out=e_[:, :tn], in_=btmp[:, ci, t0:t0 + tn], func=AF.Exp, scale=1.0), r=[btmp], w=[e_])
                            V(lambda e: e.scalar_tensor_tensor(out=qT[:, 2 * cp + ci, t0:t0 + tn], in0=bk[:, :tn], scalar=1.0 / 16, in1=e_[:, :tn],
                                                               op0=ALU.mult, op1=ALU.mult), r=[bk, e_], w=[qT])
                        if own:
                            projF(xnT, 32, pieces, w_in, 2048 + 256 * cp, 256, cons_q)

                    def cons_v(cb, cw, t0, m, bk):
                        v_ = vst[ctr[0] % 2]
                        ctr[0] += 1
                        A(lambda e: e.activation(out=v_[:m, :cw], in_=bk[:m, :cw], func=AF.Copy), r=[bk], w=[v_])
                        dma("sp", vdst[t0:t0 + m, cb:cb + cw], v_[:m, :cw], r=[v_], w=[Bv])
                    projT(xnT, 32, tiles, w_in, 4096, 2048, cons_v)

                    def cons_r(cb, cw, t0, m, bk):
                        v_ = vst[ctr[0] % 2]
                        ctr[0] += 1
                        A(lambda e: e.activation(out=v_[:m, :cw], in_=bk[:m, :cw], func=AF.Silu), r=[bk], w=[v_])
                        dma("sp", sr_scr[t0:t0 + m, cb:cb + cw], v_[:m, :cw], r=[v_], w=[B_sr])
                    if own:
                        projT(xnT, 32, tiles, w_in, 6144, 2048, cons_r)

        ep = {}

        def gla_epilogue(C, pso, h, srt, gng, ofin, st):
            jk = ep["jk"]
            tmpo = ep["tmpo"]
            V(lambda e: e.memset(st[:C, :], 0.0), w=[st])
            A(lambda e: e.activation(out=jk[:C, :], in_=pso[:C, :], func=AF.Square, accum_out=st[:C, 0:1]), r=[pso], w=[jk, st])
            V(lambda e: e.tensor_scalar(out=st[:C, 1:2], in0=st[:C, 0:1], scalar1=1.0 / 512, scalar2=EPS, op0=ALU.mult, op1=ALU.add), r=[st], w=[st])
            A(lambda e: e.activation(out=st[:C, 2:3], in_=st[:C, 1:2], func=AF.Sqrt), r=[st], w=[st])
            V(lambda e: e.reciprocal(out=st[:C, 3:4], in_=st[:C, 2:3]), r=[st], w=[st])
            V(lambda e: e.scalar_tensor_tensor(out=tmpo[:C, :], in0=pso[:C, :], scalar=st[:C, 3:4], in1=gng[:C, h * 512:(h + 1) * 512],
                                               op0=ALU.mult, op1=ALU.mult), r=[pso, st, gng], w=[tmpo])
            V(lambda e: e.tensor_tensor(out=ofin[:C, h * 512:(h + 1) * 512], in0=tmpo[:C, :], in1=srt[:C, h * 512:(h + 1) * 512], op=ALU.mult),
              r=[tmpo, srt], w=[ofin])

        def ofin_to_oT(ofin, C, t0, oT):
            for q in range(4):
                bk = nextbank()
                for k4 in range(4):
                    kc = 4 * q + k4
                    P(lambda e: e.matmul(bk[:, k4 * 128:k4 * 128 + C], lhsT=ofin[:C, kc * 128:(kc + 1) * 128], rhs=identb[:C, :C],
                                         start=True, stop=True), r=[ofin, identb], w=[bk], acc=k4 > 0, inc=(k4 == 3))
                src = bk[:, :].rearrange("p (k c) -> p k c", c=128)[:, :, :C]
                A(lambda e: e.activation(out=oT[:, 4 * q:4 * q + 4, t0:t0 + C], in_=src, func=AF.Copy), r=[bk], w=[oT])

        def gla_run(chunks, own, kT, qT, EB, vsrc, Bv, oT, gng):
            S = Sh[0]
            with kb.scope():
                Sb = kb.sb([128, 8, 512], BF16)
                V(lambda e: e.tensor_copy(out=Sb[:], in_=S[:]), r=[S], w=[Sb])
                vt = [kb.sb([128, 2048], BF16) for _ in range(2)]
                srt = [kb.sb([128, 2048], BF16) for _ in range(2)]
                ktok = kb.sb([128, 8, 128], BF16)
                scm = kb.sb([128, 128], BF16)
                ofin = kb.sb([128, 2048], BF16)
                tmpS = kb.sb([128, 512], F32)
                st = kb.sb([128, 4], F32)
                ep["jk"] = kb.sb([128, 512], BF16)
                ep["tmpo"] = kb.sb([128, 512], F32)
                for ci_, (t0, C) in enumerate(chunks):
                    v_ = vt[ci_ % 2]
                    s_ = srt[ci_ % 2]
                    dma("sp", v_[:C, :], vsrc[t0:t0 + C, :], r=[Bv], w=[v_])
                    if own:
                        dma("sp", s_[:C, :], sr_scr[t0:t0 + C, :], r=[B_sr], w=[s_])
                    for half in range(2):
                        bk = nextbank()
                        for k4 in range(4):
                            c8 = half * 4 + k4
                            P(lambda e: e.matmul(bk[:C, k4 * 128:(k4 + 1) * 128], lhsT=kT[:, c8, t0:t0 + C], rhs=identb[:, :], start=True, stop=True),
                              r=[kT, identb], w=[bk], acc=k4 > 0, inc=(k4 == 3))
                        A(lambda e: e.activation(out=ktok[:C, half * 4:half * 4 + 4, :], in_=bk[:C, :].rearrange("p (k c) -> p k c", c=128),
                                                 func=AF.Copy), r=[bk], w=[ktok])
                    for h in range(4):
                        if own:
                            pss = nextbank()
                            for kc in range(2):
                                P(lambda e: e.matmul(pss[:C, :C], lhsT=kT[:, 2 * h + kc, t0:t0 + C], rhs=qT[:, 2 * h + kc, t0:t0 + C],
                                                     start=(kc == 0), stop=(kc == 1)), r=[kT, qT], w=[pss], acc=kc > 0, inc=(kc == 1))
                            V(lambda e: e.tensor_tensor(out=scm[:C, :C], in0=pss[:C, :C], in1=cst.t[:C, CO_MT:CO_MT + C], op=ALU.mult),
                              r=[pss, cst], w=[scm])
                            pso = nextbank()
                            for kc in range(2):
                                P(lambda e: e.matmul(pso[:C, :], lhsT=qT[:, 2 * h + kc, t0:t0 + C], rhs=Sb[:, 2 * h + kc, :],
                                                     start=(kc == 0), stop=False), r=[qT, Sb], w=[pso], acc=kc > 0, inc=False)
                            P(lambda e: e.matmul(pso[:C, :], lhsT=scm[:C, :C], rhs=v_[:C, h * 512:(h + 1) * 512], start=False, stop=True),
                              r=[scm, v_], w=[pso], acc=True)
                            gla_epilogue(C, pso, h, s_, gng, ofin, st)
                        for kc in range(2):
                            c8 = 2 * h + kc
                            psk = nextbank()
                            P(lambda e: e.matmul(psk[:, :], lhsT=ktok[:C, c8, :], rhs=v_[:C, h * 512:(h + 1) * 512], start=True, stop=True),
                              r=[ktok, v_], w=[psk])
                            V(lambda e: e.tensor_tensor(out=tmpS[:, :], in0=psk[:, :], in1=S[:, c8, :], op=ALU.add), r=[psk, S], w=[tmpS])
                            V(lambda e: e.tensor_scalar(out=S[:, c8, :], in0=tmpS[:, :], scalar1=EB[:, c8, ci_:ci_ + 1], scalar2=None, op0=ALU.mult),
                              r=[tmpS, EB], w=[S])
                            A(lambda e: e.activation(out=Sb[:, c8, :], in_=tmpS[:, :], func=AF.Copy, scale=EB[:, c8, ci_:ci_ + 1]), r=[tmpS, EB], w=[Sb])
                    if own:
                        ofin_to_oT(ofin, C, t0, oT)

        def gla_samples(kT, qT, ebs, oT, gng):
            with kb.scope():
                v16 = kb.sb([16, 2048], BF16)
                sr16 = kb.sb([16, 2048], BF16)
                vm = [kb.sb([16, 2048], BF16) for _ in range(2)]
                ktok = kb.sb([16, 8, 128], BF16)
                qm = [kb.sb([128, 16], BF16) for _ in range(2)]
                S0 = [kb.sb([128, 512], F32) for _ in range(3)]
                tmpS = [kb.sb([128, 512], F32) for _ in range(2)]
                tmpb = [kb.sb([128, 512], BF16) for _ in range(2)]
                Sn = [kb.sb([128, 512], F32) for _ in range(3)]
                ofin = kb.sb([16, 2048], BF16)
                st = kb.sb([128, 4], F32)
                ep["jk"] = kb.sb([128, 512], BF16)
                ep["tmpo"] = kb.sb([128, 512], F32)
                pos = [banks[4], banks[5], banks[6], banks[7]]
                kb.rot = [0, 1, 2, 3]
                dma("sp", v16[:, :], v_scr[LO:NT, :], r=[B_v], w=[v16])
                dma("sp", sr16[:, :], sr_scr[LO:NT, :], r=[B_sr], w=[sr16])
                for half in range(2):
                    bk = nextbank()
                    for k4 in range(4):
                        c8 = half * 4 + k4
                        P(lambda e: e.matmul(bk[:NS, k4 * 128:(k4 + 1) * 128], lhsT=kT[:, c8, LO:NT], rhs=identb[:, :], start=True, stop=True),
                          r=[kT, identb], w=[bk], acc=k4 > 0, inc=(k4 == 3))
                    A(lambda e: e.activation(out=ktok[:, half * 4:half * 4 + 4, :], in_=bk[:NS, :].rearrange("p (k c) -> p k c", c=128),
                                             func=AF.Copy), r=[bk], w=[ktok])
                it = 0
                for i in range(NS):
                    vm_ = vm[i % 2]
                    V(lambda e: e.tensor_scalar(out=vm_[:, :], in0=v16[:, :], scalar1=cst.t[:16, CO_ID + i:CO_ID + i + 1], scalar2=None, op0=ALU.mult),
                      r=[v16, cst], w=[vm_])
                    for h in range(4):
                        for kc in range(2):
                            c8 = 2 * h + kc
                            s0_ = S0[it % 3]
                            sn_ = Sn[it % 3]
                            ts_ = tmpS[it % 2]
                            tb_ = tmpb[it % 2]
                            qm_ = qm[it % 2]
                            it += 1
                            dma("sp", s0_[:, :], sgla[i, h, kc * 128:(kc + 1) * 128, :], w=[s0_])
                            psk = nextbank()
                            P(lambda e: e.matmul(psk[:, :], lhsT=ktok[:, c8, :], rhs=vm_[:, h * 512:(h + 1) * 512], start=True, stop=True),
                              r=[ktok, vm_], w=[psk])
                            V(lambda e: e.tensor_tensor(out=ts_[:, :], in0=psk[:, :], in1=s0_[:, :], op=ALU.add), r=[psk, s0_], w=[ts_])
                            V(lambda e: e.tensor_scalar(out=sn_[:, :], in0=ts_[:, :], scalar1=ebs[:, c8, i:i + 1], scalar2=None, op0=ALU.mult),
                              r=[ts_, ebs], w=[sn_])
                            dma("sp", o_glas[i, c8, :, :], sn_[:, :], r=[sn_])
                            A(lambda e: e.activation(out=tb_[:, :], in_=ts_[:, :], func=AF.Copy), r=[ts_], w=[tb_])
                            V(lambda e: e.tensor_scalar(out=qm_[:, :], in0=iota256[:, 0:16], scalar1=float(i), scalar2=None, op0=ALU.is_equal),
                              r=[cst], w=[qm_])
                            V(lambda e: e.tensor_tensor(out=qm_[:, :], in0=qm_[:, :], in1=qT[:, c8, LO:NT], op=ALU.mult), r=[qm_, qT], w=[qm_])
                            first = (i == 0 and kc == 0)
                            last = (i == NS - 1 and kc == 1)
                            P(lambda e: e.matmul(pos[h][:NS, :], lhsT=qm_[:, :], rhs=tb_[:, :], start=first, stop=last),
                              r=[qm_, tb_], w=[pos[h]], acc=not first)
                for h in range(4):
                    gla_epilogue(NS, pos[h], h, sr16, gng, ofin, st)
                kb.rot = list(range(8))
                ofin_to_oT(ofin, NS, LO, oT)

        PHASES = _PH
        with kb.scope():
            PRV = PIV = None
            BT = kb.sb([128, 16, 128], F32, "BT")
            CT = kb.sb([128, 16, 128], F32, "CT")
            AH0 = kb.sb([128, NS, 64], F32, "AH0")
            HSN = kb.sb([128, NS, 64], F32, "HSN")
            HF = kb.sb([128, 128], F32, "HF")
            PRP = kb.sb([128, 11, 64], F32, "PRP")
            PIP = kb.sb([128, 11, 64], F32, "PIP")
            NPIP = kb.sb([128, 11, 64], F32, "NPIP")
            CTI = kb.sb([128, 16, 128], F32, "CTI")
            AH0I = kb.sb([128, NS, 64], F32, "AH0I")
            HSNI = kb.sb([128, NS, 64], F32, "HSNI")
            HFI = kb.sb([128, 128], F32, "HFI")
            tabs = (PRV, PIV, BT, CT, AH0, HSN, HF, PRP, PIP, NPIP, CTI, AH0I, HSNI, HFI, HINI)
            s5_setup(PRV, PIV, BT, CT, AH0, PRP, PIP, NPIP, CTI, AH0I)
            proj_u(xp, PRE_TILES, PRE_PIECES, NPF, up_scr, B_up)
            s5_run(up_scr, B_up, NPF, PRE_PIECES, False, tabs)
            proj_u(xo, OWN_TILES, OWN_PIECES, NT, u_scr, B_u)
            s5_run(u_scr, B_u, NT, OWN_PIECES, True, tabs)
            with kb.scope():
                so = [kb.sb([64, 128], F32) for _ in range(2)]
                oc = [0]

                def out_state(src_ap, srcT, dst_ap):
                    s_ = so[oc[0] % 2]
                    oc[0] += 1
                    bk = nextbank()
                    P(lambda e: e.matmul(bk[0:64, 0:128], lhsT=src_ap, rhs=ident, start=True, stop=True), r=[srcT, cst], w=[bk])
                    A(lambda e: e.activation(out=s_[:, :], in_=bk[0:64, 0:128], func=AF.Copy), r=[bk], w=[s_])
                    dma("sp", dst_ap, s_[:, :], r=[s_])
                out_state(HF[:, 0:64], HF, o_s5[0, :, :])
                out_state(HFI[:, 0:64], HFI, o_s5[1, :, :])
                for i in range(NS):
                    out_state(HSN[:, i, :], HSN, o_s5s[i, 0, :, :])
                    out_state(HSNI[:, i, :], HSNI, o_s5s[i, 1, :, :])
        s5_glu_norm()

        if PHASES >= 2:
          with kb.scope():
            Sh[0] = kb.sb([128, 8, 512], F32, "S")
            G(lambda e: e.memset(Sh[0][:], 0.0), w=[Sh[0]])
            with kb.scope():
                kT = kb.sb([128, 8, NPF], BF16, "kTp")
                EB = kb.sb([128, 8, 16], F32, "EBp")
                gla_proj(xp, NPF, PRE_PIECES, PRE_TILES, PRE_CHUNKS, False, kT, None, EB, None, vp_scr, B_vp)
                gla_run(PRE_CHUNKS, False, kT, None, EB, vp_scr, B_vp, None, None)
            with kb.scope():
                kT = kb.sb([128, 8, NT], BF16, "kTo")
                qT = kb.sb([128, 8, NT], BF16, "qTo")
                EB = kb.sb([128, 8, 16], F32, "EBo")
                ebs = kb.sb([128, 8, NS], F32, "ebs")
                gla_proj(xo, NT, OWN_PIECES, OWN_TILES, OWN_CHUNKS, True, kT, qT, EB, ebs, v_scr, B_v)
                oT = kb.sb([128, 16, NT], BF16, "oT")
                with kb.scope():
                    gng = bcast_gain(gla_norm_g, 2048)
                    gla_run(OWN_CHUNKS, True, kT, qT, EB, v_scr, B_v, oT, gng)
                    for c8 in range(8):
                        dma("sp", o_gla[c8, :, :], Sh[0][:, c8, :], r=[Sh[0]])
                    gla_samples(kT, qT, ebs, oT, gng)
                with wslots(2):
                    zT2 = kb.sb([128, 16, NT], BF16, "zT2")
                    dma("sp", zT2[:, :, :], z_scr[:, :, :], r=[B_z], w=[zT2])
                    xr = [kb.sb([128, 256], F32) for _ in range(3)]
                    ho = [kb.sb([128, 256], F32) for _ in range(3)]
                    it = 0
                    for b0 in range(0, D, 256):
                        W = loadW(w_out, 32, b0, 256)
                        for (t0, m) in OWN_TILES:
                            x_ = xr[it % 3]
                            h_ = ho[it % 3]
                            it += 1
                            dma("sp", x_[:m, :], xo[t0:t0 + m, b0:b0 + 256], w=[x_])
                            bk = nextbank()
                            for kc in range(32):
                                src = zT2 if kc < 16 else oT
                                P(lambda e: e.matmul(bk[:m, :256], lhsT=src[:, kc % 16, t0:t0 + m], rhs=W[:, kc, :256], start=(kc == 0), stop=(kc == 31)),
                                  r=[W, src], w=[bk], acc=kc > 0, inc=(kc == 31))
                            V(lambda e: e.tensor_tensor(out=h_[:m, :], in0=bk[:m, :256], in1=x_[:m, :], op=ALU.add), r=[bk, x_], w=[h_])
                            dma("sp", h_scr[t0:t0 + m, b0:b0 + 256], h_[:m, :], r=[h_], w=[B_h])

        if PHASES >= 3:
          conv_step(1000)
          with kb.scope():
            TV = kb.sb([128, 9, 16, 16], F32, "TV")
            TI = kb.sb([128, 9, 16, 16], F32, "TI")
            with kb.scope():
                xnT = kb.sb([128, 32, NT], BF16, "xnT2")
                normA(h_scr, B_h, OWN_TILES, norm_ffn_g, xnT)
                keysT = kb.sb([128, 16, 128], F32, "keysT")
                kn = [kb.sb([128, 128], F32) for _ in range(2)]
                for c16 in range(16):
                    k_ = kn[c16 % 2]
                    dma("sp", k_[:, :], keys[c16, :, :], w=[k_])
                    transp32(keysT[:, c16, :], keysT, k_[:], k_)
                qTt = kb.sb([128, NT], F32, "qTt")
                sc = [kb.sb([128, 128], F32) for _ in range(2)]
                sc2 = [kb.sb([128, 128], F32) for _ in range(2)]
                m8 = [kb.sb([128, 8], F32) for _ in range(2)]
                i8 = [kb.sb([128, 8], U32) for _ in range(2)]
                ctr = [0]

                def cons_qp(ci, cw, t0, tn, bk):
                    A(lambda e: e.activation(out=qTt[:, t0:t0 + tn], in_=bk[:, :tn], func=AF.Copy), r=[bk], w=[qTt])
                with wslots(2):
                  for c16 in range(16):
                    projF(xnT, 32, OWN_PIECES, w_q, c16 * 128, 128, cons_qp)
                    for ti, (t0, m) in enumerate(OWN_TILES):
                        bk = nextbank()
                        P(lambda e: e.matmul(bk[:m, :128], lhsT=qTt[:, t0:t0 + m], rhs=keysT[:, c16, :], start=True, stop=True),
                          r=[qTt, keysT], w=[bk])
                        s_ = sc[ctr[0] % 2]
                        s2 = sc2[ctr[0] % 2]
                        ma = m8[ctr[0] % 2]
                        ia = i8[ctr[0] % 2]
                        ctr[0] += 1
                        A(lambda e: e.activation(out=s_[:m, :], in_=bk[:m, :128], func=AF.Copy), r=[bk], w=[s_])
                        V(lambda e: e.max(out=ma[:m, :], in_=s_[:m, :]), r=[s_], w=[ma])
                        V(lambda e: e.max_index(out=ia[:m, :], in_max=ma[:m, :], in_values=s_[:m, :]), r=[ma, s_], w=[ia])
                        V(lambda e: e.tensor_copy(out=TV[:m, ti, c16, 0:8], in_=ma[:m, :]), r=[ma], w=[TV])
                        V(lambda e: e.tensor_copy(out=TI[:m, ti, c16, 0:8], in_=ia[:m, :]), r=[ia], w=[TI])
                        V(lambda e: e.match_replace(out=s2[:m, :], in_to_replace=ma[:m, :], in_values=s_[:m, :], imm_value=-1e30), r=[ma, s_], w=[s2])
                        V(lambda e: e.max(out=ma[:m, :], in_=s2[:m, :]), r=[s2], w=[ma])
                        V(lambda e: e.max_index(out=ia[:m, :], in_max=ma[:m, :], in_values=s2[:m, :]), r=[ma, s2], w=[ia])
                        V(lambda e: e.tensor_copy(out=TV[:m, ti, c16, 8:16], in_=ma[:m, :]), r=[ma], w=[TV])
                        V(lambda e: e.tensor_copy(out=TI[:m, ti, c16, 8:16], in_=ia[:m, :]), r=[ia], w=[TI])
            with kb.scope():
                gt_ffn = bcast_gain(norm_ffn_g, D)
                gt_fin = bcast_gain(norm_final_g, D)
                hxs = [kb.sb([128, D], F32, "hx%d" % i) for i in range(2)]
                xnbs = [kb.sb([128, D], F32, "xnb%d" % i) for i in range(2)]
                jk = kb.sb([128, D], BF16, "jkp")
                NR = 6
                rows = [kb.sb([128, D], BF16, "rows%d" % i) for i in range(NR)]
                dg = [kb.sb([128, 128], BF16, "dg%d" % i) for i in range(3)]
                sts = [kb.sb([128, 8], F32) for _ in range(2)]
                cand = kb.sb([128, 16, 16], F32)
                cand2 = kb.sb([128, 256], F32)
                cid = kb.sb([128, 16, 16], F32)
                ma = kb.sb([128, 8], F32)
                ia = kb.sb([128, 8], U32)
                sc16 = kb.sb([128, 8, 16], F32)
                sel = kb.sb([128, 16], F32)
                eq = kb.sb([128, 16, 256], F32)
                eidfs = [kb.sb([128, 128], F32) for _ in range(2)]
                eidis = [kb.sb([128, 128], I32) for _ in range(2)]
                gws = [kb.sb([128, 128], F32) for _ in range(2)]
                actvs = [kb.sb([128, 128], F32) for _ in range(2)]
                tg = kb.sb([128, 128], F32)
                coefv = [kb.sb([128, 128], F32) for _ in range(2)]
                sm = kb.sb([128, 8, 4], F32)
                ric = [0]

                def prep(ti):
                    t0, m = OWN_TILES[ti]
                    hx, xnb, st = hxs[ti % 2], xnbs[ti % 2], sts[ti % 2]
                    eidf, eidi, gw = eidfs[ti % 2], eidis[ti % 2], gws[ti % 2]
                    dma("sp", hx[:m, :], h_scr[t0:t0 + m, :], r=[B_h], w=[hx])
                    V(lambda e: e.memset(st[:m, :], 0.0), w=[st])
                    A(lambda e: e.activation(out=jk[:m, :], in_=hx[:m, :], func=AF.Square, accum_out=st[:m, 0:1]), r=[hx], w=[jk, st])
                    V(lambda e: e.tensor_scalar(out=st[:m, 1:2], in0=st[:m, 0:1], scalar1=1.0 / D, scalar2=EPS, op0=ALU.mult, op1=ALU.add), r=[st], w=[st])
                    A(lambda e: e.activation(out=st[:m, 2:3], in_=st[:m, 1:2], func=AF.Sqrt), r=[st], w=[st])
                    V(lambda e: e.reciprocal(out=st[:m, 3:4], in_=st[:m, 2:3]), r=[st], w=[st])
                    V(lambda e: e.scalar_tensor_tensor(out=xnb[:m, :], in0=hx[:m, :], scalar=st[:m, 3:4], in1=gt_ffn[:m, :], op0=ALU.mult, op1=ALU.mult),
                      r=[hx, st, gt_ffn], w=[xnb])
                    for h in range(8):
                        v1 = TV[:m, ti, 2 * h, :]
                        v2 = TV[:m, ti, 2 * h + 1, :]
                        i1 = TI[:m, ti, 2 * h, :]
                        i2 = TI[:m, ti, 2 * h + 1, :]
                        V(lambda e: e.tensor_tensor(out=cand[:m], in0=v1.unsqueeze(2).to_broadcast([m, 16, 16]),
                                                    in1=v2.unsqueeze(1).to_broadcast([m, 16, 16]), op=ALU.add), r=[TV], w=[cand])
                        V(lambda e: e.scalar_tensor_tensor(out=cid[:m], in0=i1.unsqueeze(2).to_broadcast([m, 16, 16]), scalar=128.0,
                                                           in1=i2.unsqueeze(1).to_broadcast([m, 16, 16]), op0=ALU.mult, op1=ALU.add), r=[TI], w=[cid])
                        cflat = cand.t[:m].rearrange("p a b -> p (a b)")
                        for rnd in range(2):
                            srcc = cflat if rnd == 0 else cand2[:m, :]
                            V(lambda e: e.max(out=ma[:m, :], in_=srcc), r=[cand, cand2], w=[ma])
                            V(lambda e: e.max_index(out=ia[:m, :], in_max=ma[:m, :], in_values=srcc), r=[ma, cand, cand2], w=[ia])
                            V(lambda e: e.tensor_copy(out=sc16[:m, h, 8 * rnd:8 * rnd + 8], in_=ma[:m, :]), r=[ma], w=[sc16])
                            V(lambda e: e.tensor_copy(out=sel[:m, 8 * rnd:8 * rnd + 8], in_=ia[:m, :]), r=[ia], w=[sel])
                            if rnd == 0:
                                V(lambda e: e.match_replace(out=cand2[:m, :], in_to_replace=ma[:m, :], in_values=cflat, imm_value=-1e30),
                                  r=[ma, cand], w=[cand2])
                        V(lambda e: e.tensor_tensor(out=eq[:m], in0=iota256[:m, :].unsqueeze(1).to_broadcast([m, 16, 256]),
                                                    in1=sel[:m, :].unsqueeze(2).to_broadcast([m, 16, 256]), op=ALU.is_equal), r=[cst, sel], w=[eq])
                        V(lambda e: e.tensor_tensor(out=eq[:m], in0=eq[:m], in1=cid.t[:m].rearrange("p a b -> p (a b)").unsqueeze(1).to_broadcast([m, 16, 256]),
                                                    op=ALU.mult), r=[eq, cid], w=[eq])
                        V(lambda e: e.reduce_sum(out=eidf[:m, 16 * h:16 * h + 16], in_=eq[:m], axis=AX.X), r=[eq], w=[eidf])
                        V(lambda e: e.tensor_scalar(out=sm[:m, h, 0:1], in0=sc16[:m, h, 0:1], scalar1=-1.0, scalar2=None, op0=ALU.mult), r=[sc16], w=[sm])
                        V(lambda e: e.memset(sm[:m, h, 1:2], 0.0), w=[sm])
                        A(lambda e: e.activation(out=gw[:m, 16 * h:16 * h + 16], in_=sc16[:m, h, :], func=AF.Exp, bias=sm[:m, h, 0:1], scale=1.0,
                                                 accum_out=sm[:m, h, 1:2]), r=[sc16, sm], w=[gw, sm])
                        V(lambda e: e.reciprocal(out=sm[:m, h, 2:3], in_=sm[:m, h, 1:2]), r=[sm], w=[sm])
                        V(lambda e: e.tensor_scalar(out=gw[:m, 16 * h:16 * h + 16], in0=gw[:m, 16 * h:16 * h + 16], scalar1=sm[:m, h, 2:3], scalar2=None,
                                                    op0=ALU.mult), r=[gw, sm], w=[gw])
                    V(lambda e: e.tensor_copy(out=eidi[:m, :], in_=eidf[:m, :]), r=[eidf], w=[eidi])
                    V(lambda e: e.memset(actvs[ti % 2][:m, :], 0.0), w=[actvs[ti % 2]])

                def u_slot(ti, sl):
                    t0, m = OWN_TILES[ti]
                    xnb, eidi, actv = xnbs[ti % 2], eidis[ti % 2], actvs[ti % 2]
                    r_ = rows[ric[0] % NR]
                    ric[0] += 1
                    dma("pool", r_[:m, :], ub_scr[:, :], r=[eidi, B_ub], w=[r_],
                        indirect=bass.IndirectOffsetOnAxis(ap=eidi[:m, sl:sl + 1], axis=0))
                    V(lambda e: e.scalar_tensor_tensor(out=jk[:m, :], in0=r_[:m, :], scalar=1.0, in1=xnb[:m, :], op0=ALU.mult, op1=ALU.mult,
                                                       accum_out=actv[:m, sl:sl + 1]), r=[r_, xnb], w=[jk, actv])

                def coefs(ti):
                    t0, m = OWN_TILES[ti]
                    actv, coef, gw = actvs[ti % 2], coefv[ti % 2], gws[ti % 2]
                    A(lambda e: e.activation(out=tg[:m, :], in_=actv[:m, :], func=AF.Square), r=[actv], w=[tg])
                    V(lambda e: e.tensor_scalar(out=tg[:m, :], in0=tg[:m, :], scalar1=0.044715, scalar2=1.0, op0=ALU.mult, op1=ALU.add), r=[tg], w=[tg])
                    V(lambda e: e.tensor_tensor(out=tg[:m, :], in0=tg[:m, :], in1=actv[:m, :], op=ALU.mult), r=[tg, actv], w=[tg])
                    A(lambda e: e.activation(out=tg[:m, :], in_=tg[:m, :], func=AF.Sigmoid, scale=1.5957691216057308), r=[tg], w=[tg])
                    V(lambda e: e.tensor_tensor(out=coef[:m, :], in0=tg[:m, :], in1=actv[:m, :], op=ALU.mult), r=[tg, actv], w=[coef])
                    V(lambda e: e.tensor_tensor(out=coef[:m, :], in0=coef[:m, :], in1=gw[:m, :], op=ALU.mult), r=[coef, gw], w=[coef])

                vcnt = [0]

                def v_slot(ti):
                    t0, m = OWN_TILES[ti]
                    sl = vcnt[0]
                    vcnt[0] += 1
                    eidi, coef = eidis[ti % 2], coefv[ti % 2]
                    r_ = rows[ric[0] % NR]
                    d_ = dg[ric[0] % 3]
                    ric[0] += 1
                    dma("pool", r_[:m, :], vb_scr[:, :], r=[eidi, B_vb], w=[r_],
                        indirect=bass.IndirectOffsetOnAxis(ap=eidi[:m, sl:sl + 1], axis=0))
                    A(lambda e: e.activation(out=d_[:m, :m], in_=identb[:m, :m], func=AF.Copy, scale=coef[:m, sl:sl + 1]), r=[identb, coef], w=[d_])
                    for c8 in range(8):
                        P(lambda e: e.matmul(banks[c8][:m, :], lhsT=d_[:m, :m], rhs=r_[:m, c8 * 512:(c8 + 1) * 512], start=(sl == 0), stop=(sl == 127)),
                          r=[d_, r_], w=[banks[c8]], acc=sl > 0, inc=(c8 == 7))

                def final(ti):
                    t0, m = OWN_TILES[ti]
                    hx, st = hxs[ti % 2], sts[ti % 2]
                    for c8 in range(8):
                        V(lambda e: e.tensor_tensor(out=hx[:m, c8 * 512:(c8 + 1) * 512], in0=hx[:m, c8 * 512:(c8 + 1) * 512], in1=banks[c8][:m, :], op=ALU.add),
                          r=[hx, banks[c8]], w=[hx])
                    V(lambda e: e.memset(st[:m, 4:8], 0.0), w=[st])
                    A(lambda e: e.activation(out=jk[:m, :], in_=hx[:m, :], func=AF.Square, accum_out=st[:m, 4:5]), r=[hx], w=[jk, st])
                    V(lambda e: e.tensor_scalar(out=st[:m, 5:6], in0=st[:m, 4:5], scalar1=1.0 / D, scalar2=EPS, op0=ALU.mult, op1=ALU.add), r=[st], w=[st])
                    A(lambda e: e.activation(out=st[:m, 6:7], in_=st[:m, 5:6], func=AF.Sqrt), r=[st], w=[st])
                    V(lambda e: e.reciprocal(out=st[:m, 7:8], in_=st[:m, 6:7]), r=[st], w=[st])
                    eqf = eq.t[:m].rearrange("p a b -> p (a b)")
                    V(lambda e: e.scalar_tensor_tensor(out=eqf, in0=hx[:m, :], scalar=st[:m, 7:8], in1=gt_fin[:m, :], op0=ALU.mult, op1=ALU.mult),
                      r=[hx, st, gt_fin], w=[eq])
                    dma("sp", yo[t0:t0 + m, :], eqf, r=[eq])

                NTL = len(OWN_TILES)
                prep(0)
                for sl in range(128):
                    u_slot(0, sl)
                coefs(0)
                for ti in range(NTL):
                    vcnt[0] = 0
                    nxt = ti + 1 < NTL
                    if nxt:
                        for _ in range(80):
                            v_slot(ti)
                        prep(ti + 1)
                        for sl in range(128):
                            u_slot(ti + 1, sl)
                            if sl % 8 < 3:
                                v_slot(ti)
                        assert vcnt[0] == 128
                        coefs(ti + 1)
                    else:
                        for _ in range(128):
                            v_slot(ti)
                    final(ti)
        kb.finish()
    return nc


_NC = [None]


def kernel(x_prompt, x_sample, state_s5_re, state_s5_im, state_gla, meta_tokens,
           norm_mix_g, w_in, s5_lam_re, s5_lam_im, s5_log_dt, s5_b_re, s5_b_im,
           s5_c_re, s5_c_im, s5_d, s5_w_glu, s5_b_glu, s5_norm_g,
           gla_w_gate2, gla_b_gate2, gla_norm_g, w_out, norm_ffn_g,
           peer_w_q, peer_keys, peer_u, peer_v, norm_final_g):
    f = lambda a: np.ascontiguousarray(np.asarray(a, dtype=np.float32))
    x_prompt, x_sample, meta_tokens = f(x_prompt), f(x_sample), f(meta_tokens)
    if _NC[0] is None:
        _NC[0] = build()
    nc = _NC[0]
    shared = {
        "cst": _consts(), "norm_mix_g": f(norm_mix_g)[0], "w_in": f(w_in)[0], "lam_re": f(s5_lam_re)[0], "lam_im": f(s5_lam_im)[0],
        "log_dt": f(s5_log_dt)[0], "b_re": f(s5_b_re)[0], "b_im": f(s5_b_im)[0], "c_re": f(s5_c_re)[0], "c_im": f(s5_c_im)[0],
        "s5_d": f(s5_d)[0], "w_glu": f(s5_w_glu)[0], "b_glu": f(s5_b_glu)[0], "s5_norm_g": f(s5_norm_g)[0],
        "w_gate2": f(gla_w_gate2)[0], "b_gate2": f(gla_b_gate2)[0], "gla_norm_g": f(gla_norm_g)[0], "w_out": f(w_out)[0],
        "norm_ffn_g": f(norm_ffn_g)[0], "w_q": f(peer_w_q)[0], "keys": f(peer_keys)[0].reshape(16, 128, 128),
        "peer_u": f(peer_u)[0], "peer_v": f(peer_v)[0], "norm_final_g": f(norm_final_g),
    }
    s5re, s5im, sgl = f(state_s5_re)[0], f(state_s5_im)[0], f(state_gla)[0]
    in_maps = []
    for c in range(8):
        b, half = divmod(c, 2)
        xs = x_sample[16 * c:16 * c + 16, 0, :]
        if half == 0:
            xo = np.concatenate([meta_tokens, x_prompt[b, :1024], xs], axis=0)
            xp = np.zeros((NPF, D), np.float32)
        else:
            xo = np.concatenate([x_prompt[b, 1008:2048], xs], axis=0)
            xp = np.concatenate([meta_tokens, x_prompt[b, :1008]], axis=0)
        m = dict(shared)
        m.update({"xo": np.ascontiguousarray(xo), "xp": np.ascontiguousarray(xp),
                  "s5re": np.ascontiguousarray(s5re[16 * c:16 * c + 16]), "s5im": np.ascontiguousarray(s5im[16 * c:16 * c + 16]),
                  "sgla": np.ascontiguousarray(sgl[16 * c:16 * c + 16])})
        in_maps.append(m)
    res = run_bass_kernel_spmd(nc, in_maps, core_ids=list(range(8)))
    R = res.results
    y_prompt = np.zeros((4, 2048, D), np.float32)
    y_sample = np.zeros((128, 1, D), np.float32)
    p_re = np.zeros((1, 4, 128, 64), np.float32)
    p_im = np.zeros((1, 4, 128, 64), np.float32)
    p_gla = np.zeros((1, 4, 4, 256, 512), np.float32)
    s_re = np.zeros((1, 128, 128, 64), np.float32)
    s_im = np.zeros((1, 128, 128, 64), np.float32)
    s_gla = np.zeros((1, 128, 4, 256, 512), np.float32)
    for c in range(8):
        b, half = divmod(c, 2)
        r = R[c]
        yo = np.asarray(r["yo"])
        y_prompt[b, 1024 * half:1024 * (half + 1)] = yo[16:1040]
        y_sample[16 * c:16 * c + 16, 0] = yo[1040:1056]
        if half == 1:
            o5 = np.asarray(r["o_s5"])
            p_re[0, b] = o5[0].reshape(128, 64)
            p_im[0, b] = o5[1].reshape(128, 64)
            p_gla[0, b] = np.asarray(r["o_gla"]).reshape(4, 256, 512)
        o5s = np.asarray(r["o_s5s"])
        s_re[0, 16 * c:16 * c + 16] = o5s[:, 0].reshape(16, 128, 64)
        s_im[0, 16 * c:16 * c + 16] = o5s[:, 1].reshape(16, 128, 64)
        s_gla[0, 16 * c:16 * c + 16] = np.asarray(r["o_glas"]).reshape(16, 4, 256, 512)
    return (y_prompt, y_sample, p_re, p_im, p_gla, s_re, s_im, s_gla)
```

```python
import contextlib
import numpy as np
import concourse.bass as bass
import concourse.mybir as mybir
from concourse.bass_utils import run_bass_kernel_spmd

F32 = mybir.dt.float32
BF16 = mybir.dt.bfloat16
I32 = mybir.dt.int32
U32 = mybir.dt.uint32
ALU = mybir.AluOpType
AF = mybir.ActivationFunctionType
AX = mybir.AxisListType

D = 4096
DIN = 8208
NT = 1056
LO = 1040
NS = 16
NPF = 1024
EPS = 1e-6
TWO_PI = 6.283185307179586
C1 = 6.28125
C2 = TWO_PI - C1
PI = 3.141592653589793

OWN_TILES = [(i * 128, 128) for i in range(8)] + [(1024, 32)]
PRE_TILES = [(i * 128, 128) for i in range(8)]
OWN_PIECES = [(0, 352), (352, 352), (704, 352)]
PRE_PIECES = [(0, 512), (512, 512)]
OWN_CHUNKS = [(0, 16)] + [(16 + 128 * i, 128) for i in range(8)]
PRE_CHUNKS = [(128 * i, 128) for i in range(8)]

CO_ID, CO_JP, CO_MT, CO_IO, CO_MR, CO_CM, CO_ON = 0, 128, 256, 384, 640, 648, 1672
CO_M2 = 1800
CO_SEL = 1800 + 512
NCST = 1800 + 512 + 128


def _consts():
    c = np.zeros((128, NCST), np.float32)
    p = np.arange(128)
    c[p, CO_ID + p] = 1.0
    c[p, CO_JP + (p + 64) % 128] = 1.0
    c[:, CO_MT:CO_MT + 128] = (p[None, :] >= p[:, None]).astype(np.float32)
    c[:, CO_IO:CO_IO + 256] = np.arange(256, dtype=np.float32)[None, :]
    for gl in range(8):
        c[:, CO_MR + gl] = (p // 16 == gl)
        c[:, CO_CM + gl * 128: CO_CM + (gl + 1) * 128] = (p[None, :] // 16 == gl)
    c[:, CO_ON:CO_ON + 128] = 1.0
    for prl in range(4):
        c[0:64, CO_M2 + prl * 128: CO_M2 + (prl + 1) * 128] = (p[None, :] // 16 == 2 * prl)
        c[64:128, CO_M2 + prl * 128: CO_M2 + (prl + 1) * 128] = (p[None, :] // 16 == 2 * prl + 1)
    pr = np.arange(64)
    c[:, CO_SEL:CO_SEL + 64] = (p[:, None] == 2 * pr[None, :])
    c[:, CO_SEL + 64:CO_SEL + 128] = (p[:, None] == 2 * pr[None, :] + 1)
    return c


class Buf:
    __slots__ = ("w", "r")

    def __init__(self):
        self.w = None
        self.r = {}


class T:
    def __init__(self, t):
        self.t = t
        self.b = Buf()

    def __getitem__(self, k):
        return self.t[k]


class KB:
    def __init__(self, nc, es):
        self.nc = nc
        self.es = [es]
        self.eng = {"pe": nc.tensor, "dve": nc.vector, "act": nc.scalar, "pool": nc.gpsimd, "sp": nc.sync}
        self.sem = {}
        self.cnt = {}
        self.waited = {e: {} for e in self.eng}
        for e in ("pe", "dve", "act", "pool"):
            self.sem[e] = es.enter_context(nc.semaphore("s_" + e))
            self.cnt[e] = 0
        self.dq = {}
        for q, n in (("sp", 8), ("pool", 6), ("act", 2)):
            keys = []
            for i in range(n):
                k = "d_%s%d" % (q, i)
                self.sem[k] = es.enter_context(nc.semaphore(k))
                self.cnt[k] = 0
                keys.append(k)
            self.dq[q] = [keys, 0]
        self.nid = 0
        self.rot = list(range(8))
        self.roti = 0

    def sb(self, shape, dt, name=None):
        self.nid += 1
        return T(self.es[-1].enter_context(self.nc.sbuf_tensor(name or ("t%d" % self.nid), list(shape), dt)))

    @contextlib.contextmanager
    def scope(self):
        es = contextlib.ExitStack()
        self.es.append(es)
        try:
            with es:
                yield
                self.barrier()
        finally:
            self.es.pop()

    def _wait(self, en, key, val):
        if val <= 0 or self.waited[en].get(key, 0) >= val:
            return
        self.eng[en].wait_ge(self.sem[key], val)
        self.waited[en][key] = val

    def _deps(self, en, r, w, acc):
        deps = {}

        def add(st):
            if st is not None:
                deps[st[0]] = max(deps.get(st[0], 0), st[1])
        for b in r:
            add(b.w)
        for b in w:
            if not acc:
                add(b.w)
            for k, v in b.r.items():
                add((k, v))
        for k, v in deps.items():
            if k == "pe" and en == "pe":
                continue
            self._wait(en, k, v)

    @staticmethod
    def _bufs(xs):
        return [x if isinstance(x, Buf) else x.b for x in xs]

    def op(self, en, fn, r=(), w=(), acc=False, inc=True):
        r = self._bufs(r)
        w = self._bufs(w)
        self._deps(en, r, w, acc)
        inst = fn(self.eng[en])
        if inc:
            self.cnt[en] += 1
            inst.then_inc(self.sem[en], 1)
            c = self.cnt[en]
        else:
            c = self.cnt[en] + 1
        for b in r:
            b.r[en] = c
        for b in w:
            b.w = (en, c)
            b.r = {}

    def V(self, fn, r=(), w=()):
        self.op("dve", fn, r, w)

    def A(self, fn, r=(), w=()):
        self.op("act", fn, r, w)

    def G(self, fn, r=(), w=()):
        self.op("pool", fn, r, w)

    def P(self, fn, r=(), w=(), acc=False, inc=True):
        self.op("pe", fn, r, w, acc, inc)

    def dma(self, q, out, in_, r=(), w=(), indirect=None):
        r = self._bufs(r)
        w = self._bufs(w)
        self._deps(q, r, w, False)
        keys, i = self.dq[q]
        key = keys[i % len(keys)]
        self.dq[q][1] = i + 1
        if indirect is not None:
            inst = self.eng[q].indirect_dma_start(out=out, out_offset=None, in_=in_, in_offset=indirect)
        else:
            inst = self.eng[q].dma_start(out=out, in_=in_)
        self.cnt[key] += 16
        inst.then_inc(self.sem[key], 16)
        c = self.cnt[key]
        for b in r:
            b.r[key] = c
        for b in w:
            b.w = (key, c)
            b.r = {}

    def barrier(self):
        for en in self.eng:
            for key in self.sem:
                self._wait(en, key, self.cnt[key])

    def finish(self):
        for key in self.sem:
            self._wait("sp", key, self.cnt[key])


_PH = 3


def build():
    nc = bass.Bass("TRN2", target_bir_lowering=False)

    def din(name, shape, dt=F32):
        return nc.dram_tensor(name, list(shape), dt, kind="ExternalInput").ap()

    def dout(name, shape, dt=F32):
        return nc.dram_tensor(name, list(shape), dt, kind="ExternalOutput").ap()

    xo = din("xo", [NT, D])
    xp = din("xp", [NPF, D])
    s5re = din("s5re", [NS, 128, 64])
    s5im = din("s5im", [NS, 128, 64])
    sgla = din("sgla", [NS, 4, 256, 512])
    cst_d = din("cst", [128, NCST])
    norm_mix_g = din("norm_mix_g", [D])
    w_in = din("w_in", [D, DIN])
    lam_re = din("lam_re", [128, 64])
    lam_im = din("lam_im", [128, 64])
    log_dt = din("log_dt", [128])
    b_re = din("b_re", [128, 64, 16])
    b_im = din("b_im", [128, 64, 16])
    c_re = din("c_re", [128, 16, 64])
    c_im = din("c_im", [128, 16, 64])
    s5_d = din("s5_d", [2048])
    w_glu = din("w_glu", [2048, 2048])
    b_glu = din("b_glu", [2048])
    s5_norm_g = din("s5_norm_g", [2048])
    w_gate2 = din("w_gate2", [16, 1024])
    b_gate2 = din("b_gate2", [1024])
    gla_norm_g = din("gla_norm_g", [2048])
    w_out = din("w_out", [D, D])
    norm_ffn_g = din("norm_ffn_g", [D])
    w_q = din("w_q", [D, 2048])
    keys = din("keys", [16, 128, 128])
    peer_u = din("peer_u", [16384, D])
    peer_v = din("peer_v", [16384, D])
    norm_final_g = din("norm_final_g", [D])

    yo = dout("yo", [NT, D])
    o_s5 = dout("o_s5", [2, 64, 128])
    o_gla = dout("o_gla", [8, 128, 512])
    o_s5s = dout("o_s5s", [NS, 2, 64, 128])
    o_glas = dout("o_glas", [NS, 8, 128, 512])
    def dscr(name, shape, dt=F32):
        return nc.dram_tensor(name, list(shape), dt, kind="Internal").ap()

    h_scr = dscr("h_scr", [NT, D])
    z_scr = dscr("z_scr", [128, 16, NT], BF16)
    v_scr = dscr("v_scr", [NT, 2048], BF16)
    sr_scr = dscr("sr_scr", [NT, 2048], BF16)
    vp_scr = dscr("vp_scr", [NPF, 2048], BF16)
    u_scr = dscr("u_scr", [16, 128, NT])
    up_scr = dscr("up_scr", [16, 128, NPF])
    ub_scr = dscr("ub_scr", [16384, D], BF16)
    vb_scr = dscr("vb_scr", [16384, D], BF16)
    B_ub, B_vb = Buf(), Buf()
    B_h, B_z, B_v, B_sr, B_vp, B_u, B_up = Buf(), Buf(), Buf(), Buf(), Buf(), Buf(), Buf()

    es = contextlib.ExitStack()
    with es:
        kb = KB(nc, es)
        V, A, G, P, dma = kb.V, kb.A, kb.G, kb.P, kb.dma
        big = [es.enter_context(nc.psum_tensor("pbig%d" % i, [128, 1024], F32)) for i in range(4)]

        class PB:
            def __init__(self, t, o):
                self.t_ = t
                self.o = o
                self.b = Buf()

            def __getitem__(self, key):
                rows, colsl = key
                a = colsl.start or 0
                bnd = 512 if colsl.stop is None else colsl.stop
                return self.t_[rows, self.o + a:self.o + bnd]
        banks = [PB(big[i // 2], 512 * (i % 2)) for i in range(8)]

        def nextbank():
            b = banks[kb.rot[kb.roti % len(kb.rot)]]
            kb.roti += 1
            return b

        cst = kb.sb([128, NCST], F32, "cst_sb")
        dma("sp", cst[:], cst_d[:, :], w=[cst])
        ident = cst.t[:, CO_ID:CO_ID + 128]
        jperm = cst.t[:, CO_JP:CO_JP + 128]
        iota256 = cst.t[:, CO_IO:CO_IO + 256]
        ones = cst.t[:, CO_ON:CO_ON + 128]
        identb = kb.sb([128, 128], BF16, "identb")
        V(lambda e: e.tensor_copy(out=identb[:], in_=ident), r=[cst], w=[identb])
        cols = kb.sb([128, 64], F32, "cols")
        HIN = kb.sb([128, 128], F32, "HIN")
        HINI = kb.sb([128, 128], F32, "HINI")
        Sh = [None]
        conv_jobs = [(ub_scr, peer_u, B_ub, i) for i in range(64)] + [(vb_scr, peer_v, B_vb, i) for i in range(64)]

        def conv_step(nmax=1):
            for _ in range(nmax):
                if not conv_jobs:
                    return
                dst, srcd, bb, i = conv_jobs.pop(0)
                dma("pool", dst[256 * i:256 * (i + 1), :], srcd[256 * i:256 * (i + 1), :], w=[bb])
        wsl = []
        wctr = [0]

        @contextlib.contextmanager
        def wslots(n):
            with kb.scope():
                wsl[:] = [kb.sb([128, 32, 256], BF16) for _ in range(n)]
                yield
                wsl[:] = []

        def colvec(vec, nt, dst_ap, neg=False):
            tmp = kb.sb([16, 128], F32)
            dma("sp", tmp[:nt, :], vec.rearrange("(j p) -> j p", p=128), w=[tmp])
            bk = nextbank()
            P(lambda e: e.matmul(bk[:, :nt], lhsT=tmp[:nt, :], rhs=cst.t[:nt, CO_ID:CO_ID + nt], start=True, stop=True),
              r=[tmp, cst], w=[bk])
            if neg:
                V(lambda e: e.tensor_scalar(out=dst_ap, in0=bk[:, :nt], scalar1=-1.0, scalar2=None, op0=ALU.mult),
                  r=[bk], w=[cols])
            else:
                V(lambda e: e.tensor_copy(out=dst_ap, in_=bk[:, :nt]), r=[bk], w=[cols])

        def transp32(dst_ap, dstT, src_ap, srcT):
            bk = nextbank()
            P(lambda e: e.matmul(bk[:, :128], lhsT=src_ap, rhs=ident, start=True, stop=True), r=[srcT, cst], w=[bk])
            A(lambda e: e.activation(out=dst_ap, in_=bk[:, :128], func=AF.Copy), r=[bk], w=[dstT])

        with kb.scope():
            colvec(s5_d, 16, cols.t[:, 0:16])
            colvec(b_glu, 16, cols.t[:, 16:32])
            colvec(s5_norm_g, 16, cols.t[:, 32:48])
            colvec(b_gate2, 8, cols.t[:, 48:56], neg=True)

        def s5_setup(PRV, PIV, BT, CT, AH0, PRP, PIP, NPIP, CTI, AH0I):
          with kb.scope():
            PRV = kb.sb([128, 11, 128], F32, "PRV")
            PIV = kb.sb([128, 11, 128], F32, "PIV")
            N1 = kb.sb([128, 128], F32)
            N2 = kb.sb([128, 128], F32)
            LR = kb.sb([128, 128], F32)
            LI = kb.sb([128, 128], F32)
            DT = kb.sb([128, 128], F32)
            RD = kb.sb([128, 128], F32)
            TH = kb.sb([128, 128], F32)
            AIpm = kb.sb([128, 128], F32)
            dma("sp", N1[:, 0:64], lam_re[:, :], w=[N1])
            dma("sp", N1[:, 64:128], lam_re[:, :], w=[N1])
            dma("sp", N2[:, 0:64], lam_im[:, :], w=[N2])
            dma("sp", N2[:, 64:128], lam_im[:, :], w=[N2])
            transp32(LR[:], LR, N1[:], N1)
            transp32(LI[:], LI, N2[:], N2)
            dma("sp", DT[:], log_dt.partition_broadcast(128), w=[DT])
            A(lambda e: e.activation(out=DT[:], in_=DT[:], func=AF.Exp), r=[DT], w=[DT])
            V(lambda e: e.tensor_tensor(out=RD[:], in0=LR[:], in1=DT[:], op=ALU.mult), r=[LR, DT], w=[RD])
            V(lambda e: e.tensor_tensor(out=TH[:], in0=LI[:], in1=DT[:], op=ALU.mult), r=[LI, DT], w=[TH])
            mag = kb.sb([128, 128], F32)
            thk = kb.sb([128, 128], F32)
            xs_ = kb.sb([128, 128], F32)
            ni = kb.sb([128, 128], I32)
            nf = kb.sb([128, 128], F32)
            rr = kb.sb([128, 128], F32)
            sn = kb.sb([128, 128], F32)
            cs = kb.sb([128, 128], F32)
            AIu = kb.sb([128, 128], F32)

            def sin_of(dst, src, shift):
                V(lambda e: e.tensor_scalar(out=xs_[:], in0=src[:], scalar1=shift, scalar2=1.0 / TWO_PI, op0=ALU.add, op1=ALU.mult),
                  r=[src], w=[xs_])
                V(lambda e: e.tensor_copy(out=ni[:], in_=xs_[:]), r=[xs_], w=[ni])
                V(lambda e: e.tensor_copy(out=nf[:], in_=ni[:]), r=[ni], w=[nf])
                V(lambda e: e.tensor_scalar(out=xs_[:], in0=src[:], scalar1=shift, scalar2=None, op0=ALU.add), r=[src], w=[xs_])
                V(lambda e: e.scalar_tensor_tensor(out=rr[:], in0=nf[:], scalar=-C1, in1=xs_[:], op0=ALU.mult, op1=ALU.add),
                  r=[nf, xs_], w=[rr])
                V(lambda e: e.scalar_tensor_tensor(out=rr[:], in0=nf[:], scalar=-C2, in1=rr[:], op0=ALU.mult, op1=ALU.add),
                  r=[nf, rr], w=[rr])
                V(lambda e: e.tensor_scalar(out=rr[:], in0=rr[:], scalar1=PI, scalar2=-PI, op0=ALU.min, op1=ALU.max), r=[rr], w=[rr])
                A(lambda e: e.activation(out=dst[:], in_=rr[:], func=AF.Sin), r=[rr], w=[dst])

            for k in range(11):
                sc = float(1 << k)
                A(lambda e: e.activation(out=mag[:], in_=RD[:], func=AF.Exp, scale=sc), r=[RD], w=[mag])
                V(lambda e: e.tensor_scalar(out=thk[:], in0=TH[:], scalar1=sc, scalar2=None, op0=ALU.mult), r=[TH], w=[thk])
                sin_of(sn, thk, 0.0)
                sin_of(cs, thk, PI / 2)
                V(lambda e: e.tensor_tensor(out=PRV[:, k, :], in0=mag[:], in1=cs[:], op=ALU.mult), r=[mag, cs], w=[PRV])
                V(lambda e: e.tensor_tensor(out=PIV[:, k, :], in0=mag[:], in1=sn[:], op=ALU.mult), r=[mag, sn], w=[PIV])
                if k == 0:
                    V(lambda e: e.tensor_copy(out=AIu[:], in_=PIV[:, 0, :]), r=[PIV], w=[AIu])
                V(lambda e: e.tensor_scalar(out=PIV[0:64, k, :], in0=PIV[0:64, k, :], scalar1=-1.0, scalar2=None, op0=ALU.mult),
                  r=[PIV], w=[PIV])
                if k == 0:
                    V(lambda e: e.tensor_copy(out=AIpm[:], in_=PIV[:, 0, :]), r=[PIV], w=[AIpm])
            den = kb.sb([128, 128], F32)
            t1 = kb.sb([128, 128], F32)
            t2 = kb.sb([128, 128], F32)
            nr = kb.sb([128, 128], F32)
            QR = kb.sb([128, 128], F32)
            QI = kb.sb([128, 128], F32)
            V(lambda e: e.tensor_tensor(out=den[:], in0=LR[:], in1=LR[:], op=ALU.mult), r=[LR], w=[den])
            V(lambda e: e.tensor_tensor(out=t1[:], in0=LI[:], in1=LI[:], op=ALU.mult), r=[LI], w=[t1])
            V(lambda e: e.tensor_tensor(out=den[:], in0=den[:], in1=t1[:], op=ALU.add), r=[den, t1], w=[den])
            V(lambda e: e.reciprocal(out=den[:], in_=den[:]), r=[den], w=[den])
            V(lambda e: e.tensor_scalar(out=nr[:], in0=PRV[:, 0, :], scalar1=-1.0, scalar2=None, op0=ALU.add), r=[PRV], w=[nr])
            V(lambda e: e.tensor_tensor(out=t1[:], in0=nr[:], in1=LR[:], op=ALU.mult), r=[nr, LR], w=[t1])
            V(lambda e: e.tensor_tensor(out=t2[:], in0=AIu[:], in1=LI[:], op=ALU.mult), r=[AIu, LI], w=[t2])
            V(lambda e: e.tensor_tensor(out=t1[:], in0=t1[:], in1=t2[:], op=ALU.add), r=[t1, t2], w=[t1])
            V(lambda e: e.tensor_tensor(out=QR[:], in0=t1[:], in1=den[:], op=ALU.mult), r=[t1, den], w=[QR])
            V(lambda e: e.tensor_tensor(out=t1[:], in0=AIu[:], in1=LR[:], op=ALU.mult), r=[AIu, LR], w=[t1])
            V(lambda e: e.tensor_tensor(out=t2[:], in0=nr[:], in1=LI[:], op=ALU.mult), r=[nr, LI], w=[t2])
            V(lambda e: e.tensor_tensor(out=t1[:], in0=t1[:], in1=t2[:], op=ALU.subtract), r=[t1, t2], w=[t1])
            V(lambda e: e.tensor_tensor(out=QI[:], in0=t1[:], in1=den[:], op=ALU.mult), r=[t1, den], w=[QI])
            V(lambda e: e.tensor_scalar(out=QI[0:64, :], in0=QI[0:64, :], scalar1=-1.0, scalar2=None, op0=ALU.mult), r=[QI], w=[QI])
            Bsame = kb.sb([128, 128, 16], F32)
            Bswap = kb.sb([128, 128, 16], F32)
            bre_v = b_re.rearrange("g p h -> p g h")
            bim_v = b_im.rearrange("g p h -> p g h")
            dma("sp", Bsame[0:64, :, :], bre_v, w=[Bsame])
            dma("sp", Bsame[64:128, :, :], bim_v, w=[Bsame])
            dma("sp", Bswap[0:64, :, :], bim_v, w=[Bswap])
            dma("sp", Bswap[64:128, :, :], bre_v, w=[Bswap])
            V(lambda e: e.tensor_tensor(out=Bsame[:], in0=Bsame[:], in1=QR[:].unsqueeze(2).to_broadcast([128, 128, 16]), op=ALU.mult),
              r=[Bsame, QR], w=[Bsame])
            V(lambda e: e.tensor_tensor(out=Bswap[:], in0=Bswap[:], in1=QI[:].unsqueeze(2).to_broadcast([128, 128, 16]), op=ALU.mult),
              r=[Bswap, QI], w=[Bswap])
            V(lambda e: e.tensor_tensor(out=Bsame[:], in0=Bsame[:], in1=Bswap[:], op=ALU.add), r=[Bsame, Bswap], w=[Bsame])
            for j in range(16):
                transp32(BT[:, j, :], BT, Bsame[:, 8 * j:8 * j + 8, :].rearrange("p g h -> p (g h)"), Bsame)
            prv4 = PRV.t[:, :, :].rearrange("p k (pr two) -> p k pr two", two=2)
            piv4 = PIV.t[:, :, :].rearrange("p k (pr two) -> p k pr two", two=2)
            V(lambda e: e.tensor_copy(out=PRP[0:64, :, :], in_=prv4[0:64, :, :, 0]), r=[PRV], w=[PRP])
            V(lambda e: e.tensor_copy(out=PRP[64:128, :, :], in_=prv4[64:128, :, :, 1]), r=[PRV], w=[PRP])
            V(lambda e: e.tensor_scalar(out=PIP[0:64, :, :], in0=piv4[0:64, :, :, 0], scalar1=-1.0, scalar2=None, op0=ALU.mult), r=[PIV], w=[PIP])
            V(lambda e: e.tensor_copy(out=PIP[64:128, :, :], in_=piv4[64:128, :, :, 1]), r=[PIV], w=[PIP])
            V(lambda e: e.tensor_scalar(out=NPIP[:, :, :], in0=PIP[:, :, :], scalar1=-1.0, scalar2=None, op0=ALU.mult), r=[PIP], w=[NPIP])
            Cn = kb.sb([128, 16, 128], F32)
            for (srcc, dstT, neg) in ((c_re, CT, False), (c_im, CTI, True)):
                dma("sp", Cn[:, :, 0:64], srcc.rearrange("(j q) h p -> (q h) j p", q=8), r=[CT, CTI], w=[Cn])
                dma("sp", Cn[:, :, 64:128], srcc.rearrange("(j q) h p -> (q h) j p", q=8), w=[Cn])
                for j in range(16):
                    transp32(dstT[:, j, :], dstT, Cn[:, j, :], Cn)
                if neg:
                    V(lambda e: e.tensor_scalar(out=dstT[:, :, :], in0=dstT[:, :, :], scalar1=-1.0, scalar2=None, op0=ALU.mult), r=[dstT], w=[dstT])
            sel0 = cst.t[:, CO_SEL:CO_SEL + 64]
            sel1 = cst.t[:, CO_SEL + 64:CO_SEL + 128]
            Nr = [kb.sb([128, 64], F32) for _ in range(2)]
            Ni = [kb.sb([128, 64], F32) for _ in range(2)]
            for i in range(NS):
                nr_, ni_ = Nr[i % 2], Ni[i % 2]
                dma("sp", nr_[:, :], s5re[i], w=[nr_])
                dma("sp", ni_[:, :], s5im[i], w=[ni_])
                b1 = nextbank()
                P(lambda e: e.matmul(b1[0:64, 0:64], lhsT=nr_[:, :], rhs=sel0, start=True, stop=True), r=[nr_, cst], w=[b1], inc=False)
                P(lambda e: e.matmul(b1[64:128, 0:64], lhsT=nr_[:, :], rhs=sel1, start=True, stop=True), r=[nr_, cst], w=[b1], acc=True)
                b2 = nextbank()
                P(lambda e: e.matmul(b2[0:64, 0:64], lhsT=ni_[:, :], rhs=sel0, start=True, stop=True), r=[ni_, cst], w=[b2], inc=False)
                P(lambda e: e.matmul(b2[64:128, 0:64], lhsT=ni_[:, :], rhs=sel1, start=True, stop=True), r=[ni_, cst], w=[b2], acc=True)
                V(lambda e: e.tensor_tensor(out=t1[:, 0:64], in0=b1[:, 0:64], in1=PRP[:, 0, :], op=ALU.mult), r=[b1, PRP], w=[t1])
                V(lambda e: e.tensor_tensor(out=t2[:, 0:64], in0=b2[:, 0:64], in1=NPIP[:, 0, :], op=ALU.mult), r=[b2, NPIP], w=[t2])
                V(lambda e: e.tensor_tensor(out=AH0[:, i, :], in0=t1[:, 0:64], in1=t2[:, 0:64], op=ALU.add), r=[t1, t2], w=[AH0])
                V(lambda e: e.tensor_tensor(out=t1[:, 0:64], in0=b2[:, 0:64], in1=PRP[:, 0, :], op=ALU.mult), r=[b2, PRP], w=[t1])
                V(lambda e: e.tensor_tensor(out=t2[:, 0:64], in0=b1[:, 0:64], in1=PIP[:, 0, :], op=ALU.mult), r=[b1, PIP], w=[t2])
                V(lambda e: e.tensor_tensor(out=AH0I[:, i, :], in0=t1[:, 0:64], in1=t2[:, 0:64], op=ALU.add), r=[t1, t2], w=[AH0I])

        def bcast_gain(vec, n):
            gt = kb.sb([128, n], F32)
            dma("sp", gt[:], vec.partition_broadcast(128), w=[gt])
            return gt

        def normA(X, XB, tiles, gvec, xnT):
            with kb.scope():
                gt = bcast_gain(gvec, D)
                as_ = [kb.sb([128, D], F32) for _ in range(2)]
                b = kb.sb([128, D], BF16)
                sts_ = [kb.sb([128, 4], F32) for _ in range(2)]
                for i, (t0, m) in enumerate(tiles):
                    a = as_[i % 2]
                    st = sts_[i % 2]
                    dma("sp", a[:m, :], X[t0:t0 + m, :], r=[XB] if XB is not None else [], w=[a])
                    V(lambda e: e.memset(st[:m, :], 0.0), w=[st])
                    A(lambda e: e.activation(out=b[:m, :], in_=a[:m, :], func=AF.Square, accum_out=st[:m, 0:1]), r=[a], w=[b, st])
                    V(lambda e: e.tensor_scalar(out=st[:m, 1:2], in0=st[:m, 0:1], scalar1=1.0 / D, scalar2=EPS, op0=ALU.mult, op1=ALU.add),
                      r=[st], w=[st])
                    A(lambda e: e.activation(out=st[:m, 2:3], in_=st[:m, 1:2], func=AF.Sqrt), r=[st], w=[st])
                    V(lambda e: e.reciprocal(out=st[:m, 3:4], in_=st[:m, 2:3]), r=[st], w=[st])
                    V(lambda e: e.scalar_tensor_tensor(out=b[:m, :], in0=a[:m, :], scalar=st[:m, 3:4], in1=gt[:m, :], op0=ALU.mult, op1=ALU.mult),
                      r=[a, st, gt], w=[b])
                    for q in range(8):
                        bk = nextbank()
                        for k4 in range(4):
                            kc = 4 * q + k4
                            P(lambda e: e.matmul(bk[:, k4 * 128:k4 * 128 + m], lhsT=b[:m, kc * 128:(kc + 1) * 128], rhs=identb[:m, :m],
                                                 start=True, stop=True), r=[b, identb], w=[bk], acc=k4 > 0, inc=(k4 == 3))
                        src = bk[:, :].rearrange("p (k c) -> p k c", c=128)[:, :, :m]
                        if q % 2 == 0:
                            A(lambda e: e.activation(out=xnT[:, 4 * q:4 * q + 4, t0:t0 + m], in_=src, func=AF.Copy), r=[bk], w=[xnT])
                        else:
                            V(lambda e: e.tensor_copy(out=xnT[:, 4 * q:4 * q + 4, t0:t0 + m], in_=src), r=[bk], w=[xnT])

        def loadW(wd, KC, b0, bw):
            W = wsl[wctr[0] % len(wsl)]
            wctr[0] += 1
            dma("pool", W[:, :KC, :bw], wd[:, b0:b0 + bw].rearrange("(kc p) c -> p kc c", p=128), w=[W])
            return W

        def projF(xnT, KC, pieces, wd, c0, ncols, cons):
            for b0 in range(c0, c0 + ncols, 256):
                bw = min(256, c0 + ncols - b0)
                W = loadW(wd, KC, b0, bw)
                for cc in range(0, bw, 128):
                    cw = min(128, bw - cc)
                    for (t0, tn) in pieces:
                        bk = nextbank()
                        for kc in range(KC):
                            P(lambda e: e.matmul(bk[:cw, :tn], lhsT=W[:, kc, cc:cc + cw], rhs=xnT[:, kc, t0:t0 + tn],
                                                 start=(kc == 0), stop=(kc == KC - 1)), r=[W, xnT], w=[bk], acc=kc > 0, inc=(kc == KC - 1))
                        cons((b0 + cc - c0) // 128, cw, t0, tn, bk)

        def projT(xnT, KC, tiles, wd, c0, ncols, cons):
            for b0 in range(c0, c0 + ncols, 256):
                W = loadW(wd, KC, b0, 256)
                for (t0, m) in tiles:
                    bk = nextbank()
                    for kc in range(KC):
                        P(lambda e: e.matmul(bk[:m, :256], lhsT=xnT[:, kc, t0:t0 + m], rhs=W[:, kc, :256],
                                             start=(kc == 0), stop=(kc == KC - 1)), r=[W, xnT], w=[bk], acc=kc > 0, inc=(kc == KC - 1))
                    cons(b0 - c0, 256, t0, m, bk)

        def proj_u(X, tiles, pieces, n, udst, Bu):
            with kb.scope():
                xnT = kb.sb([128, 32, n], BF16)
                normA(X, None, tiles, norm_mix_g, xnT)
                with wslots(2):
                    ust = [kb.sb([128, 512], F32) for _ in range(2)]
                    ctr = [0]

                    def cons_u(ci, cw, t0, tn, bk):
                        u_ = ust[ctr[0] % 2]
                        ctr[0] += 1
                        A(lambda e: e.activation(out=u_[:, :tn], in_=bk[:, :tn], func=AF.Copy), r=[bk], w=[u_])
                        dma("sp", udst[ci, :, t0:t0 + tn], u_[:, :tn], r=[u_], w=[Bu])
                    projF(xnT, 32, pieces, w_in, 0, 2048, cons_u)

        def s5_run(usrc, Bu, n, pieces, own, tabs):
            (PRV, PIV, BT, CT, AH0, HSN, HF, PRP, PIP, NPIP, CTI, AH0I, HSNI, HFI, HINI) = tabs
            with kb.scope():
                Hn = (1 + NT) if own else NPF
                nb_ = 2 if own else 1
                HB = [[[kb.sb([128, Hn], F32) for _ in range(2)] for _ in range(nb_)] for _ in range(2)]
                HD = [[[Buf() for _ in range(2)] for _ in range(nb_)] for _ in range(2)]
                BTm = [[kb.sb([128, 128], F32) for _ in range(2)] for _ in range(2)]
                CTm = [[kb.sb([128, 128], F32) for _ in range(2)] for _ in range(2)]
                uTj = [kb.sb([128, n], F32) for _ in range(2)]
                if own:
                    ysb = kb.sb([128, NT], F32)
                    tg = kb.sb([128, NT], F32)
                    zst = [kb.sb([128, NT], BF16) for _ in range(2)]
                    ybank = [banks[5], banks[6], banks[7]]
                    kb.rot = [0, 1, 2, 3, 4]
                off = 1 if own else 0
                Lt = 1 + LO
                for pp in range(32):
                    prs = (2 * pp, 2 * pp + 1)
                    j = pp // 2
                    uT = uTj[j % 2]
                    conv_step(2)
                    if pp % 2 == 0:
                        dma("sp", uT[:, :], usrc[j, :, :], r=[Bu], w=[uT])
                    for par, pr in enumerate(prs):
                        prl = pr % 4
                        glA, glB = 2 * prl, 2 * prl + 1
                        for c in range(2):
                            bm = BTm[par][c]
                            G(lambda e: e.tensor_scalar(out=bm[:, 0:64], in0=BT[:, j, 64 * c:64 * c + 64], scalar1=cst.t[:, CO_MR + glA:CO_MR + glA + 1],
                                                        scalar2=None, op0=ALU.mult), r=[BT, cst], w=[bm])
                            G(lambda e: e.tensor_scalar(out=bm[:, 64:128], in0=BT[:, j, 64 * c:64 * c + 64], scalar1=cst.t[:, CO_MR + glB:CO_MR + glB + 1],
                                                        scalar2=None, op0=ALU.mult), r=[BT, cst], w=[bm])
                            if own:
                                cm = CTm[par][c]
                                csrc = CT if c == 0 else CTI
                                G(lambda e: e.tensor_tensor(out=cm[:], in0=csrc[:, j, :], in1=cst.t[:, CO_M2 + prl * 128:CO_M2 + (prl + 1) * 128],
                                                            op=ALU.mult), r=[csrc, cst], w=[cm])
                            H0 = HB[par][0][c]
                            for (t0, tn) in pieces:
                                bk = nextbank()
                                P(lambda e: e.matmul(bk[:, :tn], lhsT=bm[:], rhs=uT[:, t0:t0 + tn], start=True, stop=True), r=[bm, uT], w=[bk])
                                A(lambda e: e.activation(out=H0[:, off + t0:off + t0 + tn], in_=bk[:, :tn], func=AF.Copy), r=[bk, HD[par][0][c]], w=[H0])
                            if own:
                                hin = HIN if c == 0 else HINI
                                A(lambda e: e.activation(out=H0[:, 0:1], in_=hin[:, pr:pr + 1], func=AF.Copy), r=[hin], w=[H0])
                    if own:
                        ci = 0
                        for k in range(11):
                            d = 1 << k
                            for par, pr in enumerate(prs):
                                sR, sI = HB[par][ci]
                                dR, dI = HB[par][1 - ci]
                                sRh, sIh = HD[par][ci]
                                dRh, dIh = HD[par][1 - ci]
                                A(lambda e: e.activation(out=dR[:, 0:d], in_=sR[:, 0:d], func=AF.Copy), r=[sR, sRh], w=[dRh])
                                A(lambda e: e.activation(out=dI[:, 0:d], in_=sI[:, 0:d], func=AF.Copy), r=[sI, sIh], w=[dIh])
                            for stage in range(2):
                                for par, pr in enumerate(prs):
                                    sR, sI = HB[par][ci]
                                    dR, dI = HB[par][1 - ci]
                                    sRh, sIh = HD[par][ci]
                                    if stage == 0:
                                        V(lambda e: e.scalar_tensor_tensor(out=dR[:, d:Lt], in0=sR[:, 0:Lt - d], scalar=PRP[:, k, pr:pr + 1], in1=sR[:, d:Lt],
                                                                           op0=ALU.mult, op1=ALU.add), r=[sR, sRh, PRP], w=[dR])
                                        V(lambda e: e.scalar_tensor_tensor(out=dI[:, d:Lt], in0=sI[:, 0:Lt - d], scalar=PRP[:, k, pr:pr + 1], in1=sI[:, d:Lt],
                                                                           op0=ALU.mult, op1=ALU.add), r=[sI, sIh, PRP], w=[dI])
                                    else:
                                        V(lambda e: e.scalar_tensor_tensor(out=dR[:, d:Lt], in0=sI[:, 0:Lt - d], scalar=NPIP[:, k, pr:pr + 1], in1=dR[:, d:Lt],
                                                                           op0=ALU.mult, op1=ALU.add), r=[sI, sIh, NPIP, dR], w=[dR])
                                        V(lambda e: e.scalar_tensor_tensor(out=dI[:, d:Lt], in0=sR[:, 0:Lt - d], scalar=PIP[:, k, pr:pr + 1], in1=dI[:, d:Lt],
                                                                           op0=ALU.mult, op1=ALU.add), r=[sR, sRh, PIP, dI], w=[dI])
                            ci = 1 - ci
                        for par, pr in enumerate(prs):
                            prl = pr % 4
                            for c in range(2):
                                H = HB[par][ci][c]
                                Hh = HD[par][ci][c]
                                H0 = HB[par][0][c]
                                ah = AH0 if c == 0 else AH0I
                                hsn = HSN if c == 0 else HSNI
                                hf = HF if c == 0 else HFI
                                V(lambda e: e.tensor_tensor(out=H[:, 1 + LO:1 + NT], in0=H0[:, 1 + LO:1 + NT], in1=ah[:, :, pr], op=ALU.add),
                                  r=[H0, ah], w=[H])
                                A(lambda e: e.activation(out=hsn[:, :, pr], in_=H[:, 1 + LO:1 + NT], func=AF.Copy), r=[H], w=[hsn])
                                A(lambda e: e.activation(out=hf[:, pr:pr + 1], in_=H[:, LO:LO + 1], func=AF.Copy), r=[H], w=[hf])
                                cm = CTm[par][c]
                                for pi, (t0, tn) in enumerate(pieces):
                                    P(lambda e: e.matmul(ybank[pi][:, :tn], lhsT=cm[:], rhs=H[:, 1 + t0:1 + t0 + tn],
                                                         start=(prl == 0 and c == 0), stop=(prl == 3 and c == 1)), r=[cm, H, Hh], w=[ybank[pi]],
                                      acc=not (prl == 0 and c == 0))
                        if pp % 2 == 1:
                            z_ = zst[j % 2]
                            for pi, (t0, tn) in enumerate(pieces):
                                V(lambda e: e.scalar_tensor_tensor(out=ysb[:, t0:t0 + tn], in0=uT[:, t0:t0 + tn], scalar=cols[:, j:j + 1],
                                                                   in1=ybank[pi][:, :tn], op0=ALU.mult, op1=ALU.add),
                                  r=[uT, cols, ybank[pi]], w=[ysb])
                            A(lambda e: e.activation(out=tg[:], in_=ysb[:], func=AF.Square), r=[ysb], w=[tg])
                            V(lambda e: e.tensor_scalar(out=tg[:], in0=tg[:], scalar1=0.044715, scalar2=1.0, op0=ALU.mult, op1=ALU.add), r=[tg], w=[tg])
                            V(lambda e: e.tensor_tensor(out=tg[:], in0=tg[:], in1=ysb[:], op=ALU.mult), r=[tg, ysb], w=[tg])
                            A(lambda e: e.activation(out=tg[:], in_=tg[:], func=AF.Sigmoid, scale=1.5957691216057308), r=[tg], w=[tg])
                            V(lambda e: e.tensor_tensor(out=z_[:, :], in0=ysb[:], in1=tg[:], op=ALU.mult), r=[tg, ysb], w=[z_])
                            dma("sp", z_scr[:, j, :], z_[:, :], r=[z_], w=[B_z])
                    else:
                        s0 = 0
                        for k in range(9, -1, -1):
                            half = 1 << k
                            lo = slice(s0, s0 + half)
                            hi = slice(s0 + half, s0 + 2 * half)
                            for stage in range(2):
                                for par, pr in enumerate(prs):
                                    R, I = HB[par][0]
                                    if stage == 0:
                                        V(lambda e: e.scalar_tensor_tensor(out=R[:, hi], in0=R[:, lo], scalar=PRP[:, k, pr:pr + 1], in1=R[:, hi],
                                                                           op0=ALU.mult, op1=ALU.add), r=[R, PRP], w=[R])
                                        V(lambda e: e.scalar_tensor_tensor(out=I[:, hi], in0=I[:, lo], scalar=PRP[:, k, pr:pr + 1], in1=I[:, hi],
                                                                           op0=ALU.mult, op1=ALU.add), r=[I, PRP], w=[I])
                                    else:
                                        V(lambda e: e.scalar_tensor_tensor(out=R[:, hi], in0=I[:, lo], scalar=NPIP[:, k, pr:pr + 1], in1=R[:, hi],
                                                                           op0=ALU.mult, op1=ALU.add), r=[I, NPIP, R], w=[R])
                                        V(lambda e: e.scalar_tensor_tensor(out=I[:, hi], in0=R[:, lo], scalar=PIP[:, k, pr:pr + 1], in1=I[:, hi],
                                                                           op0=ALU.mult, op1=ALU.add), r=[R, PIP, I], w=[I])
                            s0 += half
                        for par, pr in enumerate(prs):
                            R, I = HB[par][0]
                            A(lambda e: e.activation(out=HIN[:, pr:pr + 1], in_=R[:, NPF - 1:NPF], func=AF.Copy), r=[R], w=[HIN])
                            A(lambda e: e.activation(out=HINI[:, pr:pr + 1], in_=I[:, NPF - 1:NPF], func=AF.Copy), r=[I], w=[HINI])
                kb.rot = list(range(8))

        def s5_glu_norm():
            with kb.scope():
                zT = kb.sb([128, 16, NT], BF16)
                zg = kb.sb([128, 16, NT], F32)
                dma("sp", zT[:, :, :], z_scr[:, :, :], r=[B_z], w=[zT])
                sg = [kb.sb([128, 352], F32) for _ in range(2)]
                sq = [kb.sb([128, 352], F32) for _ in range(2)]
                rstd = kb.sb([128, NT], F32)
                ctr = [0]

                def cons(ci, cw, t0, tn, bk):
                    s_ = sg[ctr[0] % 2]
                    ctr[0] += 1
                    A(lambda e: e.activation(out=s_[:, :tn], in_=bk[:, :tn], func=AF.Sigmoid, bias=cols[:, 16 + ci:17 + ci], scale=1.0),
                      r=[bk, cols], w=[s_])
                    V(lambda e: e.tensor_tensor(out=zg[:, ci, t0:t0 + tn], in0=zT[:, ci, t0:t0 + tn], in1=s_[:, :tn], op=ALU.mult),
                      r=[zT, s_], w=[zg])
                with wslots(2):
                    projF(zT, 16, OWN_PIECES, w_glu, 0, 2048, cons)
                nb = [banks[5], banks[6], banks[7]]
                kb.rot = [0, 1, 2, 3, 4]
                for ci in range(16):
                    for pi, (t0, tn) in enumerate(OWN_PIECES):
                        s_ = sq[(ci * 3 + pi) % 2]
                        A(lambda e: e.activation(out=s_[:, :tn], in_=zg[:, ci, t0:t0 + tn], func=AF.Square), r=[zg], w=[s_])
                        P(lambda e: e.matmul(nb[pi][:, :tn], lhsT=ones, rhs=s_[:, :tn], start=(ci == 0), stop=(ci == 15)),
                          r=[s_, cst], w=[nb[pi]], acc=ci > 0)
                for pi, (t0, tn) in enumerate(OWN_PIECES):
                    V(lambda e: e.tensor_scalar(out=rstd[:, t0:t0 + tn], in0=nb[pi][:, :tn], scalar1=1.0 / 2048, scalar2=EPS, op0=ALU.mult, op1=ALU.add),
                      r=[nb[pi]], w=[rstd])
                A(lambda e: e.activation(out=rstd[:], in_=rstd[:], func=AF.Sqrt), r=[rstd], w=[rstd])
                V(lambda e: e.reciprocal(out=rstd[:], in_=rstd[:]), r=[rstd], w=[rstd])
                for ci in range(16):
                    V(lambda e: e.scalar_tensor_tensor(out=zT[:, ci, :], in0=zg[:, ci, :], scalar=cols[:, 32 + ci:33 + ci], in1=rstd[:],
                                                       op0=ALU.mult, op1=ALU.mult), r=[zg, cols, rstd], w=[zT])
                dma("sp", z_scr[:, :, :], zT[:, :, :], r=[zT], w=[B_z])
                kb.rot = list(range(8))

        def gla_proj(X, n, pieces, tiles, chunks, own, kT, qT, EB, ebs, vdst, Bv):
            with kb.scope():
                xnT = kb.sb([128, 32, n], BF16)
                normA(X, None, tiles, norm_mix_g, xnT)
                with wslots(2):
                    xgT = kb.sb([16, n], F32)
                    wg2 = kb.sb([16, 1024], F32)
                    e1 = [kb.sb([128, 512], F32) for _ in range(2)]
                    tmp = kb.sb([128, 128], F32)
                    btmp = kb.sb([128, 2, n], F32)
                    vst = [kb.sb([128, 256], BF16) for _ in range(2)]
                    ctr = [0]
                    dma("sp", wg2[:], w_gate2[:, :], w=[wg2])

                    def cons_g(ci, cw, t0, tn, bk):
                        A(lambda e: e.activation(out=xgT[:16, t0:t0 + tn], in_=bk[:16, :tn], func=AF.Copy), r=[bk], w=[xgT])
                    projF(xnT, 32, pieces, w_in, 8192, 16, cons_g)
                    for cp in range(4):
                        for cl in range(2):
                            c8 = 2 * cp + cl
                            for (t0, tn) in pieces:
                                bk = nextbank()
                                P(lambda e: e.matmul(bk[:, :tn], lhsT=wg2[:16, c8 * 128:(c8 + 1) * 128], rhs=xgT[:16, t0:t0 + tn], start=True, stop=True),
                                  r=[wg2, xgT], w=[bk])
                                e_ = e1[ctr[0] % 2]
                                ctr[0] += 1
                                A(lambda e: e.activation(out=e_[:, :tn], in_=bk[:, :tn], func=AF.Exp, scale=-1.0, bias=cols[:, 48 + c8:49 + c8]),
                                  r=[bk, cols], w=[e_])
                                A(lambda e: e.activation(out=btmp[:, cl, t0:t0 + tn], in_=e_[:, :tn], func=AF.Ln, bias=1.0, scale=1.0), r=[e_], w=[btmp])
                            for ci_, (a0, C) in enumerate(chunks):
                                V(lambda e: e.tensor_tensor_scan(out=tmp[:, :C], data0=ones[:, :C], data1=btmp[:, cl, a0:a0 + C], initial=0.0,
                                                                 op0=ALU.mult, op1=ALU.add), r=[btmp, cst], w=[tmp])
                                V(lambda e: e.tensor_scalar(out=btmp[:, cl, a0:a0 + C], in0=tmp[:, :C], scalar1=-1.0 / 16, scalar2=None, op0=ALU.mult),
                                  r=[tmp], w=[btmp])
                                A(lambda e: e.activation(out=EB[:, c8, ci_:ci_ + 1], in_=btmp[:, cl, a0 + C - 1:a0 + C], func=AF.Exp), r=[btmp], w=[EB])
                            if own:
                                V(lambda e: e.tensor_scalar(out=btmp[:, cl, LO:NT], in0=btmp[:, cl, LO:NT], scalar1=-1.0 / 16, scalar2=None, op0=ALU.mult),
                                  r=[btmp], w=[btmp])
                                A(lambda e: e.activation(out=ebs[:, c8, :], in_=btmp[:, cl, LO:NT], func=AF.Exp), r=[btmp], w=[ebs])

                        def cons_k(ci, cw, t0, tn, bk):
                            e_ = e1[ctr[0] % 2]
                            ctr[0] += 1
                            A(lambda e: e.activation(out=e_[:, :tn], in_=btmp[:, ci, t0:t0 + tn], func=AF.Exp, scale=-1.0), r=[btmp], w=[e_])
                            V(lambda e: e.tensor_tensor(out=kT[:, 2 * cp + ci, t0:t0 + tn], in0=bk[:, :tn], in1=e_[:, :tn], op=ALU.mult), r=[bk, e_], w=[kT])
                        projF(xnT, 32, pieces, w_in, 3072 + 256 * cp, 256, cons_k)

                        def cons_q(ci, cw, t0, tn, bk):
                            e_ = e1[ctr[0] % 2]
                            ctr[0] += 1
                            A(lambda e: e.activation(out=e_[:, :tn], in_=btmp[:, ci, t0:t0 + tn], func=AF.Exp, scale=1.0), r=[btmp], w=[e_])
                            V(lambda e: e.scalar_tensor_tensor(out=qT[:, 2 * cp + ci, t0:t0 + tn], in0=bk[:, :tn], scalar=1.0 / 16, in1=e_[:, :tn],
                                                               op0=ALU.mult, op1=ALU.mult), r=[bk, e_], w=[qT])
                        if own:
                            projF(xnT, 32, pieces, w_in, 2048 + 256 * cp, 256, cons_q)

                    def cons_v(cb, cw, t0, m, bk):
                        v_ = vst[ctr[0] % 2]
                        ctr[0] += 1
                        A(lambda e: e.activation(out=v_[:m, :cw], in_=bk[:m, :cw], func=AF.Copy), r=[bk], w=[v_])
                        dma("sp", vdst[t0:t0 + m, cb:cb + cw], v_[:m, :cw], r=[v_], w=[Bv])
                    projT(xnT, 32, tiles, w_in, 4096, 2048, cons_v)

                    def cons_r(cb, cw, t0, m, bk):
                        v_ = vst[ctr[0] % 2]
                        ctr[0] += 1
                        A(lambda e: e.activation(out=v_[:m, :cw], in_=bk[:m, :cw], func=AF.Silu), r=[bk], w=[v_])
                        dma("sp", sr_scr[t0:t0 + m, cb:cb + cw], v_[:m, :cw], r=[v_], w=[B_sr])
                    if own:
                        projT(xnT, 32, tiles, w_in, 6144, 2048, cons_r)

        ep = {}

        def gla_epilogue(C, pso, h, srt, gng, ofin, st):
            jk = ep["jk"]
            tmpo = ep["tmpo"]
            V(lambda e: e.memset(st[:C, :], 0.0), w=[st])
            A(lambda e: e.activation(out=jk[:C, :], in_=pso[:C, :], func=AF.Square, accum_out=st[:C, 0:1]), r=[pso], w=[jk, st])
            V(lambda e: e.tensor_scalar(out=st[:C, 1:2], in0=st[:C, 0:1], scalar1=1.0 / 512, scalar2=EPS, op0=ALU.mult, op1=ALU.add), r=[st], w=[st])
            A(lambda e: e.activation(out=st[:C, 2:3], in_=st[:C, 1:2], func=AF.Sqrt), r=[st], w=[st])
            V(lambda e: e.reciprocal(out=st[:C, 3:4], in_=st[:C, 2:3]), r=[st], w=[st])
            V(lambda e: e.scalar_tensor_tensor(out=tmpo[:C, :], in0=pso[:C, :], scalar=st[:C, 3:4], in1=gng[:C, h * 512:(h + 1) * 512],
                                               op0=ALU.mult, op1=ALU.mult), r=[pso, st, gng], w=[tmpo])
            V(lambda e: e.tensor_tensor(out=ofin[:C, h * 512:(h + 1) * 512], in0=tmpo[:C, :], in1=srt[:C, h * 512:(h + 1) * 512], op=ALU.mult),
              r=[tmpo, srt], w=[ofin])

        def ofin_to_oT(ofin, C, t0, oT):
            for q in range(4):
                bk = nextbank()
                for k4 in range(4):
                    kc = 4 * q + k4
                    P(lambda e: e.matmul(bk[:, k4 * 128:k4 * 128 + C], lhsT=ofin[:C, kc * 128:(kc + 1) * 128], rhs=identb[:C, :C],
                                         start=True, stop=True), r=[ofin, identb], w=[bk], acc=k4 > 0, inc=(k4 == 3))
                src = bk[:, :].rearrange("p (k c) -> p k c", c=128)[:, :, :C]
                A(lambda e: e.activation(out=oT[:, 4 * q:4 * q + 4, t0:t0 + C], in_=src, func=AF.Copy), r=[bk], w=[oT])

        def gla_run(chunks, own, kT, qT, EB, vsrc, Bv, oT, gng):
            S = Sh[0]
            with kb.scope():
                Sb = kb.sb([128, 8, 512], BF16)
                V(lambda e: e.tensor_copy(out=Sb[:], in_=S[:]), r=[S], w=[Sb])
                vt = [kb.sb([128, 2048], BF16) for _ in range(2)]
                srt = [kb.sb([128, 2048], BF16) for _ in range(2)]
                ktok = kb.sb([128, 8, 128], BF16)
                scm = kb.sb([128, 128], BF16)
                ofin = kb.sb([128, 2048], BF16)
                tmpS = kb.sb([128, 512], F32)
                st = kb.sb([128, 4], F32)
                ep["jk"] = kb.sb([128, 512], BF16)
                ep["tmpo"] = kb.sb([128, 512], F32)
                for ci_, (t0, C) in enumerate(chunks):
                    v_ = vt[ci_ % 2]
                    s_ = srt[ci_ % 2]
                    dma("sp", v_[:C, :], vsrc[t0:t0 + C, :], r=[Bv], w=[v_])
                    if own:
                        dma("sp", s_[:C, :], sr_scr[t0:t0 + C, :], r=[B_sr], w=[s_])
                    for half in range(2):
                        bk = nextbank()
                        for k4 in range(4):
                            c8 = half * 4 + k4
                            P(lambda e: e.matmul(bk[:C, k4 * 128:(k4 + 1) * 128], lhsT=kT[:, c8, t0:t0 + C], rhs=identb[:, :], start=True, stop=True),
                              r=[kT, identb], w=[bk], acc=k4 > 0, inc=(k4 == 3))
                        A(lambda e: e.activation(out=ktok[:C, half * 4:half * 4 + 4, :], in_=bk[:C, :].rearrange("p (k c) -> p k c", c=128),
                                                 func=AF.Copy), r=[bk], w=[ktok])
                    for h in range(4):
                        if own:
                            pss = nextbank()
                            for kc in range(2):
                                P(lambda e: e.matmul(pss[:C, :C], lhsT=kT[:, 2 * h + kc, t0:t0 + C], rhs=qT[:, 2 * h + kc, t0:t0 + C],
                                                     start=(kc == 0), stop=(kc == 1)), r=[kT, qT], w=[pss], acc=kc > 0, inc=(kc == 1))
                            V(lambda e: e.tensor_tensor(out=scm[:C, :C], in0=pss[:C, :C], in1=cst.t[:C, CO_MT:CO_MT + C], op=ALU.mult),
                              r=[pss, cst], w=[scm])
                            pso = nextbank()
                            for kc in range(2):
                                P(lambda e: e.matmul(pso[:C, :], lhsT=qT[:, 2 * h + kc, t0:t0 + C], rhs=Sb[:, 2 * h + kc, :],
                                                     start=(kc == 0), stop=False), r=[qT, Sb], w=[pso], acc=kc > 0, inc=False)
                            P(lambda e: e.matmul(pso[:C, :], lhsT=scm[:C, :C], rhs=v_[:C, h * 512:(h + 1) * 512], start=False, stop=True),
                              r=[scm, v_], w=[pso], acc=True)
                            gla_epilogue(C, pso, h, s_, gng, ofin, st)
                        for kc in range(2):
                            c8 = 2 * h + kc
                            psk = nextbank()
                            P(lambda e: e.matmul(psk[:, :], lhsT=ktok[:C, c8, :], rhs=v_[:C, h * 512:(h + 1) * 512], start=True, stop=True),
                              r=[ktok, v_], w=[psk])
                            V(lambda e: e.tensor_tensor(out=tmpS[:, :], in0=psk[:, :], in1=S[:, c8, :], op=ALU.add), r=[psk, S], w=[tmpS])
                            V(lambda e: e.tensor_scalar(out=S[:, c8, :], in0=tmpS[:, :], scalar1=EB[:, c8, ci_:ci_ + 1], scalar2=None, op0=ALU.mult),
                              r=[tmpS, EB], w=[S])
                            A(lambda e: e.activation(out=Sb[:, c8, :], in_=tmpS[:, :], func=AF.Copy, scale=EB[:, c8, ci_:ci_ + 1]), r=[tmpS, EB], w=[Sb])
                    if own:
                        ofin_to_oT(ofin, C, t0, oT)

        def gla_samples(kT, qT, ebs, oT, gng):
            with kb.scope():
                v16 = kb.sb([16, 2048], BF16)
                sr16 = kb.sb([16, 2048], BF16)
                vm = [kb.sb([16, 2048], BF16) for _ in range(2)]
                ktok = kb.sb([16, 8, 128], BF16)
                qm = [kb.sb([128, 16], BF16) for _ in range(2)]
                S0 = [kb.sb([128, 512], F32) for _ in range(3)]
                tmpS = [kb.sb([128, 512], F32) for _ in range(2)]
                tmpb = [kb.sb([128, 512], BF16) for _ in range(2)]
                Sn = [kb.sb([128, 512], F32) for _ in range(3)]
                ofin = kb.sb([16, 2048], BF16)
                st = kb.sb([128, 4], F32)
                ep["jk"] = kb.sb([128, 512], BF16)
                ep["tmpo"] = kb.sb([128, 512], F32)
                pos = [banks[4], banks[5], banks[6], banks[7]]
                kb.rot = [0, 1, 2, 3]
                dma("sp", v16[:, :], v_scr[LO:NT, :], r=[B_v], w=[v16])
                dma("sp", sr16[:, :], sr_scr[LO:NT, :], r=[B_sr], w=[sr16])
                for half in range(2):
                    bk = nextbank()
                    for k4 in range(4):
                        c8 = half * 4 + k4
                        P(lambda e: e.matmul(bk[:NS, k4 * 128:(k4 + 1) * 128], lhsT=kT[:, c8, LO:NT], rhs=identb[:, :], start=True, stop=True),
                          r=[kT, identb], w=[bk], acc=k4 > 0, inc=(k4 == 3))
                    A(lambda e: e.activation(out=ktok[:, half * 4:half * 4 + 4, :], in_=bk[:NS, :].rearrange("p (k c) -> p k c", c=128),
                                             func=AF.Copy), r=[bk], w=[ktok])
                it = 0
                for i in range(NS):
                    vm_ = vm[i % 2]
                    V(lambda e: e.tensor_scalar(out=vm_[:, :], in0=v16[:, :], scalar1=cst.t[:16, CO_ID + i:CO_ID + i + 1], scalar2=None, op0=ALU.mult),
                      r=[v16, cst], w=[vm_])
                    for h in range(4):
                        for kc in range(2):
                            c8 = 2 * h + kc
                            s0_ = S0[it % 3]
                            sn_ = Sn[it % 3]
                            ts_ = tmpS[it % 2]
                            tb_ = tmpb[it % 2]
                            qm_ = qm[it % 2]
                            it += 1
                            dma("sp", s0_[:, :], sgla[i, h, kc * 128:(kc + 1) * 128, :], w=[s0_])
                            psk = nextbank()
                            P(lambda e: e.matmul(psk[:, :], lhsT=ktok[:, c8, :], rhs=vm_[:, h * 512:(h + 1) * 512], start=True, stop=True),
                              r=[ktok, vm_], w=[psk])
                            V(lambda e: e.tensor_tensor(out=ts_[:, :], in0=psk[:, :], in1=s0_[:, :], op=ALU.add), r=[psk, s0_], w=[ts_])
                            V(lambda e: e.tensor_scalar(out=sn_[:, :], in0=ts_[:, :], scalar1=ebs[:, c8, i:i + 1], scalar2=None, op0=ALU.mult),
                              r=[ts_, ebs], w=[sn_])
                            dma("sp", o_glas[i, c8, :, :], sn_[:, :], r=[sn_])
                            A(lambda e: e.activation(out=tb_[:, :], in_=ts_[:, :], func=AF.Copy), r=[ts_], w=[tb_])
                            V(lambda e: e.tensor_scalar(out=qm_[:, :], in0=iota256[:, 0:16], scalar1=float(i), scalar2=None, op0=ALU.is_equal),
                              r=[cst], w=[qm_])
                            V(lambda e: e.tensor_tensor(out=qm_[:, :], in0=qm_[:, :], in1=qT[:, c8, LO:NT], op=ALU.mult), r=[qm_, qT], w=[qm_])
                            first = (i == 0 and kc == 0)
                            last = (i == NS - 1 and kc == 1)
                            P(lambda e: e.matmul(pos[h][:NS, :], lhsT=qm_[:, :], rhs=tb_[:, :], start=first, stop=last),
                              r=[qm_, tb_], w=[pos[h]], acc=not first)
                for h in range(4):
                    gla_epilogue(NS, pos[h], h, sr16, gng, ofin, st)
                kb.rot = list(range(8))
                ofin_to_oT(ofin, NS, LO, oT)

        PHASES = _PH
        with kb.scope():
            PRV = PIV = None
            BT = kb.sb([128, 16, 128], F32, "BT")
            CT = kb.sb([128, 16, 128], F32, "CT")
            AH0 = kb.sb([128, NS, 64], F32, "AH0")
            HSN = kb.sb([128, NS, 64], F32, "HSN")
            HF = kb.sb([128, 128], F32, "HF")
            PRP = kb.sb([128, 11, 64], F32, "PRP")
            PIP = kb.sb([128, 11, 64], F32, "PIP")
            NPIP = kb.sb([128, 11, 64], F32, "NPIP")
            CTI = kb.sb([128, 16, 128], F32, "CTI")
            AH0I = kb.sb([128, NS, 64], F32, "AH0I")
            HSNI = kb.sb([128, NS, 64], F32, "HSNI")
            HFI = kb.sb([128, 128], F32, "HFI")
            tabs = (PRV, PIV, BT, CT, AH0, HSN, HF, PRP, PIP, NPIP, CTI, AH0I, HSNI, HFI, HINI)
            s5_setup(PRV, PIV, BT, CT, AH0, PRP, PIP, NPIP, CTI, AH0I)
            proj_u(xp, PRE_TILES, PRE_PIECES, NPF, up_scr, B_up)
            s5_run(up_scr, B_up, NPF, PRE_PIECES, False, tabs)
            proj_u(xo, OWN_TILES, OWN_PIECES, NT, u_scr, B_u)
            s5_run(u_scr, B_u, NT, OWN_PIECES, True, tabs)
            with kb.scope():
                so = [kb.sb([64, 128], F32) for _ in range(2)]
                oc = [0]

                def out_state(src_ap, srcT, dst_ap):
                    s_ = so[oc[0] % 2]
                    oc[0] += 1
                    bk = nextbank()
                    P(lambda e: e.matmul(bk[0:64, 0:128], lhsT=src_ap, rhs=ident, start=True, stop=True), r=[srcT, cst], w=[bk])
                    A(lambda e: e.activation(out=s_[:, :], in_=bk[0:64, 0:128], func=AF.Copy), r=[bk], w=[s_])
                    dma("sp", dst_ap, s_[:, :], r=[s_])
                out_state(HF[:, 0:64], HF, o_s5[0, :, :])
                out_state(HFI[:, 0:64], HFI, o_s5[1, :, :])
                for i in range(NS):
                    out_state(HSN[:, i, :], HSN, o_s5s[i, 0, :, :])
                    out_state(HSNI[:, i, :], HSNI, o_s5s[i, 1, :, :])
        s5_glu_norm()

        if PHASES >= 2:
          with kb.scope():
            Sh[0] = kb.sb([128, 8, 512], F32, "S")
            G(lambda e: e.memset(Sh[0][:], 0.0), w=[Sh[0]])
            with kb.scope():
                kT = kb.sb([128, 8, NPF], BF16, "kTp")
                EB = kb.sb([128, 8, 16], F32, "EBp")
                gla_proj(xp, NPF, PRE_PIECES, PRE_TILES, PRE_CHUNKS, False, kT, None, EB, None, vp_scr, B_vp)
                gla_run(PRE_CHUNKS, False, kT, None, EB, vp_scr, B_vp, None, None)
            with kb.scope():
                kT = kb.sb([128, 8, NT], BF16, "kTo")
                qT = kb.sb([128, 8, NT], BF16, "qTo")
                EB = kb.sb([128, 8, 16], F32, "EBo")
                ebs = kb.sb([128, 8, NS], F32, "ebs")
                gla_proj(xo, NT, OWN_PIECES, OWN_TILES, OWN_CHUNKS, True, kT, qT, EB, ebs, v_scr, B_v)
                oT = kb.sb([128, 16, NT], BF16, "oT")
                with kb.scope():
                    gng = bcast_gain(gla_norm_g, 2048)
                    gla_run(OWN_CHUNKS, True, kT, qT, EB, v_scr, B_v, oT, gng)
                    for c8 in range(8):
                        dma("sp", o_gla[c8, :, :], Sh[0][:, c8, :], r=[Sh[0]])
                    gla_samples(kT, qT, ebs, oT, gng)
                with wslots(2):
                    zT2 = kb.sb([128, 16, NT], BF16, "zT2")
                    dma("sp", zT2[:, :, :], z_scr[:, :, :], r=[B_z], w=[zT2])
                    xr = [kb.sb([128, 256], F32) for _ in range(3)]
                    ho = [kb.sb([128, 256], F32) for _ in range(3)]
                    it = 0
                    for b0 in range(0, D, 256):
                        W = loadW(w_out, 32, b0, 256)
                        for (t0, m) in OWN_TILES:
                            x_ = xr[it % 3]
                            h_ = ho[it % 3]
                            it += 1
                            dma("sp", x_[:m, :], xo[t0:t0 + m, b0:b0 + 256], w=[x_])
                            bk = nextbank()
                            for kc in range(32):
                                src = zT2 if kc < 16 else oT
                                P(lambda e: e.matmul(bk[:m, :256], lhsT=src[:, kc % 16, t0:t0 + m], rhs=W[:, kc, :256], start=(kc == 0), stop=(kc == 31)),
                                  r=[W, src], w=[bk], acc=kc > 0, inc=(kc == 31))
                            V(lambda e: e.tensor_tensor(out=h_[:m, :], in0=bk[:m, :256], in1=x_[:m, :], op=ALU.add), r=[bk, x_], w=[h_])
                            dma("sp", h_scr[t0:t0 + m, b0:b0 + 256], h_[:m, :], r=[h_], w=[B_h])

        if PHASES >= 3:
          conv_step(1000)
          with kb.scope():
            TV = kb.sb([128, 9, 16, 16], F32, "TV")
            TI = kb.sb([128, 9, 16, 16], F32, "TI")
            with kb.scope():
                xnT = kb.sb([128, 32, NT], BF16, "xnT2")
                normA(h_scr, B_h, OWN_TILES, norm_ffn_g, xnT)
                keysT = kb.sb([128, 16, 128], F32, "keysT")
                kn = [kb.sb([128, 128], F32) for _ in range(2)]
                for c16 in range(16):
                    k_ = kn[c16 % 2]
                    dma("sp", k_[:, :], keys[c16, :, :], w=[k_])
                    transp32(keysT[:, c16, :], keysT, k_[:], k_)
                qTt = kb.sb([128, NT], F32, "qTt")
                sc = [kb.sb([128, 128], F32) for _ in range(2)]
                sc2 = [kb.sb([128, 128], F32) for _ in range(2)]
                m8 = [kb.sb([128, 8], F32) for _ in range(2)]
                i8 = [kb.sb([128, 8], U32) for _ in range(2)]
                ctr = [0]

                def cons_qp(ci, cw, t0, tn, bk):
                    A(lambda e: e.activation(out=qTt[:, t0:t0 + tn], in_=bk[:, :tn], func=AF.Copy), r=[bk], w=[qTt])
                with wslots(2):
                  for c16 in range(16):
                    projF(xnT, 32, OWN_PIECES, w_q, c16 * 128, 128, cons_qp)
                    for ti, (t0, m) in enumerate(OWN_TILES):
                        bk = nextbank()
                        P(lambda e: e.matmul(bk[:m, :128], lhsT=qTt[:, t0:t0 + m], rhs=keysT[:, c16, :], start=True, stop=True),
                          r=[qTt, keysT], w=[bk])
                        s_ = sc[ctr[0] % 2]
                        s2 = sc2[ctr[0] % 2]
                        ma = m8[ctr[0] % 2]
                        ia = i8[ctr[0] % 2]
                        ctr[0] += 1
                        A(lambda e: e.activation(out=s_[:m, :], in_=bk[:m, :128], func=AF.Copy), r=[bk], w=[s_])
                        V(lambda e: e.max(out=ma[:m, :], in_=s_[:m, :]), r=[s_], w=[ma])
                        V(lambda e: e.max_index(out=ia[:m, :], in_max=ma[:m, :], in_values=s_[:m, :]), r=[ma, s_], w=[ia])
                        V(lambda e: e.tensor_copy(out=TV[:m, ti, c16, 0:8], in_=ma[:m, :]), r=[ma], w=[TV])
                        V(lambda e: e.tensor_copy(out=TI[:m, ti, c16, 0:8], in_=ia[:m, :]), r=[ia], w=[TI])
                        V(lambda e: e.match_replace(out=s2[:m, :], in_to_replace=ma[:m, :], in_values=s_[:m, :], imm_value=-1e30), r=[ma, s_], w=[s2])
                        V(lambda e: e.max(out=ma[:m, :], in_=s2[:m, :]), r=[s2], w=[ma])
                        V(lambda e: e.max_index(out=ia[:m, :], in_max=ma[:m, :], in_values=s2[:m, :]), r=[ma, s2], w=[ia])
                        V(lambda e: e.tensor_copy(out=TV[:m, ti, c16, 8:16], in_=ma[:m, :]), r=[ma], w=[TV])
                        V(lambda e: e.tensor_copy(out=TI[:m, ti, c16, 8:16], in_=ia[:m, :]), r=[ia], w=[TI])
            with kb.scope():
                gt_ffn = bcast_gain(norm_ffn_g, D)
                gt_fin = bcast_gain(norm_final_g, D)
                hxs = [kb.sb([128, D], F32, "hx%d" % i) for i in range(2)]
                xnbs = [kb.sb([128, D], F32, "xnb%d" % i) for i in range(2)]
                jk = kb.sb([128, D], BF16, "jkp")
                NR = 6
                rows = [kb.sb([128, D], BF16, "rows%d" % i) for i in range(NR)]
                dg = [kb.sb([128, 128], BF16, "dg%d" % i) for i in range(2)]
                sts = [kb.sb([128, 8], F32) for _ in range(2)]
                cand = kb.sb([128, 16, 16], F32)
                cand2 = kb.sb([128, 256], F32)
                cid = kb.sb([128, 16, 16], F32)
                ma = kb.sb([128, 8], F32)
                ia = kb.sb([128, 8], U32)
                sc16 = kb.sb([128, 8, 16], F32)
                sel = kb.sb([128, 16], F32)
                saf = kb.sb([128, 16], F32)
                sbf = kb.sb([128, 16], F32)
                sai = kb.sb([128, 16], I32)
                i1s = kb.sb([128, 16], F32)
                i2s = kb.sb([128, 16], F32)
                eqs = cid
                eq = kb.sb([128, 16, 256], F32)
                eidfs = [kb.sb([128, 128], F32) for _ in range(2)]
                eidis = [kb.sb([128, 128], I32) for _ in range(2)]
                gws = [kb.sb([128, 128], F32) for _ in range(2)]
                actvs = [kb.sb([128, 128], F32) for _ in range(2)]
                tg = kb.sb([128, 128], F32)
                coefv = [kb.sb([128, 128], F32) for _ in range(2)]
                sm = kb.sb([128, 8, 4], F32)
                ric = [0]

                def prep(ti):
                    t0, m = OWN_TILES[ti]
                    hx, xnb, st = hxs[ti % 2], xnbs[ti % 2], sts[ti % 2]
                    eidf, eidi, gw = eidfs[ti % 2], eidis[ti % 2], gws[ti % 2]
                    dma("sp", hx[:m, :], h_scr[t0:t0 + m, :], r=[B_h], w=[hx])
                    V(lambda e: e.memset(st[:m, :], 0.0), w=[st])
                    A(lambda e: e.activation(out=jk[:m, :], in_=hx[:m, :], func=AF.Square, accum_out=st[:m, 0:1]), r=[hx], w=[jk, st])
                    V(lambda e: e.tensor_scalar(out=st[:m, 1:2], in0=st[:m, 0:1], scalar1=1.0 / D, scalar2=EPS, op0=ALU.mult, op1=ALU.add), r=[st], w=[st])
                    A(lambda e: e.activation(out=st[:m, 2:3], in_=st[:m, 1:2], func=AF.Sqrt), r=[st], w=[st])
                    V(lambda e: e.reciprocal(out=st[:m, 3:4], in_=st[:m, 2:3]), r=[st], w=[st])
                    V(lambda e: e.scalar_tensor_tensor(out=xnb[:m, :], in0=hx[:m, :], scalar=st[:m, 3:4], in1=gt_ffn[:m, :], op0=ALU.mult, op1=ALU.mult),
                      r=[hx, st, gt_ffn], w=[xnb])
                    for h in range(8):
                        v1 = TV[:m, ti, 2 * h, :]
                        v2 = TV[:m, ti, 2 * h + 1, :]
                        i1 = TI[:m, ti, 2 * h, :]
                        i2 = TI[:m, ti, 2 * h + 1, :]
                        V(lambda e: e.tensor_tensor(out=cand[:m], in0=v1.unsqueeze(2).to_broadcast([m, 16, 16]),
                                                    in1=v2.unsqueeze(1).to_broadcast([m, 16, 16]), op=ALU.add), r=[TV], w=[cand])
                        cflat = cand.t[:m].rearrange("p a b -> p (a b)")
                        for rnd in range(2):
                            srcc = cflat if rnd == 0 else cand2[:m, :]
                            V(lambda e: e.max(out=ma[:m, :], in_=srcc), r=[cand, cand2], w=[ma])
                            V(lambda e: e.max_index(out=ia[:m, :], in_max=ma[:m, :], in_values=srcc), r=[ma, cand, cand2], w=[ia])
                            V(lambda e: e.tensor_copy(out=sc16[:m, h, 8 * rnd:8 * rnd + 8], in_=ma[:m, :]), r=[ma], w=[sc16])
                            V(lambda e: e.tensor_copy(out=sel[:m, 8 * rnd:8 * rnd + 8], in_=ia[:m, :]), r=[ia], w=[sel])
                            if rnd == 0:
                                V(lambda e: e.match_replace(out=cand2[:m, :], in_to_replace=ma[:m, :], in_values=cflat, imm_value=-1e30),
                                  r=[ma, cand], w=[cand2])
                        V(lambda e: e.tensor_scalar(out=saf[:m, :], in0=sel[:m, :], scalar1=-7.5, scalar2=0.0625, op0=ALU.add, op1=ALU.mult), r=[sel], w=[saf])
                        V(lambda e: e.tensor_copy(out=sai[:m, :], in_=saf[:m, :]), r=[saf], w=[sai])
                        V(lambda e: e.tensor_copy(out=saf[:m, :], in_=sai[:m, :]), r=[sai], w=[saf])
                        V(lambda e: e.scalar_tensor_tensor(out=sbf[:m, :], in0=saf[:m, :], scalar=-16.0, in1=sel[:m, :], op0=ALU.mult, op1=ALU.add),
                          r=[saf, sel], w=[sbf])
                        for (idxv, tabv, dstv) in ((saf, i1, i1s), (sbf, i2, i2s)):
                            V(lambda e: e.tensor_tensor(out=eqs[:m], in0=iota256[:m, 0:16].unsqueeze(1).to_broadcast([m, 16, 16]),
                                                        in1=idxv[:m, :].unsqueeze(2).to_broadcast([m, 16, 16]), op=ALU.is_equal), r=[cst, idxv], w=[eqs])
                            V(lambda e: e.tensor_tensor(out=eqs[:m], in0=eqs[:m], in1=tabv.unsqueeze(1).to_broadcast([m, 16, 16]), op=ALU.mult),
                              r=[eqs, TI], w=[eqs])
                            V(lambda e: e.reduce_sum(out=dstv[:m, :], in_=eqs[:m], axis=AX.X), r=[eqs], w=[dstv])
                        V(lambda e: e.scalar_tensor_tensor(out=eidf[:m, 16 * h:16 * h + 16], in0=i1s[:m, :], scalar=128.0, in1=i2s[:m, :],
                                                           op0=ALU.mult, op1=ALU.add), r=[i1s, i2s], w=[eidf])
                        V(lambda e: e.tensor_scalar(out=sm[:m, h, 0:1], in0=sc16[:m, h, 0:1], scalar1=-1.0, scalar2=None, op0=ALU.mult), r=[sc16], w=[sm])
                        V(lambda e: e.memset(sm[:m, h, 1:2], 0.0), w=[sm])
                        A(lambda e: e.activation(out=gw[:m, 16 * h:16 * h + 16], in_=sc16[:m, h, :], func=AF.Exp, bias=sm[:m, h, 0:1], scale=1.0,
                                                 accum_out=sm[:m, h, 1:2]), r=[sc16, sm], w=[gw, sm])
                        V(lambda e: e.reciprocal(out=sm[:m, h, 2:3], in_=sm[:m, h, 1:2]), r=[sm], w=[sm])
                        V(lambda e: e.tensor_scalar(out=gw[:m, 16 * h:16 * h + 16], in0=gw[:m, 16 * h:16 * h + 16], scalar1=sm[:m, h, 2:3], scalar2=None,
                                                    op0=ALU.mult), r=[gw, sm], w=[gw])
                    V(lambda e: e.tensor_copy(out=eidi[:m, :], in_=eidf[:m, :]), r=[eidf], w=[eidi])
                    V(lambda e: e.memset(actvs[ti % 2][:m, :], 0.0), w=[actvs[ti % 2]])

                def u_slot(ti, sl):
                    t0, m = OWN_TILES[ti]
                    xnb, eidi, actv = xnbs[ti % 2], eidis[ti % 2], actvs[ti % 2]
                    r_ = rows[ric[0] % NR]
                    ric[0] += 1
                    dma("pool", r_[:m, :], ub_scr[:, :], r=[eidi, B_ub], w=[r_],
                        indirect=bass.IndirectOffsetOnAxis(ap=eidi[:m, sl:sl + 1], axis=0))
                    V(lambda e: e.scalar_tensor_tensor(out=jk[:m, :], in0=r_[:m, :], scalar=1.0, in1=xnb[:m, :], op0=ALU.mult, op1=ALU.mult,
                                                       accum_out=actv[:m, sl:sl + 1]), r=[r_, xnb], w=[jk, actv])

                def coefs(ti):
                    t0, m = OWN_TILES[ti]
                    actv, coef, gw = actvs[ti % 2], coefv[ti % 2], gws[ti % 2]
                    A(lambda e: e.activation(out=tg[:m, :], in_=actv[:m, :], func=AF.Square), r=[actv], w=[tg])
                    V(lambda e: e.tensor_scalar(out=tg[:m, :], in0=tg[:m, :], scalar1=0.044715, scalar2=1.0, op0=ALU.mult, op1=ALU.add), r=[tg], w=[tg])
                    V(lambda e: e.tensor_tensor(out=tg[:m, :], in0=tg[:m, :], in1=actv[:m, :], op=ALU.mult), r=[tg, actv], w=[tg])
                    A(lambda e: e.activation(out=tg[:m, :], in_=tg[:m, :], func=AF.Sigmoid, scale=1.5957691216057308), r=[tg], w=[tg])
                    V(lambda e: e.tensor_tensor(out=coef[:m, :], in0=tg[:m, :], in1=actv[:m, :], op=ALU.mult), r=[tg, actv], w=[coef])
                    V(lambda e: e.tensor_tensor(out=coef[:m, :], in0=coef[:m, :], in1=gw[:m, :], op=ALU.mult), r=[coef, gw], w=[coef])

                vcnt = [0]

                def v_slot(ti):
                    t0, m = OWN_TILES[ti]
                    sl = vcnt[0]
                    vcnt[0] += 1
                    eidi, coef = eidis[ti % 2], coefv[ti % 2]
                    r_ = rows[ric[0] % NR]
                    d_ = dg[ric[0] % 2]
                    ric[0] += 1
                    dma("pool", r_[:m, :], vb_scr[:, :], r=[eidi, B_vb], w=[r_],
                        indirect=bass.IndirectOffsetOnAxis(ap=eidi[:m, sl:sl + 1], axis=0))
                    A(lambda e: e.activation(out=d_[:m, :m], in_=identb[:m, :m], func=AF.Copy, scale=coef[:m, sl:sl + 1]), r=[identb, coef], w=[d_])
                    for c8 in range(8):
                        P(lambda e: e.matmul(banks[c8][:m, :], lhsT=d_[:m, :m], rhs=r_[:m, c8 * 512:(c8 + 1) * 512], start=(sl == 0), stop=(sl == 127)),
                          r=[d_, r_], w=[banks[c8]], acc=sl > 0, inc=(c8 == 7))

                def final(ti):
                    t0, m = OWN_TILES[ti]
                    hx, st = hxs[ti % 2], sts[ti % 2]
                    for c8 in range(8):
                        V(lambda e: e.tensor_tensor(out=hx[:m, c8 * 512:(c8 + 1) * 512], in0=hx[:m, c8 * 512:(c8 + 1) * 512], in1=banks[c8][:m, :], op=ALU.add),
                          r=[hx, banks[c8]], w=[hx])
                    V(lambda e: e.memset(st[:m, 4:8], 0.0), w=[st])
                    A(lambda e: e.activation(out=jk[:m, :], in_=hx[:m, :], func=AF.Square, accum_out=st[:m, 4:5]), r=[hx], w=[jk, st])
                    V(lambda e: e.tensor_scalar(out=st[:m, 5:6], in0=st[:m, 4:5], scalar1=1.0 / D, scalar2=EPS, op0=ALU.mult, op1=ALU.add), r=[st], w=[st])
                    A(lambda e: e.activation(out=st[:m, 6:7], in_=st[:m, 5:6], func=AF.Sqrt), r=[st], w=[st])
                    V(lambda e: e.reciprocal(out=st[:m, 7:8], in_=st[:m, 6:7]), r=[st], w=[st])
                    eqf = eq.t[:m].rearrange("p a b -> p (a b)")
                    V(lambda e: e.scalar_tensor_tensor(out=eqf, in0=hx[:m, :], scalar=st[:m, 7:8], in1=gt_fin[:m, :], op0=ALU.mult, op1=ALU.mult),
                      r=[hx, st, gt_fin], w=[eq])
                    dma("sp", yo[t0:t0 + m, :], eqf, r=[eq])

                NTL = len(OWN_TILES)
                prep(0)
                for sl in range(128):
                    u_slot(0, sl)
                coefs(0)
                for ti in range(NTL):
                    vcnt[0] = 0
                    nxt = ti + 1 < NTL
                    if nxt:
                        for _ in range(48):
                            v_slot(ti)
                        prep(ti + 1)
                        for sl in range(128):
                            u_slot(ti + 1, sl)
                            if sl % 8 < 5:
                                v_slot(ti)
                        assert vcnt[0] == 128
                        coefs(ti + 1)
                    else:
                        for _ in range(128):
                            v_slot(ti)
                    final(ti)
        kb.finish()
    return nc


_NC = [None]


def kernel(x_prompt, x_sample, state_s5_re, state_s5_im, state_gla, meta_tokens,
           norm_mix_g, w_in, s5_lam_re, s5_lam_im, s5_log_dt, s5_b_re, s5_b_im,
           s5_c_re, s5_c_im, s5_d, s5_w_glu, s5_b_glu, s5_norm_g,
           gla_w_gate2, gla_b_gate2, gla_norm_g, w_out, norm_ffn_g,
           peer_w_q, peer_keys, peer_u, peer_v, norm_final_g):
    f = lambda a: np.ascontiguousarray(np.asarray(a, dtype=np.float32))
    x_prompt, x_sample, meta_tokens = f(x_prompt), f(x_sample), f(meta_tokens)
    if _NC[0] is None:
        _NC[0] = build()
    nc = _NC[0]
    shared = {
        "cst": _consts(), "norm_mix_g": f(norm_mix_g)[0], "w_in": f(w_in)[0], "lam_re": f(s5_lam_re)[0], "lam_im": f(s5_lam_im)[0],
        "log_dt": f(s5_log_dt)[0], "b_re": f(s5_b_re)[0], "b_im": f(s5_b_im)[0], "c_re": f(s5_c_re)[0], "c_im": f(s5_c_im)[0],
        "s5_d": f(s5_d)[0], "w_glu": f(s5_w_glu)[0], "b_glu": f(s5_b_glu)[0], "s5_norm_g": f(s5_norm_g)[0],
        "w_gate2": f(gla_w_gate2)[0], "b_gate2": f(gla_b_gate2)[0], "gla_norm_g": f(gla_norm_g)[0], "w_out": f(w_out)[0],
        "norm_ffn_g": f(norm_ffn_g)[0], "w_q": f(peer_w_q)[0], "keys": f(peer_keys)[0].reshape(16, 128, 128),
        "peer_u": f(peer_u)[0], "peer_v": f(peer_v)[0], "norm_final_g": f(norm_final_g),
    }
    s5re, s5im, sgl = f(state_s5_re)[0], f(state_s5_im)[0], f(state_gla)[0]
    in_maps = []
    for c in range(8):
        b, half = divmod(c, 2)
        xs = x_sample[16 * c:16 * c + 16, 0, :]
        if half == 0:
            xo = np.concatenate([meta_tokens, x_prompt[b, :1024], xs], axis=0)
            xp = np.zeros((NPF, D), np.float32)
        else:
            xo = np.concatenate([x_prompt[b, 1008:2048], xs], axis=0)
            xp = np.concatenate([meta_tokens, x_prompt[b, :1008]], axis=0)
        m = dict(shared)
        m.update({"xo": np.ascontiguousarray(xo), "xp": np.ascontiguousarray(xp),
                  "s5re": np.ascontiguousarray(s5re[16 * c:16 * c + 16]), "s5im": np.ascontiguousarray(s5im[16 * c:16 * c + 16]),
                  "sgla": np.ascontiguousarray(sgl[16 * c:16 * c + 16])})
        in_maps.append(m)
    res = run_bass_kernel_spmd(nc, in_maps, core_ids=list(range(8)))
    R = res.results
    y_prompt = np.zeros((4, 2048, D), np.float32)
    y_sample = np.zeros((128, 1, D), np.float32)
    p_re = np.zeros((1, 4, 128, 64), np.float32)
    p_im = np.zeros((1, 4, 128, 64), np.float32)
    p_gla = np.zeros((1, 4, 4, 256, 512), np.float32)
    s_re = np.zeros((1, 128, 128, 64), np.float32)
    s_im = np.zeros((1, 128, 128, 64), np.float32)
    s_gla = np.zeros((1, 128, 4, 256, 512), np.float32)
    for c in range(8):
        b, half = divmod(c, 2)
        r = R[c]
        yo = np.asarray(r["yo"])
        y_prompt[b, 1024 * half:1024 * (half + 1)] = yo[16:1040]
        y_sample[16 * c:16 * c + 16, 0] = yo[1040:1056]
        if half == 1:
            o5 = np.asarray(r["o_s5"])
            p_re[0, b] = o5[0].reshape(128, 64)
            p_im[0, b] = o5[1].reshape(128, 64)
            p_gla[0, b] = np.asarray(r["o_gla"]).reshape(4, 256, 512)
        o5s = np.asarray(r["o_s5s"])
        s_re[0, 16 * c:16 * c + 16] = o5s[:, 0].reshape(16, 128, 64)
        s_im[0, 16 * c:16 * c + 16] = o5s[:, 1].reshape(16, 128, 64)
        s_gla[0, 16 * c:16 * c + 16] = np.asarray(r["o_glas"]).reshape(16, 4, 256, 512)
    return (y_prompt, y_sample, p_re, p_im, p_gla, s_re, s_im, s_gla)
```

```python
import contextlib
import numpy as np
import concourse.bass as bass
import concourse.mybir as mybir
from concourse.bass_utils import run_bass_kernel_spmd

F32 = mybir.dt.float32
BF16 = mybir.dt.bfloat16
I32 = mybir.dt.int32
U32 = mybir.dt.uint32
ALU = mybir.AluOpType
AF = mybir.ActivationFunctionType
AX = mybir.AxisListType

D = 4096
DIN = 8208
NT = 1056
LO = 1040
NS = 16
NPF = 1024
EPS = 1e-6
TWO_PI = 6.283185307179586
C1 = 6.28125
C2 = TWO_PI - C1
PI = 3.141592653589793

OWN_TILES = [(i * 128, 128) for i in range(8)] + [(1024, 32)]
PRE_TILES = [(i * 128, 128) for i in range(8)]
OWN_PIECES = [(0, 352), (352, 352), (704, 352)]
PRE_PIECES = [(0, 512), (512, 512)]
OWN_CHUNKS = [(0, 16)] + [(16 + 128 * i, 128) for i in range(8)]
PRE_CHUNKS = [(128 * i, 128) for i in range(8)]

CO_ID, CO_JP, CO_MT, CO_IO, CO_MR, CO_CM, CO_ON = 0, 128, 256, 384, 640, 648, 1672
CO_M2 = 1800
CO_SEL = 1800 + 512
NCST = 1800 + 512 + 128


def _consts():
    c = np.zeros((128, NCST), np.float32)
    p = np.arange(128)
    c[p, CO_ID + p] = 1.0
    c[p, CO_JP + (p + 64) % 128] = 1.0
    c[:, CO_MT:CO_MT + 128] = (p[None, :] >= p[:, None]).astype(np.float32)
    c[:, CO_IO:CO_IO + 256] = np.arange(256, dtype=np.float32)[None, :]
    for gl in range(8):
        c[:, CO_MR + gl] = (p // 16 == gl)
        c[:, CO_CM + gl * 128: CO_CM + (gl + 1) * 128] = (p[None, :] // 16 == gl)
    c[:, CO_ON:CO_ON + 128] = 1.0
    for prl in range(4):
        c[0:64, CO_M2 + prl * 128: CO_M2 + (prl + 1) * 128] = (p[None, :] // 16 == 2 * prl)
        c[64:128, CO_M2 + prl * 128: CO_M2 + (prl + 1) * 128] = (p[None, :] // 16 == 2 * prl + 1)
    pr = np.arange(64)
    c[:, CO_SEL:CO_SEL + 64] = (p[:, None] == 2 * pr[None, :])
    c[:, CO_SEL + 64:CO_SEL + 128] = (p[:, None] == 2 * pr[None, :] + 1)
    return c


class Buf:
    __slots__ = ("w", "r")

    def __init__(self):
        self.w = None
        self.r = {}


class T:
    def __init__(self, t):
        self.t = t
        self.b = Buf()

    def __getitem__(self, k):
        return self.t[k]


class KB:
    def __init__(self, nc, es):
        self.nc = nc
        self.es = [es]
        self.eng = {"pe": nc.tensor, "dve": nc.vector, "act": nc.scalar, "pool": nc.gpsimd, "sp": nc.sync}
        self.sem = {}
        self.cnt = {}
        self.waited = {e: {} for e in self.eng}
        for e in ("pe", "dve", "act", "pool"):
            self.sem[e] = es.enter_context(nc.semaphore("s_" + e))
            self.cnt[e] = 0
        self.dq = {}
        for q, n in (("sp", 8), ("pool", 6), ("act", 2)):
            keys = []
            for i in range(n):
                k = "d_%s%d" % (q, i)
                self.sem[k] = es.enter_context(nc.semaphore(k))
                self.cnt[k] = 0
                keys.append(k)
            self.dq[q] = [keys, 0]
        self.nid = 0
        self.rot = list(range(8))
        self.roti = 0

    def sb(self, shape, dt, name=None):
        self.nid += 1
        return T(self.es[-1].enter_context(self.nc.sbuf_tensor(name or ("t%d" % self.nid), list(shape), dt)))

    @contextlib.contextmanager
    def scope(self):
        es = contextlib.ExitStack()
        self.es.append(es)
        try:
            with es:
                yield
                self.barrier()
        finally:
            self.es.pop()

    def _wait(self, en, key, val):
        if val <= 0 or self.waited[en].get(key, 0) >= val:
            return
        self.eng[en].wait_ge(self.sem[key], val)
        self.waited[en][key] = val

    def _deps(self, en, r, w, acc):
        deps = {}

        def add(st):
            if st is not None:
                deps[st[0]] = max(deps.get(st[0], 0), st[1])
        for b in r:
            add(b.w)
        for b in w:
            if not acc:
                add(b.w)
            for k, v in b.r.items():
                add((k, v))
        for k, v in deps.items():
            if k == "pe" and en == "pe":
                continue
            self._wait(en, k, v)

    @staticmethod
    def _bufs(xs):
        return [x if isinstance(x, Buf) else x.b for x in xs]

    def op(self, en, fn, r=(), w=(), acc=False, inc=True):
        r = self._bufs(r)
        w = self._bufs(w)
        self._deps(en, r, w, acc)
        inst = fn(self.eng[en])
        if inc:
            self.cnt[en] += 1
            inst.then_inc(self.sem[en], 1)
            c = self.cnt[en]
        else:
            c = self.cnt[en] + 1
        for b in r:
            b.r[en] = c
        for b in w:
            b.w = (en, c)
            b.r = {}

    def V(self, fn, r=(), w=()):
        self.op("dve", fn, r, w)

    def A(self, fn, r=(), w=()):
        self.op("act", fn, r, w)

    def G(self, fn, r=(), w=()):
        self.op("pool", fn, r, w)

    def P(self, fn, r=(), w=(), acc=False, inc=True):
        self.op("pe", fn, r, w, acc, inc)

    def dma(self, q, out, in_, r=(), w=(), indirect=None):
        r = self._bufs(r)
        w = self._bufs(w)
        self._deps(q, r, w, False)
        keys, i = self.dq[q]
        key = keys[i % len(keys)]
        self.dq[q][1] = i + 1
        if indirect is not None:
            inst = self.eng[q].indirect_dma_start(out=out, out_offset=None, in_=in_, in_offset=indirect)
        else:
            inst = self.eng[q].dma_start(out=out, in_=in_)
        self.cnt[key] += 16
        inst.then_inc(self.sem[key], 16)
        c = self.cnt[key]
        for b in r:
            b.r[key] = c
        for b in w:
            b.w = (key, c)
            b.r = {}

    def barrier(self):
        for en in self.eng:
            for key in self.sem:
                self._wait(en, key, self.cnt[key])

    def finish(self):
        for key in self.sem:
            self._wait("sp", key, self.cnt[key])


_PH = 3


def build():
    nc = bass.Bass("TRN2", target_bir_lowering=False)

    def din(name, shape, dt=F32):
        return nc.dram_tensor(name, list(shape), dt, kind="ExternalInput").ap()

    def dout(name, shape, dt=F32):
        return nc.dram_tensor(name, list(shape), dt, kind="ExternalOutput").ap()

    xo = din("xo", [NT, D])
    xp = din("xp", [NPF, D])
    s5re = din("s5re", [NS, 128, 64])
    s5im = din("s5im", [NS, 128, 64])
    sgla = din("sgla", [NS, 4, 256, 512])
    cst_d = din("cst", [128, NCST])
    norm_mix_g = din("norm_mix_g", [D])
    w_in = din("w_in", [D, DIN])
    lam_re = din("lam_re", [128, 64])
    lam_im = din("lam_im", [128, 64])
    log_dt = din("log_dt", [128])
    b_re = din("b_re", [128, 64, 16])
    b_im = din("b_im", [128, 64, 16])
    c_re = din("c_re", [128, 16, 64])
    c_im = din("c_im", [128, 16, 64])
    s5_d = din("s5_d", [2048])
    w_glu = din("w_glu", [2048, 2048])
    b_glu = din("b_glu", [2048])
    s5_norm_g = din("s5_norm_g", [2048])
    w_gate2 = din("w_gate2", [16, 1024])
    b_gate2 = din("b_gate2", [1024])
    gla_norm_g = din("gla_norm_g", [2048])
    w_out = din("w_out", [D, D])
    norm_ffn_g = din("norm_ffn_g", [D])
    w_q = din("w_q", [D, 2048])
    keys = din("keys", [16, 128, 128])
    peer_u = din("peer_u", [16384, D])
    peer_v = din("peer_v", [16384, D])
    norm_final_g = din("norm_final_g", [D])

    yo = dout("yo", [NT, D])
    o_s5 = dout("o_s5", [2, 64, 128])
    o_gla = dout("o_gla", [8, 128, 512])
    o_s5s = dout("o_s5s", [NS, 2, 64, 128])
    o_glas = dout("o_glas", [NS, 8, 128, 512])
    def dscr(name, shape, dt=F32):
        return nc.dram_tensor(name, list(shape), dt, kind="Internal").ap()

    h_scr = dscr("h_scr", [NT, D])
    z_scr = dscr("z_scr", [128, 16, NT], BF16)
    v_scr = dscr("v_scr", [NT, 2048], BF16)
    sr_scr = dscr("sr_scr", [NT, 2048], BF16)
    vp_scr = dscr("vp_scr", [NPF, 2048], BF16)
    u_scr = dscr("u_scr", [16, 128, NT])
    up_scr = dscr("up_scr", [16, 128, NPF])
    ub_scr = dscr("ub_scr", [16384, D], BF16)
    vb_scr = dscr("vb_scr", [16384, D], BF16)
    B_ub, B_vb = Buf(), Buf()
    B_h, B_z, B_v, B_sr, B_vp, B_u, B_up = Buf(), Buf(), Buf(), Buf(), Buf(), Buf(), Buf()

    es = contextlib.ExitStack()
    with es:
        kb = KB(nc, es)
        V, A, G, P, dma = kb.V, kb.A, kb.G, kb.P, kb.dma
        big = [es.enter_context(nc.psum_tensor("pbig%d" % i, [128, 1024], F32)) for i in range(4)]

        class PB:
            def __init__(self, t, o):
                self.t_ = t
                self.o = o
                self.b = Buf()

            def __getitem__(self, key):
                rows, colsl = key
                a = colsl.start or 0
                bnd = 512 if colsl.stop is None else colsl.stop
                return self.t_[rows, self.o + a:self.o + bnd]
        banks = [PB(big[i // 2], 512 * (i % 2)) for i in range(8)]

        def nextbank():
            b = banks[kb.rot[kb.roti % len(kb.rot)]]
            kb.roti += 1
            return b

        cst = kb.sb([128, NCST], F32, "cst_sb")
        dma("sp", cst[:], cst_d[:, :], w=[cst])
        ident = cst.t[:, CO_ID:CO_ID + 128]
        jperm = cst.t[:, CO_JP:CO_JP + 128]
        iota256 = cst.t[:, CO_IO:CO_IO + 256]
        ones = cst.t[:, CO_ON:CO_ON + 128]
        identb = kb.sb([128, 128], BF16, "identb")
        V(lambda e: e.tensor_copy(out=identb[:], in_=ident), r=[cst], w=[identb])
        cols = kb.sb([128, 64], F32, "cols")
        HIN = kb.sb([128, 128], F32, "HIN")
        HINI = kb.sb([128, 128], F32, "HINI")
        Sh = [None]
        conv_jobs = [(ub_scr, peer_u, B_ub, i) for i in range(64)] + [(vb_scr, peer_v, B_vb, i) for i in range(64)]

        def conv_step(nmax=1):
            for _ in range(nmax):
                if not conv_jobs:
                    return
                dst, srcd, bb, i = conv_jobs.pop(0)
                dma("pool", dst[256 * i:256 * (i + 1), :], srcd[256 * i:256 * (i + 1), :], w=[bb])
        wsl = []
        wctr = [0]

        @contextlib.contextmanager
        def wslots(n):
            with kb.scope():
                wsl[:] = [kb.sb([128, 32, 256], BF16) for _ in range(n)]
                yield
                wsl[:] = []

        def colvec(vec, nt, dst_ap, neg=False):
            tmp = kb.sb([16, 128], F32)
            dma("sp", tmp[:nt, :], vec.rearrange("(j p) -> j p", p=128), w=[tmp])
            bk = nextbank()
            P(lambda e: e.matmul(bk[:, :nt], lhsT=tmp[:nt, :], rhs=cst.t[:nt, CO_ID:CO_ID + nt], start=True, stop=True),
              r=[tmp, cst], w=[bk])
            if neg:
                V(lambda e: e.tensor_scalar(out=dst_ap, in0=bk[:, :nt], scalar1=-1.0, scalar2=None, op0=ALU.mult),
                  r=[bk], w=[cols])
            else:
                V(lambda e: e.tensor_copy(out=dst_ap, in_=bk[:, :nt]), r=[bk], w=[cols])

        def transp32(dst_ap, dstT, src_ap, srcT):
            bk = nextbank()
            P(lambda e: e.matmul(bk[:, :128], lhsT=src_ap, rhs=ident, start=True, stop=True), r=[srcT, cst], w=[bk])
            A(lambda e: e.activation(out=dst_ap, in_=bk[:, :128], func=AF.Copy), r=[bk], w=[dstT])

        with kb.scope():
            colvec(s5_d, 16, cols.t[:, 0:16])
            colvec(b_glu, 16, cols.t[:, 16:32])
            colvec(s5_norm_g, 16, cols.t[:, 32:48])
            colvec(b_gate2, 8, cols.t[:, 48:56], neg=True)

        def s5_setup(PRV, PIV, BT, CT, AH0, PRP, PIP, NPIP, CTI, AH0I):
          with kb.scope():
            PRV = kb.sb([128, 11, 128], F32, "PRV")
            PIV = kb.sb([128, 11, 128], F32, "PIV")
            N1 = kb.sb([128, 128], F32)
            N2 = kb.sb([128, 128], F32)
            LR = kb.sb([128, 128], F32)
            LI = kb.sb([128, 128], F32)
            DT = kb.sb([128, 128], F32)
            RD = kb.sb([128, 128], F32)
            TH = kb.sb([128, 128], F32)
            AIpm = kb.sb([128, 128], F32)
            dma("sp", N1[:, 0:64], lam_re[:, :], w=[N1])
            dma("sp", N1[:, 64:128], lam_re[:, :], w=[N1])
            dma("sp", N2[:, 0:64], lam_im[:, :], w=[N2])
            dma("sp", N2[:, 64:128], lam_im[:, :], w=[N2])
            transp32(LR[:], LR, N1[:], N1)
            transp32(LI[:], LI, N2[:], N2)
            dma("sp", DT[:], log_dt.partition_broadcast(128), w=[DT])
            A(lambda e: e.activation(out=DT[:], in_=DT[:], func=AF.Exp), r=[DT], w=[DT])
            V(lambda e: e.tensor_tensor(out=RD[:], in0=LR[:], in1=DT[:], op=ALU.mult), r=[LR, DT], w=[RD])
            V(lambda e: e.tensor_tensor(out=TH[:], in0=LI[:], in1=DT[:], op=ALU.mult), r=[LI, DT], w=[TH])
            mag = kb.sb([128, 128], F32)
            thk = kb.sb([128, 128], F32)
            xs_ = kb.sb([128, 128], F32)
            ni = kb.sb([128, 128], I32)
            nf = kb.sb([128, 128], F32)
            rr = kb.sb([128, 128], F32)
            sn = kb.sb([128, 128], F32)
            cs = kb.sb([128, 128], F32)
            AIu = kb.sb([128, 128], F32)

            def sin_of(dst, src, shift):
                V(lambda e: e.tensor_scalar(out=xs_[:], in0=src[:], scalar1=shift, scalar2=1.0 / TWO_PI, op0=ALU.add, op1=ALU.mult),
                  r=[src], w=[xs_])
                V(lambda e: e.tensor_copy(out=ni[:], in_=xs_[:]), r=[xs_], w=[ni])
                V(lambda e: e.tensor_copy(out=nf[:], in_=ni[:]), r=[ni], w=[nf])
                V(lambda e: e.tensor_scalar(out=xs_[:], in0=src[:], scalar1=shift, scalar2=None, op0=ALU.add), r=[src], w=[xs_])
                V(lambda e: e.scalar_tensor_tensor(out=rr[:], in0=nf[:], scalar=-C1, in1=xs_[:], op0=ALU.mult, op1=ALU.add),
                  r=[nf, xs_], w=[rr])
                V(lambda e: e.scalar_tensor_tensor(out=rr[:], in0=nf[:], scalar=-C2, in1=rr[:], op0=ALU.mult, op1=ALU.add),
                  r=[nf, rr], w=[rr])
                V(lambda e: e.tensor_scalar(out=rr[:], in0=rr[:], scalar1=PI, scalar2=-PI, op0=ALU.min, op1=ALU.max), r=[rr], w=[rr])
                A(lambda e: e.activation(out=dst[:], in_=rr[:], func=AF.Sin), r=[rr], w=[dst])

            for k in range(11):
                sc = float(1 << k)
                A(lambda e: e.activation(out=mag[:], in_=RD[:], func=AF.Exp, scale=sc), r=[RD], w=[mag])
                V(lambda e: e.tensor_scalar(out=thk[:], in0=TH[:], scalar1=sc, scalar2=None, op0=ALU.mult), r=[TH], w=[thk])
                sin_of(sn, thk, 0.0)
                sin_of(cs, thk, PI / 2)
                V(lambda e: e.tensor_tensor(out=PRV[:, k, :], in0=mag[:], in1=cs[:], op=ALU.mult), r=[mag, cs], w=[PRV])
                V(lambda e: e.tensor_tensor(out=PIV[:, k, :], in0=mag[:], in1=sn[:], op=ALU.mult), r=[mag, sn], w=[PIV])
                if k == 0:
                    V(lambda e: e.tensor_copy(out=AIu[:], in_=PIV[:, 0, :]), r=[PIV], w=[AIu])
                V(lambda e: e.tensor_scalar(out=PIV[0:64, k, :], in0=PIV[0:64, k, :], scalar1=-1.0, scalar2=None, op0=ALU.mult),
                  r=[PIV], w=[PIV])
                if k == 0:
                    V(lambda e: e.tensor_copy(out=AIpm[:], in_=PIV[:, 0, :]), r=[PIV], w=[AIpm])
            den = kb.sb([128, 128], F32)
            t1 = kb.sb([128, 128], F32)
            t2 = kb.sb([128, 128], F32)
            nr = kb.sb([128, 128], F32)
            QR = kb.sb([128, 128], F32)
            QI = kb.sb([128, 128], F32)
            V(lambda e: e.tensor_tensor(out=den[:], in0=LR[:], in1=LR[:], op=ALU.mult), r=[LR], w=[den])
            V(lambda e: e.tensor_tensor(out=t1[:], in0=LI[:], in1=LI[:], op=ALU.mult), r=[LI], w=[t1])
            V(lambda e: e.tensor_tensor(out=den[:], in0=den[:], in1=t1[:], op=ALU.add), r=[den, t1], w=[den])
            V(lambda e: e.reciprocal(out=den[:], in_=den[:]), r=[den], w=[den])
            V(lambda e: e.tensor_scalar(out=nr[:], in0=PRV[:, 0, :], scalar1=-1.0, scalar2=None, op0=ALU.add), r=[PRV], w=[nr])
            V(lambda e: e.tensor_tensor(out=t1[:], in0=nr[:], in1=LR[:], op=ALU.mult), r=[nr, LR], w=[t1])
            V(lambda e: e.tensor_tensor(out=t2[:], in0=AIu[:], in1=LI[:], op=ALU.mult), r=[AIu, LI], w=[t2])
            V(lambda e: e.tensor_tensor(out=t1[:], in0=t1[:], in1=t2[:], op=ALU.add), r=[t1, t2], w=[t1])
            V(lambda e: e.tensor_tensor(out=QR[:], in0=t1[:], in1=den[:], op=ALU.mult), r=[t1, den], w=[QR])
            V(lambda e: e.tensor_tensor(out=t1[:], in0=AIu[:], in1=LR[:], op=ALU.mult), r=[AIu, LR], w=[t1])
            V(lambda e: e.tensor_tensor(out=t2[:], in0=nr[:], in1=LI[:], op=ALU.mult), r=[nr, LI], w=[t2])
            V(lambda e: e.tensor_tensor(out=t1[:], in0=t1[:], in1=t2[:], op=ALU.subtract), r=[t1, t2], w=[t1])
            V(lambda e: e.tensor_tensor(out=QI[:], in0=t1[:], in1=den[:], op=ALU.mult), r=[t1, den], w=[QI])
            V(lambda e: e.tensor_scalar(out=QI[0:64, :], in0=QI[0:64, :], scalar1=-1.0, scalar2=None, op0=ALU.mult), r=[QI], w=[QI])
            Bsame = kb.sb([128, 128, 16], F32)
            Bswap = kb.sb([128, 128, 16], F32)
            bre_v = b_re.rearrange("g p h -> p g h")
            bim_v = b_im.rearrange("g p h -> p g h")
            dma("sp", Bsame[0:64, :, :], bre_v, w=[Bsame])
            dma("sp", Bsame[64:128, :, :], bim_v, w=[Bsame])
            dma("sp", Bswap[0:64, :, :], bim_v, w=[Bswap])
            dma("sp", Bswap[64:128, :, :], bre_v, w=[Bswap])
            V(lambda e: e.tensor_tensor(out=Bsame[:], in0=Bsame[:], in1=QR[:].unsqueeze(2).to_broadcast([128, 128, 16]), op=ALU.mult),
              r=[Bsame, QR], w=[Bsame])
            V(lambda e: e.tensor_tensor(out=Bswap[:], in0=Bswap[:], in1=QI[:].unsqueeze(2).to_broadcast([128, 128, 16]), op=ALU.mult),
              r=[Bswap, QI], w=[Bswap])
            V(lambda e: e.tensor_tensor(out=Bsame[:], in0=Bsame[:], in1=Bswap[:], op=ALU.add), r=[Bsame, Bswap], w=[Bsame])
            for j in range(16):
                transp32(BT[:, j, :], BT, Bsame[:, 8 * j:8 * j + 8, :].rearrange("p g h -> p (g h)"), Bsame)
            prv4 = PRV.t[:, :, :].rearrange("p k (pr two) -> p k pr two", two=2)
            piv4 = PIV.t[:, :, :].rearrange("p k (pr two) -> p k pr two", two=2)
            V(lambda e: e.tensor_copy(out=PRP[0:64, :, :], in_=prv4[0:64, :, :, 0]), r=[PRV], w=[PRP])
            V(lambda e: e.tensor_copy(out=PRP[64:128, :, :], in_=prv4[64:128, :, :, 1]), r=[PRV], w=[PRP])
            V(lambda e: e.tensor_scalar(out=PIP[0:64, :, :], in0=piv4[0:64, :, :, 0], scalar1=-1.0, scalar2=None, op0=ALU.mult), r=[PIV], w=[PIP])
            V(lambda e: e.tensor_copy(out=PIP[64:128, :, :], in_=piv4[64:128, :, :, 1]), r=[PIV], w=[PIP])
            V(lambda e: e.tensor_scalar(out=NPIP[:, :, :], in0=PIP[:, :, :], scalar1=-1.0, scalar2=None, op0=ALU.mult), r=[PIP], w=[NPIP])
            Cn = kb.sb([128, 16, 128], F32)
            for (srcc, dstT, neg) in ((c_re, CT, False), (c_im, CTI, True)):
                dma("sp", Cn[:, :, 0:64], srcc.rearrange("(j q) h p -> (q h) j p", q=8), r=[CT, CTI], w=[Cn])
                dma("sp", Cn[:, :, 64:128], srcc.rearrange("(j q) h p -> (q h) j p", q=8), w=[Cn])
                for j in range(16):
                    transp32(dstT[:, j, :], dstT, Cn[:, j, :], Cn)
                if neg:
                    V(lambda e: e.tensor_scalar(out=dstT[:, :, :], in0=dstT[:, :, :], scalar1=-1.0, scalar2=None, op0=ALU.mult), r=[dstT], w=[dstT])
            sel0 = cst.t[:, CO_SEL:CO_SEL + 64]
            sel1 = cst.t[:, CO_SEL + 64:CO_SEL + 128]
            Nr = [kb.sb([128, 64], F32) for _ in range(2)]
            Ni = [kb.sb([128, 64], F32) for _ in range(2)]
            for i in range(NS):
                nr_, ni_ = Nr[i % 2], Ni[i % 2]
                dma("sp", nr_[:, :], s5re[i], w=[nr_])
                dma("sp", ni_[:, :], s5im[i], w=[ni_])
                b1 = nextbank()
                P(lambda e: e.matmul(b1[0:64, 0:64], lhsT=nr_[:, :], rhs=sel0, start=True, stop=True), r=[nr_, cst], w=[b1], inc=False)
                P(lambda e: e.matmul(b1[64:128, 0:64], lhsT=nr_[:, :], rhs=sel1, start=True, stop=True), r=[nr_, cst], w=[b1], acc=True)
                b2 = nextbank()
                P(lambda e: e.matmul(b2[0:64, 0:64], lhsT=ni_[:, :], rhs=sel0, start=True, stop=True), r=[ni_, cst], w=[b2], inc=False)
                P(lambda e: e.matmul(b2[64:128, 0:64], lhsT=ni_[:, :], rhs=sel1, start=True, stop=True), r=[ni_, cst], w=[b2], acc=True)
                V(lambda e: e.tensor_tensor(out=t1[:, 0:64], in0=b1[:, 0:64], in1=PRP[:, 0, :], op=ALU.mult), r=[b1, PRP], w=[t1])
                V(lambda e: e.tensor_tensor(out=t2[:, 0:64], in0=b2[:, 0:64], in1=NPIP[:, 0, :], op=ALU.mult), r=[b2, NPIP], w=[t2])
                V(lambda e: e.tensor_tensor(out=AH0[:, i, :], in0=t1[:, 0:64], in1=t2[:, 0:64], op=ALU.add), r=[t1, t2], w=[AH0])
                V(lambda e: e.tensor_tensor(out=t1[:, 0:64], in0=b2[:, 0:64], in1=PRP[:, 0, :], op=ALU.mult), r=[b2, PRP], w=[t1])
                V(lambda e: e.tensor_tensor(out=t2[:, 0:64], in0=b1[:, 0:64], in1=PIP[:, 0, :], op=ALU.mult), r=[b1, PIP], w=[t2])
                V(lambda e: e.tensor_tensor(out=AH0I[:, i, :], in0=t1[:, 0:64], in1=t2[:, 0:64], op=ALU.add), r=[t1, t2], w=[AH0I])

        def bcast_gain(vec, n):
            gt = kb.sb([128, n], F32)
            dma("sp", gt[:], vec.partition_broadcast(128), w=[gt])
            return gt

        def normA(X, XB, tiles, gvec, xnT):
            with kb.scope():
                gt = bcast_gain(gvec, D)
                as_ = [kb.sb([128, D], F32) for _ in range(2)]
                b = kb.sb([128, D], BF16)
                sts_ = [kb.sb([128, 4], F32) for _ in range(2)]
                for i, (t0, m) in enumerate(tiles):
                    a = as_[i % 2]
                    st = sts_[i % 2]
                    dma("sp", a[:m, :], X[t0:t0 + m, :], r=[XB] if XB is not None else [], w=[a])
                    V(lambda e: e.memset(st[:m, :], 0.0), w=[st])
                    A(lambda e: e.activation(out=b[:m, :], in_=a[:m, :], func=AF.Square, accum_out=st[:m, 0:1]), r=[a], w=[b, st])
                    V(lambda e: e.tensor_scalar(out=st[:m, 1:2], in0=st[:m, 0:1], scalar1=1.0 / D, scalar2=EPS, op0=ALU.mult, op1=ALU.add),
                      r=[st], w=[st])
                    A(lambda e: e.activation(out=st[:m, 2:3], in_=st[:m, 1:2], func=AF.Sqrt), r=[st], w=[st])
                    V(lambda e: e.reciprocal(out=st[:m, 3:4], in_=st[:m, 2:3]), r=[st], w=[st])
                    V(lambda e: e.scalar_tensor_tensor(out=b[:m, :], in0=a[:m, :], scalar=st[:m, 3:4], in1=gt[:m, :], op0=ALU.mult, op1=ALU.mult),
                      r=[a, st, gt], w=[b])
                    for q in range(8):
                        bk = nextbank()
                        for k4 in range(4):
                            kc = 4 * q + k4
                            P(lambda e: e.matmul(bk[:, k4 * 128:k4 * 128 + m], lhsT=b[:m, kc * 128:(kc + 1) * 128], rhs=identb[:m, :m],
                                                 start=True, stop=True), r=[b, identb], w=[bk], acc=k4 > 0, inc=(k4 == 3))
                        src = bk[:, :].rearrange("p (k c) -> p k c", c=128)[:, :, :m]
                        if q % 2 == 0:
                            A(lambda e: e.activation(out=xnT[:, 4 * q:4 * q + 4, t0:t0 + m], in_=src, func=AF.Copy), r=[bk], w=[xnT])
                        else:
                            V(lambda e: e.tensor_copy(out=xnT[:, 4 * q:4 * q + 4, t0:t0 + m], in_=src), r=[bk], w=[xnT])

        def loadW(wd, KC, b0, bw):
            W = wsl[wctr[0] % len(wsl)]
            wctr[0] += 1
            dma("pool", W[:, :KC, :bw], wd[:, b0:b0 + bw].rearrange("(kc p) c -> p kc c", p=128), w=[W])
            return W

        def projF(xnT, KC, pieces, wd, c0, ncols, cons):
            for b0 in range(c0, c0 + ncols, 256):
                bw = min(256, c0 + ncols - b0)
                W = loadW(wd, KC, b0, bw)
                for cc in range(0, bw, 128):
                    cw = min(128, bw - cc)
                    for (t0, tn) in pieces:
                        bk = nextbank()
                        for kc in range(KC):
                            P(lambda e: e.matmul(bk[:cw, :tn], lhsT=W[:, kc, cc:cc + cw], rhs=xnT[:, kc, t0:t0 + tn],
                                                 start=(kc == 0), stop=(kc == KC - 1)), r=[W, xnT], w=[bk], acc=kc > 0, inc=(kc == KC - 1))
                        cons((b0 + cc - c0) // 128, cw, t0, tn, bk)

        def projT(xnT, KC, tiles, wd, c0, ncols, cons):
            for b0 in range(c0, c0 + ncols, 256):
                W = loadW(wd, KC, b0, 256)
                for (t0, m) in tiles:
                    bk = nextbank()
                    for kc in range(KC):
                        P(lambda e: e.matmul(bk[:m, :256], lhsT=xnT[:, kc, t0:t0 + m], rhs=W[:, kc, :256],
                                             start=(kc == 0), stop=(kc == KC - 1)), r=[W, xnT], w=[bk], acc=kc > 0, inc=(kc == KC - 1))
                    cons(b0 - c0, 256, t0, m, bk)

        def proj_u(X, tiles, pieces, n, udst, Bu):
            with kb.scope():
                xnT = kb.sb([128, 32, n], BF16)
                normA(X, None, tiles, norm_mix_g, xnT)
                with wslots(2):
                    ust = [kb.sb([128, 512], F32) for _ in range(2)]
                    ctr = [0]

                    def cons_u(ci, cw, t0, tn, bk):
                        u_ = ust[ctr[0] % 2]
                        ctr[0] += 1
                        A(lambda e: e.activation(out=u_[:, :tn], in_=bk[:, :tn], func=AF.Copy), r=[bk], w=[u_])
                        dma("sp", udst[ci, :, t0:t0 + tn], u_[:, :tn], r=[u_], w=[Bu])
                    projF(xnT, 32, pieces, w_in, 0, 2048, cons_u)

        def s5_run(usrc, Bu, n, pieces, own, tabs):
            (PRV, PIV, BT, CT, AH0, HSN, HF, PRP, PIP, NPIP, CTI, AH0I, HSNI, HFI, HINI) = tabs
            with kb.scope():
                Hn = (1 + NT) if own else NPF
                nb_ = 2 if own else 1
                HB = [[[kb.sb([128, Hn], F32) for _ in range(2)] for _ in range(nb_)] for _ in range(2)]
                HD = [[[Buf() for _ in range(2)] for _ in range(nb_)] for _ in range(2)]
                BTm = [[kb.sb([128, 128], F32) for _ in range(2)] for _ in range(2)]
                CTm = [[kb.sb([128, 128], F32) for _ in range(2)] for _ in range(2)]
                uTj = [kb.sb([128, n], F32) for _ in range(2)]
                if own:
                    ysb = kb.sb([128, NT], F32)
                    tg = kb.sb([128, NT], F32)
                    zst = [kb.sb([128, NT], BF16) for _ in range(2)]
                    ybank = [banks[5], banks[6], banks[7]]
                    kb.rot = [0, 1, 2, 3, 4]
                off = 1 if own else 0
                Lt = 1 + LO
                for pp in range(32):
                    prs = (2 * pp, 2 * pp + 1)
                    j = pp // 2
                    uT = uTj[j % 2]
                    conv_step(2)
                    if pp % 2 == 0:
                        dma("sp", uT[:, :], usrc[j, :, :], r=[Bu], w=[uT])
                    for par, pr in enumerate(prs):
                        prl = pr % 4
                        glA, glB = 2 * prl, 2 * prl + 1
                        for c in range(2):
                            bm = BTm[par][c]
                            G(lambda e: e.tensor_scalar(out=bm[:, 0:64], in0=BT[:, j, 64 * c:64 * c + 64], scalar1=cst.t[:, CO_MR + glA:CO_MR + glA + 1],
                                                        scalar2=None, op0=ALU.mult), r=[BT, cst], w=[bm])
                            G(lambda e: e.tensor_scalar(out=bm[:, 64:128], in0=BT[:, j, 64 * c:64 * c + 64], scalar1=cst.t[:, CO_MR + glB:CO_MR + glB + 1],
                                                        scalar2=None, op0=ALU.mult), r=[BT, cst], w=[bm])
                            if own:
                                cm = CTm[par][c]
                                csrc = CT if c == 0 else CTI
                                G(lambda e: e.tensor_tensor(out=cm[:], in0=csrc[:, j, :], in1=cst.t[:, CO_M2 + prl * 128:CO_M2 + (prl + 1) * 128],
                                                            op=ALU.mult), r=[csrc, cst], w=[cm])
                            H0 = HB[par][0][c]
                            for (t0, tn) in pieces:
                                bk = nextbank()
                                P(lambda e: e.matmul(bk[:, :tn], lhsT=bm[:], rhs=uT[:, t0:t0 + tn], start=True, stop=True), r=[bm, uT], w=[bk])
                                A(lambda e: e.activation(out=H0[:, off + t0:off + t0 + tn], in_=bk[:, :tn], func=AF.Copy), r=[bk, HD[par][0][c]], w=[H0])
                            if own:
                                hin = HIN if c == 0 else HINI
                                A(lambda e: e.activation(out=H0[:, 0:1], in_=hin[:, pr:pr + 1], func=AF.Copy), r=[hin], w=[H0])
                    if own:
                        ci = 0
                        for k in range(11):
                            d = 1 << k
                            for par, pr in enumerate(prs):
                                sR, sI = HB[par][ci]
                                dR, dI = HB[par][1 - ci]
                                sRh, sIh = HD[par][ci]
                                dRh, dIh = HD[par][1 - ci]
                                A(lambda e: e.activation(out=dR[:, 0:d], in_=sR[:, 0:d], func=AF.Copy), r=[sR, sRh], w=[dRh])
                                A(lambda e: e.activation(out=dI[:, 0:d], in_=sI[:, 0:d], func=AF.Copy), r=[sI, sIh], w=[dIh])
                            for stage in range(2):
                                for par, pr in enumerate(prs):
                                    sR, sI = HB[par][ci]
                                    dR, dI = HB[par][1 - ci]
                                    sRh, sIh = HD[par][ci]
                                    if stage == 0:
                                        V(lambda e: e.scalar_tensor_tensor(out=dR[:, d:Lt], in0=sR[:, 0:Lt - d], scalar=PRP[:, k, pr:pr + 1], in1=sR[:, d:Lt],
                                                                           op0=ALU.mult, op1=ALU.add), r=[sR, sRh, PRP], w=[dR])
                                        V(lambda e: e.scalar_tensor_tensor(out=dI[:, d:Lt], in0=sI[:, 0:Lt - d], scalar=PRP[:, k, pr:pr + 1], in1=sI[:, d:Lt],
                                                                           op0=ALU.mult, op1=ALU.add), r=[sI, sIh, PRP], w=[dI])
                                    else:
                                        V(lambda e: e.scalar_tensor_tensor(out=dR[:, d:Lt], in0=sI[:, 0:Lt - d], scalar=NPIP[:, k, pr:pr + 1], in1=dR[:, d:Lt],
                                                                           op0=ALU.mult, op1=ALU.add), r=[sI, sIh, NPIP, dR], w=[dR])
                                        V(lambda e: e.scalar_tensor_tensor(out=dI[:, d:Lt], in0=sR[:, 0:Lt - d], scalar=PIP[:, k, pr:pr + 1], in1=dI[:, d:Lt],
                                                                           op0=ALU.mult, op1=ALU.add), r=[sR, sRh, PIP, dI], w=[dI])
                            ci = 1 - ci
                        for par, pr in enumerate(prs):
                            prl = pr % 4
                            for c in range(2):
                                H = HB[par][ci][c]
                                Hh = HD[par][ci][c]
                                H0 = HB[par][0][c]
                                ah = AH0 if c == 0 else AH0I
                                hsn = HSN if c == 0 else HSNI
                                hf = HF if c == 0 else HFI
                                V(lambda e: e.tensor_tensor(out=H[:, 1 + LO:1 + NT], in0=H0[:, 1 + LO:1 + NT], in1=ah[:, :, pr], op=ALU.add),
                                  r=[H0, ah], w=[H])
                                A(lambda e: e.activation(out=hsn[:, :, pr], in_=H[:, 1 + LO:1 + NT], func=AF.Copy), r=[H], w=[hsn])
                                A(lambda e: e.activation(out=hf[:, pr:pr + 1], in_=H[:, LO:LO + 1], func=AF.Copy), r=[H], w=[hf])
                                cm = CTm[par][c]
                                for pi, (t0, tn) in enumerate(pieces):
                                    P(lambda e: e.matmul(ybank[pi][:, :tn], lhsT=cm[:], rhs=H[:, 1 + t0:1 + t0 + tn],
                                                         start=(prl == 0 and c == 0), stop=(prl == 3 and c == 1)), r=[cm, H, Hh], w=[ybank[pi]],
                                      acc=not (prl == 0 and c == 0))
                        if pp % 2 == 1:
                            z_ = zst[j % 2]
                            for pi, (t0, tn) in enumerate(pieces):
                                V(lambda e: e.scalar_tensor_tensor(out=ysb[:, t0:t0 + tn], in0=uT[:, t0:t0 + tn], scalar=cols[:, j:j + 1],
                                                                   in1=ybank[pi][:, :tn], op0=ALU.mult, op1=ALU.add),
                                  r=[uT, cols, ybank[pi]], w=[ysb])
                            A(lambda e: e.activation(out=tg[:], in_=ysb[:], func=AF.Square), r=[ysb], w=[tg])
                            V(lambda e: e.tensor_scalar(out=tg[:], in0=tg[:], scalar1=0.044715, scalar2=1.0, op0=ALU.mult, op1=ALU.add), r=[tg], w=[tg])
                            V(lambda e: e.tensor_tensor(out=tg[:], in0=tg[:], in1=ysb[:], op=ALU.mult), r=[tg, ysb], w=[tg])
                            A(lambda e: e.activation(out=tg[:], in_=tg[:], func=AF.Sigmoid, scale=1.5957691216057308), r=[tg], w=[tg])
                            V(lambda e: e.tensor_tensor(out=z_[:, :], in0=ysb[:], in1=tg[:], op=ALU.mult), r=[tg, ysb], w=[z_])
                            dma("sp", z_scr[:, j, :], z_[:, :], r=[z_], w=[B_z])
                    else:
                        s0 = 0
                        for k in range(9, -1, -1):
                            half = 1 << k
                            lo = slice(s0, s0 + half)
                            hi = slice(s0 + half, s0 + 2 * half)
                            for stage in range(2):
                                for par, pr in enumerate(prs):
                                    R, I = HB[par][0]
                                    if stage == 0:
                                        V(lambda e: e.scalar_tensor_tensor(out=R[:, hi], in0=R[:, lo], scalar=PRP[:, k, pr:pr + 1], in1=R[:, hi],
                                                                           op0=ALU.mult, op1=ALU.add), r=[R, PRP], w=[R])
                                        V(lambda e: e.scalar_tensor_tensor(out=I[:, hi], in0=I[:, lo], scalar=PRP[:, k, pr:pr + 1], in1=I[:, hi],
                                                                           op0=ALU.mult, op1=ALU.add), r=[I, PRP], w=[I])
                                    else:
                                        V(lambda e: e.scalar_tensor_tensor(out=R[:, hi], in0=I[:, lo], scalar=NPIP[:, k, pr:pr + 1], in1=R[:, hi],
                                                                           op0=ALU.mult, op1=ALU.add), r=[I, NPIP, R], w=[R])
                                        V(lambda e: e.scalar_tensor_tensor(out=I[:, hi], in0=R[:, lo], scalar=PIP[:, k, pr:pr + 1], in1=I[:, hi],
                                                                           op0=ALU.mult, op1=ALU.add), r=[R, PIP, I], w=[I])
                            s0 += half
                        for par, pr in enumerate(prs):
                            R, I = HB[par][0]
                            A(lambda e: e.activation(out=HIN[:, pr:pr + 1], in_=R[:, NPF - 1:NPF], func=AF.Copy), r=[R], w=[HIN])
                            A(lambda e: e.activation(out=HINI[:, pr:pr + 1], in_=I[:, NPF - 1:NPF], func=AF.Copy), r=[I], w=[HINI])
                kb.rot = list(range(8))

        def s5_glu_norm():
            with kb.scope():
                zT = kb.sb([128, 16, NT], BF16)
                zg = kb.sb([128, 16, NT], F32)
                dma("sp", zT[:, :, :], z_scr[:, :, :], r=[B_z], w=[zT])
                sg = [kb.sb([128, 352], F32) for _ in range(2)]
                sq = [kb.sb([128, 352], F32) for _ in range(2)]
                rstd = kb.sb([128, NT], F32)
                ctr = [0]

                def cons(ci, cw, t0, tn, bk):
                    s_ = sg[ctr[0] % 2]
                    ctr[0] += 1
                    A(lambda e: e.activation(out=s_[:, :tn], in_=bk[:, :tn], func=AF.Sigmoid, bias=cols[:, 16 + ci:17 + ci], scale=1.0),
                      r=[bk, cols], w=[s_])
                    V(lambda e: e.tensor_tensor(out=zg[:, ci, t0:t0 + tn], in0=zT[:, ci, t0:t0 + tn], in1=s_[:, :tn], op=ALU.mult),
                      r=[zT, s_], w=[zg])
                with wslots(2):
                    projF(zT, 16, OWN_PIECES, w_glu, 0, 2048, cons)
                nb = [banks[5], banks[6], banks[7]]
                kb.rot = [0, 1, 2, 3, 4]
                for ci in range(16):
                    for pi, (t0, tn) in enumerate(OWN_PIECES):
                        s_ = sq[(ci * 3 + pi) % 2]
                        A(lambda e: e.activation(out=s_[:, :tn], in_=zg[:, ci, t0:t0 + tn], func=AF.Square), r=[zg], w=[s_])
                        P(lambda e: e.matmul(nb[pi][:, :tn], lhsT=ones, rhs=s_[:, :tn], start=(ci == 0), stop=(ci == 15)),
                          r=[s_, cst], w=[nb[pi]], acc=ci > 0)
                for pi, (t0, tn) in enumerate(OWN_PIECES):
                    V(lambda e: e.tensor_scalar(out=rstd[:, t0:t0 + tn], in0=nb[pi][:, :tn], scalar1=1.0 / 2048, scalar2=EPS, op0=ALU.mult, op1=ALU.add),
                      r=[nb[pi]], w=[rstd])
                A(lambda e: e.activation(out=rstd[:], in_=rstd[:], func=AF.Sqrt), r=[rstd], w=[rstd])
                V(lambda e: e.reciprocal(out=rstd[:], in_=rstd[:]), r=[rstd], w=[rstd])
                for ci in range(16):
                    V(lambda e: e.scalar_tensor_tensor(out=zT[:, ci, :], in0=zg[:, ci, :], scalar=cols[:, 32 + ci:33 + ci], in1=rstd[:],
                                                       op0=ALU.mult, op1=ALU.mult), r=[zg, cols, rstd], w=[zT])
                dma("sp", z_scr[:, :, :], zT[:, :, :], r=[zT], w=[B_z])
                kb.rot = list(range(8))

        def gla_proj(X, n, pieces, tiles, chunks, own, kT, qT, EB, ebs, vdst, Bv):
            with kb.scope():
                xnT = kb.sb([128, 32, n], BF16)
                normA(X, None, tiles, norm_mix_g, xnT)
                with wslots(2):
                    xgT = kb.sb([16, n], F32)
                    wg2 = kb.sb([16, 1024], F32)
                    e1 = [kb.sb([128, 512], F32) for _ in range(2)]
                    tmp = kb.sb([128, 128], F32)
                    btmp = kb.sb([128, 2, n], F32)
                    vst = [kb.sb([128, 256], BF16) for _ in range(2)]
                    ctr = [0]
                    dma("sp", wg2[:], w_gate2[:, :], w=[wg2])

                    def cons_g(ci, cw, t0, tn, bk):
                        A(lambda e: e.activation(out=xgT[:16, t0:t0 + tn], in_=bk[:16, :tn], func=AF.Copy), r=[bk], w=[xgT])
                    projF(xnT, 32, pieces, w_in, 8192, 16, cons_g)
                    for cp in range(4):
                        for cl in range(2):
                            c8 = 2 * cp + cl
                            for (t0, tn) in pieces:
                                bk = nextbank()
                                P(lambda e: e.matmul(bk[:, :tn], lhsT=wg2[:16, c8 * 128:(c8 + 1) * 128], rhs=xgT[:16, t0:t0 + tn], start=True, stop=True),
                                  r=[wg2, xgT], w=[bk])
                                e_ = e1[ctr[0] % 2]
                                ctr[0] += 1
                                A(lambda e: e.activation(out=e_[:, :tn], in_=bk[:, :tn], func=AF.Exp, scale=-1.0, bias=cols[:, 48 + c8:49 + c8]),
                                  r=[bk, cols], w=[e_])
                                A(lambda e: e.activation(out=btmp[:, cl, t0:t0 + tn], in_=e_[:, :tn], func=AF.Ln, bias=1.0, scale=1.0), r=[e_], w=[btmp])
                            for ci_, (a0, C) in enumerate(chunks):
                                V(lambda e: e.tensor_tensor_scan(out=tmp[:, :C], data0=ones[:, :C], data1=btmp[:, cl, a0:a0 + C], initial=0.0,
                                                                 op0=ALU.mult, op1=ALU.add), r=[btmp, cst], w=[tmp])
                                V(lambda e: e.tensor_scalar(out=btmp[:, cl, a0:a0 + C], in0=tmp[:, :C], scalar1=-1.0 / 16, scalar2=None, op0=ALU.mult),
                                  r=[tmp], w=[btmp])
                                A(lambda e: e.activation(out=EB[:, c8, ci_:ci_ + 1], in_=btmp[:, cl, a0 + C - 1:a0 + C], func=AF.Exp), r=[btmp], w=[EB])
                            if own:
                                V(lambda e: e.tensor_scalar(out=btmp[:, cl, LO:NT], in0=btmp[:, cl, LO:NT], scalar1=-1.0 / 16, scalar2=None, op0=ALU.mult),
                                  r=[btmp], w=[btmp])
                                A(lambda e: e.activation(out=ebs[:, c8, :], in_=btmp[:, cl, LO:NT], func=AF.Exp), r=[btmp], w=[ebs])

                        def cons_k(ci, cw, t0, tn, bk):
                            e_ = e1[ctr[0] % 2]
                            ctr[0] += 1
                            A(lambda e: e.activation(out=e_[:, :tn], in_=btmp[:, ci, t0:t0 + tn], func=AF.Exp, scale=-1.0), r=[btmp], w=[e_])
                            V(lambda e: e.tensor_tensor(out=kT[:, 2 * cp + ci, t0:t0 + tn], in0=bk[:, :tn], in1=e_[:, :tn], op=ALU.mult), r=[bk, e_], w=[kT])
                        projF(xnT, 32, pieces, w_in, 3072 + 256 * cp, 256, cons_k)

                        def cons_q(ci, cw, t0, tn, bk):
                            e_ = e1[ctr[0] % 2]
                            ctr[0] += 1
                            A(lambda e: e.activation(out=e_[:, :tn], in_=btmp[:, ci, t0:t0 + tn], func=AF.Exp, scale=1.0), r=[btmp], w=[e_])
                            V(lambda e: e.scalar_tensor_tensor(out=qT[:, 2 * cp + ci, t0:t0 + tn], in0=bk[:, :tn], scalar=1.0 / 16, in1=e_[:, :tn],
                                                               op0=ALU.mult, op1=ALU.mult), r=[bk, e_], w=[qT])
                        if own:
                            projF(xnT, 32, pieces, w_in, 2048 + 256 * cp, 256, cons_q)

                    def cons_v(cb, cw, t0, m, bk):
                        v_ = vst[ctr[0] % 2]
                        ctr[0] += 1
                        A(lambda e: e.activation(out=v_[:m, :cw], in_=bk[:m, :cw], func=AF.Copy), r=[bk], w=[v_])
                        dma("sp", vdst[t0:t0 + m, cb:cb + cw], v_[:m, :cw], r=[v_], w=[Bv])
                    projT(xnT, 32, tiles, w_in, 4096, 2048, cons_v)

                    def cons_r(cb, cw, t0, m, bk):
                        v_ = vst[ctr[0] % 2]
                        ctr[0] += 1
                        A(lambda e: e.activation(out=v_[:m, :cw], in_=bk[:m, :cw], func=AF.Silu), r=[bk], w=[v_])
                        dma("sp", sr_scr[t0:t0 + m, cb:cb + cw], v_[:m, :cw], r=[v_], w=[B_sr])
                    if own:
                        projT(xnT, 32, tiles, w_in, 6144, 2048, cons_r)

        ep = {}

        def gla_epilogue(C, pso, h, srt, gng, ofin, st):
            jk = ep["jk"]
            tmpo = ep["tmpo"]
            V(lambda e: e.memset(st[:C, :], 0.0), w=[st])
            A(lambda e: e.activation(out=jk[:C, :], in_=pso[:C, :], func=AF.Square, accum_out=st[:C, 0:1]), r=[pso], w=[jk, st])
            V(lambda e: e.tensor_scalar(out=st[:C, 1:2], in0=st[:C, 0:1], scalar1=1.0 / 512, scalar2=EPS, op0=ALU.mult, op1=ALU.add), r=[st], w=[st])
            A(lambda e: e.activation(out=st[:C, 2:3], in_=st[:C, 1:2], func=AF.Sqrt), r=[st], w=[st])
            V(lambda e: e.reciprocal(out=st[:C, 3:4], in_=st[:C, 2:3]), r=[st], w=[st])
            V(lambda e: e.scalar_tensor_tensor(out=tmpo[:C, :], in0=pso[:C, :], scalar=st[:C, 3:4], in1=gng[:C, h * 512:(h + 1) * 512],
                                               op0=ALU.mult, op1=ALU.mult), r=[pso, st, gng], w=[tmpo])
            V(lambda e: e.tensor_tensor(out=ofin[:C, h * 512:(h + 1) * 512], in0=tmpo[:C, :], in1=srt[:C, h * 512:(h + 1) * 512], op=ALU.mult),
              r=[tmpo, srt], w=[ofin])

        def ofin_to_oT(ofin, C, t0, oT):
            for q in range(4):
                bk = nextbank()
                for k4 in range(4):
                    kc = 4 * q + k4
                    P(lambda e: e.matmul(bk[:, k4 * 128:k4 * 128 + C], lhsT=ofin[:C, kc * 128:(kc + 1) * 128], rhs=identb[:C, :C],
                                         start=True, stop=True), r=[ofin, identb], w=[bk], acc=k4 > 0, inc=(k4 == 3))
                src = bk[:, :].rearrange("p (k c) -> p k c", c=128)[:, :, :C]
                A(lambda e: e.activation(out=oT[:, 4 * q:4 * q + 4, t0:t0 + C], in_=src, func=AF.Copy), r=[bk], w=[oT])

        def gla_run(chunks, own, kT, qT, EB, vsrc, Bv, oT, gng):
            S = Sh[0]
            with kb.scope():
                Sb = kb.sb([128, 8, 512], BF16)
                V(lambda e: e.tensor_copy(out=Sb[:], in_=S[:]), r=[S], w=[Sb])
                vt = [kb.sb([128, 2048], BF16) for _ in range(2)]
                srt = [kb.sb([128, 2048], BF16) for _ in range(2)]
                ktok = kb.sb([128, 8, 128], BF16)
                scm = kb.sb([128, 128], BF16)
                ofin = kb.sb([128, 2048], BF16)
                tmpS = kb.sb([128, 512], F32)
                st = kb.sb([128, 4], F32)
                ep["jk"] = kb.sb([128, 512], BF16)
                ep["tmpo"] = kb.sb([128, 512], F32)
                for ci_, (t0, C) in enumerate(chunks):
                    v_ = vt[ci_ % 2]
                    s_ = srt[ci_ % 2]
                    dma("sp", v_[:C, :], vsrc[t0:t0 + C, :], r=[Bv], w=[v_])
                    if own:
                        dma("sp", s_[:C, :], sr_scr[t0:t0 + C, :], r=[B_sr], w=[s_])
                    for half in range(2):
                        bk = nextbank()
                        for k4 in range(4):
                            c8 = half * 4 + k4
                            P(lambda e: e.matmul(bk[:C, k4 * 128:(k4 + 1) * 128], lhsT=kT[:, c8, t0:t0 + C], rhs=identb[:, :], start=True, stop=True),
                              r=[kT, identb], w=[bk], acc=k4 > 0, inc=(k4 == 3))
                        A(lambda e: e.activation(out=ktok[:C, half * 4:half * 4 + 4, :], in_=bk[:C, :].rearrange("p (k c) -> p k c", c=128),
                                                 func=AF.Copy), r=[bk], w=[ktok])
                    for h in range(4):
                        if own:
                            pss = nextbank()
                            for kc in range(2):
                                P(lambda e: e.matmul(pss[:C, :C], lhsT=kT[:, 2 * h + kc, t0:t0 + C], rhs=qT[:, 2 * h + kc, t0:t0 + C],
                                                     start=(kc == 0), stop=(kc == 1)), r=[kT, qT], w=[pss], acc=kc > 0, inc=(kc == 1))
                            V(lambda e: e.tensor_tensor(out=scm[:C, :C], in0=pss[:C, :C], in1=cst.t[:C, CO_MT:CO_MT + C], op=ALU.mult),
                              r=[pss, cst], w=[scm])
                            pso = nextbank()
                            for kc in range(2):
                                P(lambda e: e.matmul(pso[:C, :], lhsT=qT[:, 2 * h + kc, t0:t0 + C], rhs=Sb[:, 2 * h + kc, :],
                                                     start=(kc == 0), stop=False), r=[qT, Sb], w=[pso], acc=kc > 0, inc=False)
                            P(lambda e: e.matmul(pso[:C, :], lhsT=scm[:C, :C], rhs=v_[:C, h * 512:(h + 1) * 512], start=False, stop=True),
                              r=[scm, v_], w=[pso], acc=True)
                            gla_epilogue(C, pso, h, s_, gng, ofin, st)
                        for kc in range(2):
                            c8 = 2 * h + kc
                            psk = nextbank()
                            P(lambda e: e.matmul(psk[:, :], lhsT=ktok[:C, c8, :], rhs=v_[:C, h * 512:(h + 1) * 512], start=True, stop=True),
                              r=[ktok, v_], w=[psk])
                            V(lambda e: e.tensor_tensor(out=tmpS[:, :], in0=psk[:, :], in1=S[:, c8, :], op=ALU.add), r=[psk, S], w=[tmpS])
                            V(lambda e: e.tensor_scalar(out=S[:, c8, :], in0=tmpS[:, :], scalar1=EB[:, c8, ci_:ci_ + 1], scalar2=None, op0=ALU.mult),
                              r=[tmpS, EB], w=[S])
                            A(lambda e: e.activation(out=Sb[:, c8, :], in_=tmpS[:, :], func=AF.Copy, scale=EB[:, c8, ci_:ci_ + 1]), r=[tmpS, EB], w=[Sb])
                    if own:
                        ofin_to_oT(ofin, C, t0, oT)

        def gla_samples(kT, qT, ebs, oT, gng):
            with kb.scope():
                v16 = kb.sb([16, 2048], BF16)
                sr16 = kb.sb([16, 2048], BF16)
                vm = [kb.sb([16, 2048], BF16) for _ in range(2)]
                ktok = kb.sb([16, 8, 128], BF16)
                qm = [kb.sb([128, 16], BF16) for _ in range(2)]
                S0 = [kb.sb([128, 512], F32) for _ in range(3)]
                tmpS = [kb.sb([128, 512], F32) for _ in range(2)]
                tmpb = [kb.sb([128, 512], BF16) for _ in range(2)]
                Sn = [kb.sb([128, 512], F32) for _ in range(3)]
                ofin = kb.sb([16, 2048], BF16)
                st = kb.sb([128, 4], F32)
                ep["jk"] = kb.sb([128, 512], BF16)
                ep["tmpo"] = kb.sb([128, 512], F32)
                pos = [banks[4], banks[5], banks[6], banks[7]]
                kb.rot = [0, 1, 2, 3]
                dma("sp", v16[:, :], v_scr[LO:NT, :], r=[B_v], w=[v16])
                dma("sp", sr16[:, :], sr_scr[LO:NT, :], r=[B_sr], w=[sr16])
                for half in range(2):
                    bk = nextbank()
                    for k4 in range(4):
                        c8 = half * 4 + k4
                        P(lambda e: e.matmul(bk[:NS, k4 * 128:(k4 + 1) * 128], lhsT=kT[:, c8, LO:NT], rhs=identb[:, :], start=True, stop=True),
                          r=[kT, identb], w=[bk], acc=k4 > 0, inc=(k4 == 3))
                    A(lambda e: e.activation(out=ktok[:, half * 4:half * 4 + 4, :], in_=bk[:NS, :].rearrange("p (k c) -> p k c", c=128),
                                             func=AF.Copy), r=[bk], w=[ktok])
                it = 0
                for i in range(NS):
                    vm_ = vm[i % 2]
                    V(lambda e: e.tensor_scalar(out=vm_[:, :], in0=v16[:, :], scalar1=cst.t[:16, CO_ID + i:CO_ID + i + 1], scalar2=None, op0=ALU.mult),
                      r=[v16, cst], w=[vm_])
                    for h in range(4):
                        for kc in range(2):
                            c8 = 2 * h + kc
                            s0_ = S0[it % 3]
                            sn_ = Sn[it % 3]
                            ts_ = tmpS[it % 2]
                            tb_ = tmpb[it % 2]
                            qm_ = qm[it % 2]
                            it += 1
                            dma("sp", s0_[:, :], sgla[i, h, kc * 128:(kc + 1) * 128, :], w=[s0_])
                            psk = nextbank()
                            P(lambda e: e.matmul(psk[:, :], lhsT=ktok[:, c8, :], rhs=vm_[:, h * 512:(h + 1) * 512], start=True, stop=True),
                              r=[ktok, vm_], w=[psk])
                            V(lambda e: e.tensor_tensor(out=ts_[:, :], in0=psk[:, :], in1=s0_[:, :], op=ALU.add), r=[psk, s0_], w=[ts_])
                            V(lambda e: e.tensor_scalar(out=sn_[:, :], in0=ts_[:, :], scalar1=ebs[:, c8, i:i + 1], scalar2=None, op0=ALU.mult),
                              r=[ts_, ebs], w=[sn_])
                            dma("sp", o_glas[i, c8, :, :], sn_[:, :], r=[sn_])
                            A(lambda e: e.activation(out=tb_[:, :], in_=ts_[:, :], func=AF.Copy), r=[ts_], w=[tb_])
                            V(lambda e: e.tensor_scalar(out=qm_[:, :], in0=iota256[:, 0:16], scalar1=float(i), scalar2=None, op0=ALU.is_equal),
                              r=[cst], w=[qm_])
                            V(lambda e: e.tensor_tensor(out=qm_[:, :], in0=qm_[:, :], in1=qT[:, c8, LO:NT], op=ALU.mult), r=[qm_, qT], w=[qm_])
                            first = (i == 0 and kc == 0)
                            last = (i == NS - 1 and kc == 1)
                            P(lambda e: e.matmul(pos[h][:NS, :], lhsT=qm_[:, :], rhs=tb_[:, :], start=first, stop=last),
                              r=[qm_, tb_], w=[pos[h]], acc=not first)
                for h in range(4):
                    gla_epilogue(NS, pos[h], h, sr16, gng, ofin, st)
                kb.rot = list(range(8))
                ofin_to_oT(ofin, NS, LO, oT)

        PHASES = _PH
        with kb.scope():
            PRV = PIV = None
            BT = kb.sb([128, 16, 128], F32, "BT")
            CT = kb.sb([128, 16, 128], F32, "CT")
            AH0 = kb.sb([128, NS, 64], F32, "AH0")
            HSN = kb.sb([128, NS, 64], F32, "HSN")
            HF = kb.sb([128, 128], F32, "HF")
            PRP = kb.sb([128, 11, 64], F32, "PRP")
            PIP = kb.sb([128, 11, 64], F32, "PIP")
            NPIP = kb.sb([128, 11, 64], F32, "NPIP")
            CTI = kb.sb([128, 16, 128], F32, "CTI")
            AH0I = kb.sb([128, NS, 64], F32, "AH0I")
            HSNI = kb.sb([128, NS, 64], F32, "HSNI")
            HFI = kb.sb([128, 128], F32, "HFI")
            tabs = (PRV, PIV, BT, CT, AH0, HSN, HF, PRP, PIP, NPIP, CTI, AH0I, HSNI, HFI, HINI)
            s5_setup(PRV, PIV, BT, CT, AH0, PRP, PIP, NPIP, CTI, AH0I)
            proj_u(xp, PRE_TILES, PRE_PIECES, NPF, up_scr, B_up)
            s5_run(up_scr, B_up, NPF, PRE_PIECES, False, tabs)
            proj_u(xo, OWN_TILES, OWN_PIECES, NT, u_scr, B_u)
            s5_run(u_scr, B_u, NT, OWN_PIECES, True, tabs)
            with kb.scope():
                so = [kb.sb([64, 128], F32) for _ in range(2)]
                oc = [0]

                def out_state(src_ap, srcT, dst_ap):
                    s_ = so[oc[0] % 2]
                    oc[0] += 1
                    bk = nextbank()
                    P(lambda e: e.matmul(bk[0:64, 0:128], lhsT=src_ap, rhs=ident, start=True, stop=True), r=[srcT, cst], w=[bk])
                    A(lambda e: e.activation(out=s_[:, :], in_=bk[0:64, 0:128], func=AF.Copy), r=[bk], w=[s_])
                    dma("sp", dst_ap, s_[:, :], r=[s_])
                out_state(HF[:, 0:64], HF, o_s5[0, :, :])
                out_state(HFI[:, 0:64], HFI, o_s5[1, :, :])
                for i in range(NS):
                    out_state(HSN[:, i, :], HSN, o_s5s[i, 0, :, :])
                    out_state(HSNI[:, i, :], HSNI, o_s5s[i, 1, :, :])
        s5_glu_norm()

        if PHASES >= 2:
          with kb.scope():
            Sh[0] = kb.sb([128, 8, 512], F32, "S")
            G(lambda e: e.memset(Sh[0][:], 0.0), w=[Sh[0]])
            with kb.scope():
                kT = kb.sb([128, 8, NPF], BF16, "kTp")
                EB = kb.sb([128, 8, 16], F32, "EBp")
                gla_proj(xp, NPF, PRE_PIECES, PRE_TILES, PRE_CHUNKS, False, kT, None, EB, None, vp_scr, B_vp)
                gla_run(PRE_CHUNKS, False, kT, None, EB, vp_scr, B_vp, None, None)
            with kb.scope():
                kT = kb.sb([128, 8, NT], BF16, "kTo")
                qT = kb.sb([128, 8, NT], BF16, "qTo")
                EB = kb.sb([128, 8, 16], F32, "EBo")
                ebs = kb.sb([128, 8, NS], F32, "ebs")
                gla_proj(xo, NT, OWN_PIECES, OWN_TILES, OWN_CHUNKS, True, kT, qT, EB, ebs, v_scr, B_v)
                oT = kb.sb([128, 16, NT], BF16, "oT")
                with kb.scope():
                    gng = bcast_gain(gla_norm_g, 2048)
                    gla_run(OWN_CHUNKS, True, kT, qT, EB, v_scr, B_v, oT, gng)
                    for c8 in range(8):
                        dma("sp", o_gla[c8, :, :], Sh[0][:, c8, :], r=[Sh[0]])
                    gla_samples(kT, qT, ebs, oT, gng)
                with wslots(2):
                    zT2 = kb.sb([128, 16, NT], BF16, "zT2")
                    dma("sp", zT2[:, :, :], z_scr[:, :, :], r=[B_z], w=[zT2])
                    xr = [kb.sb([128, 256], F32) for _ in range(3)]
                    ho = [kb.sb([128, 256], F32) for _ in range(3)]
                    it = 0
                    for b0 in range(0, D, 256):
                        W = loadW(w_out, 32, b0, 256)
                        for (t0, m) in OWN_TILES:
                            x_ = xr[it % 3]
                            h_ = ho[it % 3]
                            it += 1
                            dma("sp", x_[:m, :], xo[t0:t0 + m, b0:b0 + 256], w=[x_])
                            bk = nextbank()
                            for kc in range(32):
                                src = zT2 if kc < 16 else oT
                                P(lambda e: e.matmul(bk[:m, :256], lhsT=src[:, kc % 16, t0:t0 + m], rhs=W[:, kc, :256], start=(kc == 0), stop=(kc == 31)),
                                  r=[W, src], w=[bk], acc=kc > 0, inc=(kc == 31))
                            V(lambda e: e.tensor_tensor(out=h_[:m, :], in0=bk[:m, :256], in1=x_[:m, :], op=ALU.add), r=[bk, x_], w=[h_])
                            dma("sp", h_scr[t0:t0 + m, b0:b0 + 256], h_[:m, :], r=[h_], w=[B_h])

        if PHASES >= 3:
          conv_step(1000)
          with kb.scope():
            TV = kb.sb([128, 9, 16, 16], F32, "TV")
            TI = kb.sb([128, 9, 16, 16], F32, "TI")
            with kb.scope():
                xnT = kb.sb([128, 32, NT], BF16, "xnT2")
                normA(h_scr, B_h, OWN_TILES, norm_ffn_g, xnT)
                keysT = kb.sb([128, 16, 128], F32, "keysT")
                kn = [kb.sb([128, 128], F32) for _ in range(2)]
                for c16 in range(16):
                    k_ = kn[c16 % 2]
                    dma("sp", k_[:, :], keys[c16, :, :], w=[k_])
                    transp32(keysT[:, c16, :], keysT, k_[:], k_)
                qTt = kb.sb([128, NT], F32, "qTt")
                sc = [kb.sb([128, 128], F32) for _ in range(2)]
                sc2 = [kb.sb([128, 128], F32) for _ in range(2)]
                m8 = [kb.sb([128, 8], F32) for _ in range(2)]
                i8 = [kb.sb([128, 8], U32) for _ in range(2)]
                ctr = [0]

                def cons_qp(ci, cw, t0, tn, bk):
                    A(lambda e: e.activation(out=qTt[:, t0:t0 + tn], in_=bk[:, :tn], func=AF.Copy), r=[bk], w=[qTt])
                with wslots(2):
                  for c16 in range(16):
                    projF(xnT, 32, OWN_PIECES, w_q, c16 * 128, 128, cons_qp)
                    for ti, (t0, m) in enumerate(OWN_TILES):
                        bk = nextbank()
                        P(lambda e: e.matmul(bk[:m, :128], lhsT=qTt[:, t0:t0 + m], rhs=keysT[:, c16, :], start=True, stop=True),
                          r=[qTt, keysT], w=[bk])
                        s_ = sc[ctr[0] % 2]
                        s2 = sc2[ctr[0] % 2]
                        ma = m8[ctr[0] % 2]
                        ia = i8[ctr[0] % 2]
                        ctr[0] += 1
                        A(lambda e: e.activation(out=s_[:m, :], in_=bk[:m, :128], func=AF.Copy), r=[bk], w=[s_])
                        V(lambda e: e.max(out=ma[:m, :], in_=s_[:m, :]), r=[s_], w=[ma])
                        V(lambda e: e.max_index(out=ia[:m, :], in_max=ma[:m, :], in_values=s_[:m, :]), r=[ma, s_], w=[ia])
                        V(lambda e: e.tensor_copy(out=TV[:m, ti, c16, 0:8], in_=ma[:m, :]), r=[ma], w=[TV])
                        V(lambda e: e.tensor_copy(out=TI[:m, ti, c16, 0:8], in_=ia[:m, :]), r=[ia], w=[TI])
                        V(lambda e: e.match_replace(out=s2[:m, :], in_to_replace=ma[:m, :], in_values=s_[:m, :], imm_value=-1e30), r=[ma, s_], w=[s2])
                        V(lambda e: e.max(out=ma[:m, :], in_=s2[:m, :]), r=[s2], w=[ma])
                        V(lambda e: e.max_index(out=ia[:m, :], in_max=ma[:m, :], in_values=s2[:m, :]), r=[ma, s2], w=[ia])
                        V(lambda e: e.tensor_copy(out=TV[:m, ti, c16, 8:16], in_=ma[:m, :]), r=[ma], w=[TV])
                        V(lambda e: e.tensor_copy(out=TI[:m, ti, c16, 8:16], in_=ia[:m, :]), r=[ia], w=[TI])
            with kb.scope():
                gt_ffn = bcast_gain(norm_ffn_g, D)
                gt_fin = bcast_gain(norm_final_g, D)
                hxs = [kb.sb([128, D], F32, "hx%d" % i) for i in range(2)]
                xnbs = [kb.sb([128, D], F32, "xnb%d" % i) for i in range(2)]
                NR = 6
                rows = [kb.sb([128, D], BF16, "rows%d" % i) for i in range(NR)]
                dg = [kb.sb([128, 128], BF16, "dg%d" % i) for i in range(2)]
                sts = [kb.sb([128, 8], F32) for _ in range(2)]
                cand = kb.sb([128, 16, 16], F32)
                cand2 = kb.sb([128, 256], F32)
                cid = kb.sb([128, 16, 16], F32)
                ma = kb.sb([128, 8], F32)
                ia = kb.sb([128, 8], U32)
                sc16 = kb.sb([128, 8, 16], F32)
                sel = kb.sb([128, 16], F32)
                saf = kb.sb([128, 16], F32)
                sbf = kb.sb([128, 16], F32)
                sai = kb.sb([128, 16], I32)
                i1s = kb.sb([128, 16], F32)
                i2s = kb.sb([128, 16], F32)
                eqs = cid
                eq = kb.sb([128, 16, 256], F32)
                eidfs = [kb.sb([128, 128], F32) for _ in range(3)]
                eidis = [kb.sb([128, 128], I32) for _ in range(3)]
                gws = [kb.sb([128, 128], F32) for _ in range(3)]

                class JK:
                    b = eq.b

                    def __getitem__(self, key):
                        return eq.t[key[0]].rearrange("p a b -> p (a b)").bitcast(BF16)[:, 0:D]
                jk = JK()
                actvs = [kb.sb([128, 128], F32) for _ in range(2)]
                tg = kb.sb([128, 128], F32)
                coefv = [kb.sb([128, 128], F32) for _ in range(2)]
                sm = kb.sb([128, 8, 4], F32)
                ric = [0]

                def prepB(ti):
                    t0, m = OWN_TILES[ti]
                    hx, xnb, st = hxs[ti % 2], xnbs[ti % 2], sts[ti % 2]
                    dma("sp", hx[:m, :], h_scr[t0:t0 + m, :], r=[B_h], w=[hx])
                    V(lambda e: e.memset(st[:m, :], 0.0), w=[st])
                    A(lambda e: e.activation(out=jk[:m, :], in_=hx[:m, :], func=AF.Square, accum_out=st[:m, 0:1]), r=[hx], w=[jk, st])
                    V(lambda e: e.tensor_scalar(out=st[:m, 1:2], in0=st[:m, 0:1], scalar1=1.0 / D, scalar2=EPS, op0=ALU.mult, op1=ALU.add), r=[st], w=[st])
                    A(lambda e: e.activation(out=st[:m, 2:3], in_=st[:m, 1:2], func=AF.Sqrt), r=[st], w=[st])
                    V(lambda e: e.reciprocal(out=st[:m, 3:4], in_=st[:m, 2:3]), r=[st], w=[st])
                    V(lambda e: e.scalar_tensor_tensor(out=xnb[:m, :], in0=hx[:m, :], scalar=st[:m, 3:4], in1=gt_ffn[:m, :], op0=ALU.mult, op1=ALU.mult),
                      r=[hx, st, gt_ffn], w=[xnb])
                    V(lambda e: e.memset(actvs[ti % 2][:m, :], 0.0), w=[actvs[ti % 2]])

                def prepA(ti):
                    t0, m = OWN_TILES[ti]
                    eidf, eidi, gw = eidfs[ti % 3], eidis[ti % 3], gws[ti % 3]
                    for h in range(8):
                        v1 = TV[:m, ti, 2 * h, :]
                        v2 = TV[:m, ti, 2 * h + 1, :]
                        i1 = TI[:m, ti, 2 * h, :]
                        i2 = TI[:m, ti, 2 * h + 1, :]
                        V(lambda e: e.tensor_tensor(out=cand[:m], in0=v1.unsqueeze(2).to_broadcast([m, 16, 16]),
                                                    in1=v2.unsqueeze(1).to_broadcast([m, 16, 16]), op=ALU.add), r=[TV], w=[cand])
                        cflat = cand.t[:m].rearrange("p a b -> p (a b)")
                        for rnd in range(2):
                            srcc = cflat if rnd == 0 else cand2[:m, :]
                            V(lambda e: e.max(out=ma[:m, :], in_=srcc), r=[cand, cand2], w=[ma])
                            V(lambda e: e.max_index(out=ia[:m, :], in_max=ma[:m, :], in_values=srcc), r=[ma, cand, cand2], w=[ia])
                            V(lambda e: e.tensor_copy(out=sc16[:m, h, 8 * rnd:8 * rnd + 8], in_=ma[:m, :]), r=[ma], w=[sc16])
                            V(lambda e: e.tensor_copy(out=sel[:m, 8 * rnd:8 * rnd + 8], in_=ia[:m, :]), r=[ia], w=[sel])
                            if rnd == 0:
                                V(lambda e: e.match_replace(out=cand2[:m, :], in_to_replace=ma[:m, :], in_values=cflat, imm_value=-1e30),
                                  r=[ma, cand], w=[cand2])
                        V(lambda e: e.tensor_scalar(out=saf[:m, :], in0=sel[:m, :], scalar1=-7.5, scalar2=0.0625, op0=ALU.add, op1=ALU.mult), r=[sel], w=[saf])
                        V(lambda e: e.tensor_copy(out=sai[:m, :], in_=saf[:m, :]), r=[saf], w=[sai])
                        V(lambda e: e.tensor_copy(out=saf[:m, :], in_=sai[:m, :]), r=[sai], w=[saf])
                        V(lambda e: e.scalar_tensor_tensor(out=sbf[:m, :], in0=saf[:m, :], scalar=-16.0, in1=sel[:m, :], op0=ALU.mult, op1=ALU.add),
                          r=[saf, sel], w=[sbf])
                        for (idxv, tabv, dstv) in ((saf, i1, i1s), (sbf, i2, i2s)):
                            V(lambda e: e.tensor_tensor(out=eqs[:m], in0=iota256[:m, 0:16].unsqueeze(1).to_broadcast([m, 16, 16]),
                                                        in1=idxv[:m, :].unsqueeze(2).to_broadcast([m, 16, 16]), op=ALU.is_equal), r=[cst, idxv], w=[eqs])
                            V(lambda e: e.tensor_tensor(out=eqs[:m], in0=eqs[:m], in1=tabv.unsqueeze(1).to_broadcast([m, 16, 16]), op=ALU.mult),
                              r=[eqs, TI], w=[eqs])
                            V(lambda e: e.reduce_sum(out=dstv[:m, :], in_=eqs[:m], axis=AX.X), r=[eqs], w=[dstv])
                        V(lambda e: e.scalar_tensor_tensor(out=eidf[:m, 16 * h:16 * h + 16], in0=i1s[:m, :], scalar=128.0, in1=i2s[:m, :],
                                                           op0=ALU.mult, op1=ALU.add), r=[i1s, i2s], w=[eidf])
                        V(lambda e: e.tensor_scalar(out=sm[:m, h, 0:1], in0=sc16[:m, h, 0:1], scalar1=-1.0, scalar2=None, op0=ALU.mult), r=[sc16], w=[sm])
                        V(lambda e: e.memset(sm[:m, h, 1:2], 0.0), w=[sm])
                        A(lambda e: e.activation(out=gw[:m, 16 * h:16 * h + 16], in_=sc16[:m, h, :], func=AF.Exp, bias=sm[:m, h, 0:1], scale=1.0,
                                                 accum_out=sm[:m, h, 1:2]), r=[sc16, sm], w=[gw, sm])
                        V(lambda e: e.reciprocal(out=sm[:m, h, 2:3], in_=sm[:m, h, 1:2]), r=[sm], w=[sm])
                        V(lambda e: e.tensor_scalar(out=gw[:m, 16 * h:16 * h + 16], in0=gw[:m, 16 * h:16 * h + 16], scalar1=sm[:m, h, 2:3], scalar2=None,
                                                    op0=ALU.mult), r=[gw, sm], w=[gw])
                    V(lambda e: e.tensor_copy(out=eidi[:m, :], in_=eidf[:m, :]), r=[eidf], w=[eidi])

                def u_slot(ti, sl):
                    t0, m = OWN_TILES[ti]
                    xnb, eidi, actv = xnbs[ti % 2], eidis[ti % 3], actvs[ti % 2]
                    r_ = rows[ric[0] % NR]
                    ric[0] += 1
                    dma("pool", r_[:m, :], ub_scr[:, :], r=[eidi, B_ub], w=[r_],
                        indirect=bass.IndirectOffsetOnAxis(ap=eidi[:m, sl:sl + 1], axis=0))
                    V(lambda e: e.scalar_tensor_tensor(out=jk[:m, :], in0=r_[:m, :], scalar=1.0, in1=xnb[:m, :], op0=ALU.mult, op1=ALU.mult,
                                                       accum_out=actv[:m, sl:sl + 1]), r=[r_, xnb], w=[jk, actv])

                def coefs(ti):
                    t0, m = OWN_TILES[ti]
                    actv, coef, gw = actvs[ti % 2], coefv[ti % 2], gws[ti % 3]
                    A(lambda e: e.activation(out=tg[:m, :], in_=actv[:m, :], func=AF.Square), r=[actv], w=[tg])
                    V(lambda e: e.tensor_scalar(out=tg[:m, :], in0=tg[:m, :], scalar1=0.044715, scalar2=1.0, op0=ALU.mult, op1=ALU.add), r=[tg], w=[tg])
                    V(lambda e: e.tensor_tensor(out=tg[:m, :], in0=tg[:m, :], in1=actv[:m, :], op=ALU.mult), r=[tg, actv], w=[tg])
                    A(lambda e: e.activation(out=tg[:m, :], in_=tg[:m, :], func=AF.Sigmoid, scale=1.5957691216057308), r=[tg], w=[tg])
                    V(lambda e: e.tensor_tensor(out=coef[:m, :], in0=tg[:m, :], in1=actv[:m, :], op=ALU.mult), r=[tg, actv], w=[coef])
                    V(lambda e: e.tensor_tensor(out=coef[:m, :], in0=coef[:m, :], in1=gw[:m, :], op=ALU.mult), r=[coef, gw], w=[coef])

                vcnt = [0]

                def v_slot(ti):
                    t0, m = OWN_TILES[ti]
                    sl = vcnt[0]
                    vcnt[0] += 1
                    eidi, coef = eidis[ti % 3], coefv[ti % 2]
                    r_ = rows[ric[0] % NR]
                    d_ = dg[ric[0] % 2]
                    ric[0] += 1
                    dma("pool", r_[:m, :], vb_scr[:, :], r=[eidi, B_vb], w=[r_],
                        indirect=bass.IndirectOffsetOnAxis(ap=eidi[:m, sl:sl + 1], axis=0))
                    A(lambda e: e.activation(out=d_[:m, :m], in_=identb[:m, :m], func=AF.Copy, scale=coef[:m, sl:sl + 1]), r=[identb, coef], w=[d_])
                    for c8 in range(8):
                        P(lambda e: e.matmul(banks[c8][:m, :], lhsT=d_[:m, :m], rhs=r_[:m, c8 * 512:(c8 + 1) * 512], start=(sl == 0), stop=(sl == 127)),
                          r=[d_, r_], w=[banks[c8]], acc=sl > 0, inc=(c8 == 7))

                def final(ti):
                    t0, m = OWN_TILES[ti]
                    hx, st = hxs[ti % 2], sts[ti % 2]
                    for c8 in range(8):
                        V(lambda e: e.tensor_tensor(out=hx[:m, c8 * 512:(c8 + 1) * 512], in0=hx[:m, c8 * 512:(c8 + 1) * 512], in1=banks[c8][:m, :], op=ALU.add),
                          r=[hx, banks[c8]], w=[hx])
                    V(lambda e: e.memset(st[:m, 4:8], 0.0), w=[st])
                    A(lambda e: e.activation(out=jk[:m, :], in_=hx[:m, :], func=AF.Square, accum_out=st[:m, 4:5]), r=[hx], w=[jk, st])
                    V(lambda e: e.tensor_scalar(out=st[:m, 5:6], in0=st[:m, 4:5], scalar1=1.0 / D, scalar2=EPS, op0=ALU.mult, op1=ALU.add), r=[st], w=[st])
                    A(lambda e: e.activation(out=st[:m, 6:7], in_=st[:m, 5:6], func=AF.Sqrt), r=[st], w=[st])
                    V(lambda e: e.reciprocal(out=st[:m, 7:8], in_=st[:m, 6:7]), r=[st], w=[st])
                    eqf = eq.t[:m].rearrange("p a b -> p (a b)")
                    V(lambda e: e.scalar_tensor_tensor(out=eqf, in0=hx[:m, :], scalar=st[:m, 7:8], in1=gt_fin[:m, :], op0=ALU.mult, op1=ALU.mult),
                      r=[hx, st, gt_fin], w=[eq])
                    dma("sp", yo[t0:t0 + m, :], eqf, r=[eq])

                NTL = len(OWN_TILES)
                prepA(0)
                prepB(0)
                if NTL > 1:
                    prepA(1)
                for sl in range(128):
                    u_slot(0, sl)
                coefs(0)
                for ti in range(NTL):
                    vcnt[0] = 0
                    nxt = ti + 1 < NTL
                    if nxt:
                        for _ in range(48):
                            v_slot(ti)
                        prepB(ti + 1)
                        if ti + 2 < NTL:
                            prepA(ti + 2)
                        for sl in range(128):
                            u_slot(ti + 1, sl)
                            if sl % 8 < 5:
                                v_slot(ti)
                        assert vcnt[0] == 128
                        coefs(ti + 1)
                    else:
                        for _ in range(128):
                            v_slot(ti)
                    final(ti)
        kb.finish()
    return nc


_NC = [None]


def kernel(x_prompt, x_sample, state_s5_re, state_s5_im, state_gla, meta_tokens,
           norm_mix_g, w_in, s5_lam_re, s5_lam_im, s5_log_dt, s5_b_re, s5_b_im,
           s5_c_re, s5_c_im, s5_d, s5_w_glu, s5_b_glu, s5_norm_g,
           gla_w_gate2, gla_b_gate2, gla_norm_g, w_out, norm_ffn_g,
           peer_w_q, peer_keys, peer_u, peer_v, norm_final_g):
    f = lambda a: np.ascontiguousarray(np.asarray(a, dtype=np.float32))
    x_prompt, x_sample, meta_tokens = f(x_prompt), f(x_sample), f(meta_tokens)
    if _NC[0] is None:
        _NC[0] = build()
    nc = _NC[0]
    shared = {
        "cst": _consts(), "norm_mix_g": f(norm_mix_g)[0], "w_in": f(w_in)[0], "lam_re": f(s5_lam_re)[0], "lam_im": f(s5_lam_im)[0],
        "log_dt": f(s5_log_dt)[0], "b_re": f(s5_b_re)[0], "b_im": f(s5_b_im)[0], "c_re": f(s5_c_re)[0], "c_im": f(s5_c_im)[0],
        "s5_d": f(s5_d)[0], "w_glu": f(s5_w_glu)[0], "b_glu": f(s5_b_glu)[0], "s5_norm_g": f(s5_norm_g)[0],
        "w_gate2": f(gla_w_gate2)[0], "b_gate2": f(gla_b_gate2)[0], "gla_norm_g": f(gla_norm_g)[0], "w_out": f(w_out)[0],
        "norm_ffn_g": f(norm_ffn_g)[0], "w_q": f(peer_w_q)[0], "keys": f(peer_keys)[0].reshape(16, 128, 128),
        "peer_u": f(peer_u)[0], "peer_v": f(peer_v)[0], "norm_final_g": f(norm_final_g),
    }
    s5re, s5im, sgl = f(state_s5_re)[0], f(state_s5_im)[0], f(state_gla)[0]
    in_maps = []
    for c in range(8):
        b, half = divmod(c, 2)
        xs = x_sample[16 * c:16 * c + 16, 0, :]
        if half == 0:
            xo = np.concatenate([meta_tokens, x_prompt[b, :1024], xs], axis=0)
            xp = np.zeros((NPF, D), np.float32)
        else:
            xo = np.concatenate([x_prompt[b, 1008:2048], xs], axis=0)
            xp = np.concatenate([meta_tokens, x_prompt[b, :1008]], axis=0)
        m = dict(shared)
        m.update({"xo": np.ascontiguousarray(xo), "xp": np.ascontiguousarray(xp),
                  "s5re": np.ascontiguousarray(s5re[16 * c:16 * c + 16]), "s5im": np.ascontiguousarray(s5im[16 * c:16 * c + 16]),
                  "sgla": np.ascontiguousarray(sgl[16 * c:16 * c + 16])})
        in_maps.append(m)
    res = run_bass_kernel_spmd(nc, in_maps, core_ids=list(range(8)))
    R = res.results
    y_prompt = np.zeros((4, 2048, D), np.float32)
    y_sample = np.zeros((128, 1, D), np.float32)
    p_re = np.zeros((1, 4, 128, 64), np.float32)
    p_im = np.zeros((1, 4, 128, 64), np.float32)
    p_gla = np.zeros((1, 4, 4, 256, 512), np.float32)
    s_re = np.zeros((1, 128, 128, 64), np.float32)
    s_im = np.zeros((1, 128, 128, 64), np.float32)
    s_gla = np.zeros((1, 128, 4, 256, 512), np.float32)
    for c in range(8):
        b, half = divmod(c, 2)
        r = R[c]
        yo = np.asarray(r["yo"])
        y_prompt[b, 1024 * half:1024 * (half + 1)] = yo[16:1040]
        y_sample[16 * c:16 * c + 16, 0] = yo[1040:1056]
        if half == 1:
            o5 = np.asarray(r["o_s5"])
            p_re[0, b] = o5[0].reshape(128, 64)
            p_im[0, b] = o5[1].reshape(128, 64)
            p_gla[0, b] = np.asarray(r["o_gla"]).reshape(4, 256, 512)
        o5s = np.asarray(r["o_s5s"])
        s_re[0, 16 * c:16 * c + 16] = o5s[:, 0].reshape(16, 128, 64)
        s_im[0, 16 * c:16 * c + 16] = o5s[:, 1].reshape(16, 128, 64)
        s_gla[0, 16 * c:16 * c + 16] = np.asarray(r["o_glas"]).reshape(16, 4, 256, 512)
    return (y_prompt, y_sample, p_re, p_im, p_gla, s_re, s_im, s_gla)
```
